# Optimizing a Trainium2 kernel written in Bass

```python
import math
import jax, jax.numpy as jnp
from jax import lax
import numpy as np

D_MODEL = 1024
BATCH = 4
SEQ = 8192
DEPTH = 2

GRID_W = 64
Q_BLOCK = 128
ROPE_THETA = 10000.0
EPS = 1e-6

MLA_HEADS = 8
MLA_Q_LORA = 512
MLA_KV_LORA = 256
MLA_NOPE = 64
MLA_ROPE = 32
MLA_V = 64
GQA_Q_HEADS = 8
GQA_KV_HEADS = 2
GQA_HEAD_DIM = 64
DIFF_HEADS = 8
DIFF_QK = 64
DIFF_V = 2 * DIFF_QK
REL_BUCKETS = 32
REL_MAX_DIST = 128
MOE_GROUPS = 4
MOE_EXPERTS_PER_GROUP = 8
MOE_EXPERTS = MOE_GROUPS * MOE_EXPERTS_PER_GROUP
MOE_TOP_K = 2
MOE_HIDDEN = 512
MOE_BLOCK = 128

AB_SPLITS = [MLA_Q_LORA, MLA_KV_LORA, MLA_ROPE, GQA_Q_HEADS * GQA_HEAD_DIM, GQA_KV_HEADS * GQA_HEAD_DIM, GQA_KV_HEADS * GQA_HEAD_DIM]
AB_IN = sum(AB_SPLITS)
AB_OUT = MLA_HEADS * MLA_V + GQA_Q_HEADS * GQA_HEAD_DIM
C_IN = DIFF_HEADS * (4 * DIFF_QK + DIFF_V)
C_OUT = DIFF_HEADS * DIFF_V

kernel_name = "hybrid_mla_axialgqa_diffattn_hmoe_encoder"


def _rms(x, g):
    xf = x.astype(jnp.float32)
    y = xf * lax.rsqrt(jnp.mean(xf * xf, axis=-1, keepdims=True) + EPS)
    return (y * g.astype(jnp.float32)).astype(x.dtype)


def _rope_tables(pos, dim):
    inv = ROPE_THETA ** (-jnp.arange(0, dim, 2, dtype=jnp.float32) / dim)
    ang = pos.astype(jnp.float32)[:, None] * inv[None, :]
    ang = jnp.concatenate([ang, ang], axis=-1)
    return jnp.cos(ang), jnp.sin(ang)


def _apply_rope(x, cos, sin):
    half = x.shape[-1] // 2
    rot = jnp.concatenate([-x[..., half:], x[..., :half]], axis=-1)
    return x * cos[None, :, None, :].astype(x.dtype) + rot * sin[None, :, None, :].astype(x.dtype)


def _to_blocks(t):
    b, s = t.shape[:2]
    t = t.reshape((b, s // Q_BLOCK, Q_BLOCK) + t.shape[2:])
    return jnp.moveaxis(t, 1, 0)


def _from_blocks(t):
    t = jnp.moveaxis(t, 0, 1)
    return t.reshape((t.shape[0], t.shape[1] * t.shape[2]) + t.shape[3:])


def _block_softmax_attention(q, k, v):
    b, s, hq, d = q.shape
    hkv = k.shape[2]
    grp = hq // hkv
    dv = v.shape[-1]
    scale = 1.0 / math.sqrt(d)

    def one(qb):
        qg = qb.reshape(b, Q_BLOCK, hkv, grp, d)
        logits = jnp.einsum('bqhgd,bkhd->bhgqk', qg, k).astype(jnp.float32) * scale
        p = jax.nn.softmax(logits, axis=-1).astype(v.dtype)
        o = jnp.einsum('bhgqk,bkhe->bqhge', p, v)
        return o.reshape(b, Q_BLOCK, hq, dv)

    return _from_blocks(lax.map(one, _to_blocks(q)))


def _t5_bucket(rel):
    nb = REL_BUCKETS // 2
    max_exact = nb // 2
    ret = jnp.where(rel > 0, nb, 0)
    n = jnp.abs(rel)
    nf = jnp.maximum(n, 1).astype(jnp.float32)
    large = max_exact + (jnp.log(nf / max_exact) / math.log(REL_MAX_DIST / max_exact) * (nb - max_exact)).astype(jnp.int32)
    large = jnp.minimum(large, nb - 1)
    return ret + jnp.where(n < max_exact, n, large)


def _diff_attention(q, k, v, lam, rel_bias):
    b, s, h, _, d = q.shape
    scale = 1.0 / math.sqrt(d)
    kpos = jnp.arange(s, dtype=jnp.int32)

    def one(args):
        qb, start = args
        qpos = start + jnp.arange(Q_BLOCK, dtype=jnp.int32)
        bias = rel_bias[_t5_bucket(kpos[None, :] - qpos[:, None])]
        bias = jnp.transpose(bias, (2, 0, 1)).astype(jnp.float32)
        logits = jnp.einsum('bqhcd,bkhcd->bhcqk', qb, k).astype(jnp.float32) * scale + bias[None, :, None]
        p = jax.nn.softmax(logits, axis=-1)
        a = (p[:, :, 0] - lam * p[:, :, 1]).astype(v.dtype)
        return jnp.einsum('bhqk,bkhe->bqhe', a, v)

    starts = jnp.arange(s // Q_BLOCK, dtype=jnp.int32) * Q_BLOCK
    return _from_blocks(lax.map(one, (_to_blocks(q), starts)))


def _mixer_ab(h, w_in, q_a_norm, w_qb, kv_a_norm, w_kvb, mla_qn, mla_kn, gqa_qn, gqa_kn, w_out):
    b, s, _ = h.shape
    idx = [int(v) for v in np.cumsum(AB_SPLITS)[:-1]]
    cq, ckv, k_pe, qg, kg, vg = jnp.split(h @ w_in, idx, axis=-1)
    q = (_rms(cq, q_a_norm) @ w_qb).reshape(b, s, MLA_HEADS, MLA_NOPE + MLA_ROPE)
    kv = (_rms(ckv, kv_a_norm) @ w_kvb).reshape(b, s, MLA_HEADS, MLA_NOPE + MLA_V)
    k_nope, v_a = kv[..., :MLA_NOPE], kv[..., MLA_NOPE:]
    k_pe = jnp.broadcast_to(k_pe[:, :, None, :], (b, s, MLA_HEADS, MLA_ROPE))
    k = jnp.concatenate([k_nope, k_pe], axis=-1)
    q = _rms(q, mla_qn)
    k = _rms(k, mla_kn)
    cos, sin = _rope_tables(jnp.arange(s, dtype=jnp.int32), MLA_ROPE)
    q = jnp.concatenate([q[..., :MLA_NOPE], _apply_rope(q[..., MLA_NOPE:], cos, sin)], axis=-1)
    k = jnp.concatenate([k[..., :MLA_NOPE], _apply_rope(k[..., MLA_NOPE:], cos, sin)], axis=-1)
    o_a = _block_softmax_attention(q, k, v_a).reshape(b, s, MLA_HEADS * MLA_V)
    rows = s // GRID_W
    row = jnp.broadcast_to(jnp.arange(rows, dtype=jnp.int32)[:, None], (rows, GRID_W)).reshape(s)
    col = jnp.broadcast_to(jnp.arange(GRID_W, dtype=jnp.int32)[None, :], (rows, GRID_W)).reshape(s)
    half = GQA_HEAD_DIM // 2
    rc, rs = _rope_tables(row, half)
    cc, cs = _rope_tables(col, half)

    def axial(t):
        return jnp.concatenate([_apply_rope(t[..., :half], rc, rs), _apply_rope(t[..., half:], cc, cs)], axis=-1)

    qb = axial(_rms(qg.reshape(b, s, GQA_Q_HEADS, GQA_HEAD_DIM), gqa_qn))
    kb = axial(_rms(kg.reshape(b, s, GQA_KV_HEADS, GQA_HEAD_DIM), gqa_kn))
    vb = vg.reshape(b, s, GQA_KV_HEADS, GQA_HEAD_DIM)
    o_b = _block_softmax_attention(qb, kb, vb).reshape(b, s, GQA_Q_HEADS * GQA_HEAD_DIM)
    return jnp.concatenate([o_a, o_b], axis=-1) @ w_out


def _mixer_c(h, w_in, qn, kn, lam_q1, lam_k1, lam_q2, lam_k2, subln, w_out, rel_bias, lambda_init):
    b, s, _ = h.shape
    nq = DIFF_HEADS * 2 * DIFF_QK
    proj = h @ w_in
    q = _rms(proj[..., :nq].reshape(b, s, DIFF_HEADS, 2, DIFF_QK), qn)
    k = _rms(proj[..., nq:2 * nq].reshape(b, s, DIFF_HEADS, 2, DIFF_QK), kn)
    v = proj[..., 2 * nq:].reshape(b, s, DIFF_HEADS, DIFF_V)
    f32 = jnp.float32
    lam = (jnp.exp(jnp.sum(lam_q1.astype(f32) * lam_k1.astype(f32)))
           - jnp.exp(jnp.sum(lam_q2.astype(f32) * lam_k2.astype(f32))) + lambda_init)
    o = _diff_attention(q, k, v, lam, rel_bias)
    o = _rms(o, subln) * (1.0 - lambda_init)
    return o.reshape(b, s, C_OUT) @ w_out


def _hier_moe(h, w_group, b_group, w_expert, b_expert, w_up, w_down):
    b, s, d = h.shape
    t = b * s
    xt = h.reshape(t, d)
    pg = jax.nn.softmax((xt @ w_group).astype(jnp.float32) + b_group.astype(jnp.float32), axis=-1)
    pg_top, g_idx = lax.top_k(pg, 1)
    g = g_idx[:, 0]
    el = jnp.einsum('td,gde->tge', xt, w_expert).astype(jnp.float32) + b_expert.astype(jnp.float32)
    el = el[jnp.arange(t), g]
    pe = jax.nn.softmax(el, axis=-1)
    pe_top, e_top = lax.top_k(pe, MOE_TOP_K)
    gate = pg_top * pe_top / jnp.sum(pe_top, axis=-1, keepdims=True)
    expert = g[:, None] * MOE_EXPERTS_PER_GROUP + e_top
    n = t * MOE_TOP_K
    p_rows = n + MOE_EXPERTS * MOE_BLOCK
    n_blocks = p_rows // MOE_BLOCK
    flat_e = expert.reshape(n).astype(jnp.int32)
    flat_w = gate.reshape(n)
    flat_t = jnp.arange(n, dtype=jnp.int32) // MOE_TOP_K
    order = jnp.argsort(flat_e)
    se = flat_e[order]
    counts = jnp.zeros((MOE_EXPERTS,), jnp.int32).at[flat_e].add(1)
    starts = jnp.cumsum(counts) - counts
    padded = (counts + MOE_BLOCK - 1) // MOE_BLOCK * MOE_BLOCK
    pad_end = jnp.cumsum(padded)
    pad_start = pad_end - padded
    dest = pad_start[se] + (jnp.arange(n, dtype=jnp.int32) - starts[se])
    slot_t = jnp.zeros((p_rows,), jnp.int32).at[dest].set(flat_t[order])
    slot_w = jnp.zeros((p_rows,), jnp.float32).at[dest].set(flat_w[order])
    block_e = jnp.minimum(jnp.searchsorted(pad_end, jnp.arange(n_blocks, dtype=jnp.int32) * MOE_BLOCK, side='right'), MOE_EXPERTS - 1)
    xs = xt[slot_t].reshape(n_blocks, MOE_BLOCK, d)

    def expert_block(args):
        xb, e = args
        gu = xb @ w_up[e]
        return (jax.nn.silu(gu[:, :MOE_HIDDEN]) * gu[:, MOE_HIDDEN:]) @ w_down[e]

    ys = lax.map(expert_block, (xs, block_e)).reshape(p_rows, d)
    out = jax.ops.segment_sum(ys * slot_w[:, None].astype(ys.dtype), slot_t, num_segments=t)
    return out.reshape(b, s, d)


def setup_inputs(seed: int = 0) -> dict:
    key = jax.random.key(seed)
    keys = jax.random.split(key, 40)
    counter = [0]

    def nrm(shape, scale):
        k = keys[counter[0]]
        counter[0] += 1
        return jax.random.normal(k, shape, jnp.float32) * scale

    def gain(shape):
        return 1.0 + nrm(shape, 0.02)

    D = D_MODEL
    n_even = (DEPTH + 1) // 2
    n_odd = DEPTH // 2
    return {
        "x": nrm((BATCH, SEQ, D), 1.0),
        "c": nrm((BATCH, D), 1.0),
        "ada_w": nrm((DEPTH, D, 6 * D), 0.5 * D ** -0.5),
        "ada_b": nrm((DEPTH, 6 * D), 0.02),
        "norm_mix": gain((DEPTH, D)),
        "norm_ffn": gain((DEPTH, D)),
        "rel_bias": nrm((REL_BUCKETS, DIFF_HEADS), 0.5),
        "ab_w_in": nrm((n_even, D, AB_IN), D ** -0.5),
        "ab_q_a_norm": gain((n_even, MLA_Q_LORA)),
        "ab_w_qb": nrm((n_even, MLA_Q_LORA, MLA_HEADS * (MLA_NOPE + MLA_ROPE)), MLA_Q_LORA ** -0.5),
        "ab_kv_a_norm": gain((n_even, MLA_KV_LORA)),
        "ab_w_kvb": nrm((n_even, MLA_KV_LORA, MLA_HEADS * (MLA_NOPE + MLA_V)), MLA_KV_LORA ** -0.5),
        "ab_mla_qn": gain((n_even, MLA_NOPE + MLA_ROPE)),
        "ab_mla_kn": gain((n_even, MLA_NOPE + MLA_ROPE)),
        "ab_gqa_qn": gain((n_even, GQA_HEAD_DIM)),
        "ab_gqa_kn": gain((n_even, GQA_HEAD_DIM)),
        "ab_w_out": nrm((n_even, AB_OUT, D), AB_OUT ** -0.5),
        "c_w_in": nrm((n_odd, D, C_IN), D ** -0.5),
        "c_qn": gain((n_odd, 2, DIFF_QK)),
        "c_kn": gain((n_odd, 2, DIFF_QK)),
        "c_lam_q1": nrm((n_odd, DIFF_QK), 0.1),
        "c_lam_k1": nrm((n_odd, DIFF_QK), 0.1),
        "c_lam_q2": nrm((n_odd, DIFF_QK), 0.1),
        "c_lam_k2": nrm((n_odd, DIFF_QK), 0.1),
        "c_subln": gain((n_odd, DIFF_V)),
        "c_w_out": nrm((n_odd, C_OUT, D), C_OUT ** -0.5),
        "moe_w_group": nrm((DEPTH, D, MOE_GROUPS), D ** -0.5),
        "moe_b_group": nrm((DEPTH, MOE_GROUPS), 0.01),
        "moe_w_expert": nrm((DEPTH, MOE_GROUPS, D, MOE_EXPERTS_PER_GROUP), D ** -0.5),
        "moe_b_expert": nrm((DEPTH, MOE_GROUPS, MOE_EXPERTS_PER_GROUP), 0.01),
        "moe_w_up": nrm((DEPTH, MOE_EXPERTS, D, 2 * MOE_HIDDEN), D ** -0.5),
        "moe_w_down": nrm((DEPTH, MOE_EXPERTS, MOE_HIDDEN, D), MOE_HIDDEN ** -0.5),
    }


def reference(x, c, ada_w, ada_b, norm_mix, norm_ffn, rel_bias,
              ab_w_in, ab_q_a_norm, ab_w_qb, ab_kv_a_norm, ab_w_kvb, ab_mla_qn, ab_mla_kn,
              ab_gqa_qn, ab_gqa_kn, ab_w_out,
              c_w_in, c_qn, c_kn, c_lam_q1, c_lam_k1, c_lam_q2, c_lam_k2, c_subln, c_w_out,
              moe_w_group, moe_b_group, moe_w_expert, moe_b_expert, moe_w_up, moe_w_down):
    cond = jax.nn.silu(c)
    for l in range(DEPTH):
        mod = (cond @ ada_w[l] + ada_b[l])[:, None, :]
        shift1, scale1, gate1, shift2, scale2, gate2 = jnp.split(mod, 6, axis=-1)
        h = _rms(x, norm_mix[l]) * (1.0 + scale1) + shift1
        i = l // 2
        if l % 2 == 0:
            y = _mixer_ab(h, ab_w_in[i], ab_q_a_norm[i], ab_w_qb[i], ab_kv_a_norm[i], ab_w_kvb[i],
                          ab_mla_qn[i], ab_mla_kn[i], ab_gqa_qn[i], ab_gqa_kn[i], ab_w_out[i])
        else:
            lambda_init = 0.8 - 0.6 * math.exp(-0.3 * l)
            y = _mixer_c(h, c_w_in[i], c_qn[i], c_kn[i], c_lam_q1[i], c_lam_k1[i], c_lam_q2[i], c_lam_k2[i],
                         c_subln[i], c_w_out[i], rel_bias, lambda_init)
        x = x + gate1 * y
        h = _rms(x, norm_ffn[l]) * (1.0 + scale2) + shift2
        x = x + gate2 * _hier_moe(h, moe_w_group[l], moe_b_group[l], moe_w_expert[l], moe_b_expert[l],
                                   moe_w_up[l], moe_w_down[l])
    return x
```

```python
import math
from contextlib import ExitStack

import numpy as np
import concourse.bass as bass
import concourse.mybir as mybir
from concourse.bass_utils import run_bass_kernel_spmd

F32 = mybir.dt.float32
BF16 = mybir.dt.bfloat16
I32 = mybir.dt.int32
ALU = mybir.AluOpType
AF = mybir.ActivationFunctionType
AX = mybir.AxisListType

D = 1024
S = 8192
SH = 4096
NT = 64
NTO = 32
EPS = 1e-6
AB_IN = 1568


class Buf:
    __slots__ = ("name", "w", "r", "sb")

    def __init__(self, name):
        self.name = name
        self.w = {}
        self.r = {}
        self.sb = False


class T:
    __slots__ = ("ap", "buf")

    def __init__(self, ap, buf):
        self.ap = ap
        self.buf = buf

    def __getitem__(self, k):
        return T(self.ap[k], self.buf)

    def rearrange(self, s, **kw):
        return T(self.ap.rearrange(s, **kw), self.buf)

    def bitcast(self, dt):
        return T(self.ap.bitcast(dt), self.buf)

    def bcast(self, shape):
        return T(self.ap.to_broadcast(list(shape)), self.buf)

    def unsq(self, ax):
        return T(self.ap.unsqueeze(ax), self.buf)

    def pbc(self, n=128):
        return T(self.ap.partition_broadcast(n), self.buf)

    @property
    def shape(self):
        return self.ap.shape


def _ap(x):
    return x.ap if isinstance(x, T) else x


def _bufs(*xs):
    out = []
    for x in xs:
        if isinstance(x, T) and x.buf not in out:
            out.append(x.buf)
    return out


class Prog:
    ENG = ("pe", "act", "dve", "pool", "sp")

    def __init__(self, nc, es):
        self.nc = nc
        self.es = es
        self.q = {e: [] for e in self.ENG}
        self.esem = {e: es.enter_context(nc.semaphore("s_" + e)) for e in ("pe", "act", "dve", "pool")}
        self.ecnt = {e: 0 for e in ("pe", "act", "dve", "pool")}
        self.waited = {e: {} for e in self.ENG}
        self.dsems = {}
        self.free_dsems = []
        self.bg = set()
        self.nsem = 0
        self.nbuf = 0

    def buf(self, name=None):
        self.nbuf += 1
        return Buf(name or ("b%d" % self.nbuf))

    def _dsem(self, name):
        if name not in self.dsems:
            if self.free_dsems:
                self.dsems[name] = self.free_dsems.pop()
            else:
                self.nsem += 1
                self.dsems[name] = [self.es.enter_context(self.nc.semaphore("d_%d" % self.nsem)), 0]
        return self.dsems[name]

    def op(self, eng, fn, reads=(), writes=(), dma=None, n=1, accum_w=False, dma_inc=16, noinc=False):
        need = {}

        def add(tok):
            sem, val, src = tok
            if src == "pe" and eng == "pe":
                return
            k = id(sem)
            if k not in need or need[k][1] < val:
                need[k] = (sem, val)

        for b in reads:
            for tok in b.w.values():
                add(tok)
        for b in writes:
            if not accum_w:
                for tok in b.w.values():
                    add(tok)
            for tok in b.r.values():
                add(tok)
        waits = []
        wd = self.waited[eng]
        for k, (sem, val) in need.items():
            if wd.get(k, 0) < val:
                wd[k] = val
                waits.append((sem, val))
        if dma is not None:
            rec = self._dsem(dma)
            rec[1] += dma_inc * n
            tok = (rec[0], rec[1], "dma")
            inc = (rec[0], dma_inc)
        else:
            self.ecnt[eng] += 1
            tok = (self.esem[eng], self.ecnt[eng], eng)
            inc = (self.esem[eng], 1)
        if noinc:
            self.ecnt[eng] -= 1
            self.q[eng].append((waits, fn, None))
            return None
        self.q[eng].append((waits, fn, inc))
        k = id(tok[0])
        for b in reads:
            b.r[k] = tok
        for b in writes:
            if accum_w:
                b.w[k] = tok
            else:
                b.w = {k: tok}
            b.r = {}
        return tok

    def mm(self, out, lhsT, rhs, start=True, stop=True, extra_r=()):
        o, a, b = _ap(out), _ap(lhsT), _ap(rhs)
        self.op("pe", lambda e: e.matmul(o, lhsT=a, rhs=b, start=start, stop=stop),
                reads=_bufs(lhsT, rhs) + list(extra_r), writes=_bufs(out))

    def tr(self, out, in_, ident):
        o, a, i = _ap(out), _ap(in_), _ap(ident)
        self.op("pe", lambda e: e.transpose(o, a, i), reads=_bufs(in_, ident), writes=_bufs(out))

    def act(self, out, in_, func, bias=None, scale=None, accum=None):
        o, a = _ap(out), _ap(in_)
        kw = {}
        if bias is not None:
            kw["bias"] = _ap(bias)
        if scale is not None:
            kw["scale"] = _ap(scale)
        if accum is not None:
            kw["accum_out"] = _ap(accum)
        self.op("act", lambda e: e.activation(out=o, in_=a, func=func, **kw),
                reads=_bufs(in_, bias, scale), writes=_bufs(out, accum))

    def tt(self, eng, out, in0, in1, op):
        o, a, b = _ap(out), _ap(in0), _ap(in1)
        self.op(eng, lambda e: e.tensor_tensor(out=o, in0=a, in1=b, op=op),
                reads=_bufs(in0, in1), writes=_bufs(out))

    def ts(self, eng, out, in0, s1, s2, op0, op1=None):
        o, a, x1, x2 = _ap(out), _ap(in0), _ap(s1), _ap(s2)
        if op1 is None:
            self.op(eng, lambda e: e.tensor_scalar(out=o, in0=a, scalar1=x1, scalar2=None, op0=op0),
                    reads=_bufs(in0, s1), writes=_bufs(out))
        else:
            self.op(eng, lambda e: e.tensor_scalar(out=o, in0=a, scalar1=x1, scalar2=x2, op0=op0, op1=op1),
                    reads=_bufs(in0, s1, s2), writes=_bufs(out))

    def stt(self, eng, out, in0, scalar, in1, op0, op1):
        o, a, s, b = _ap(out), _ap(in0), _ap(scalar), _ap(in1)
        self.op(eng, lambda e: e.scalar_tensor_tensor(out=o, in0=a, scalar=s, in1=b, op0=op0, op1=op1),
                reads=_bufs(in0, scalar, in1), writes=_bufs(out))

    def copy(self, eng, out, in_):
        o, a = _ap(out), _ap(in_)
        if eng == "act":
            self.op(eng, lambda e: e.activation(out=o, in_=a, func=AF.Copy), reads=_bufs(in_), writes=_bufs(out))
        else:
            self.op(eng, lambda e: e.tensor_copy(out=o, in_=a), reads=_bufs(in_), writes=_bufs(out))

    def memset(self, eng, out, val):
        o = _ap(out)
        self.op(eng, lambda e: e.memset(o, val), writes=_bufs(out))

    def reduce(self, eng, out, in_, op=ALU.add, axis=AX.X):
        o, a = _ap(out), _ap(in_)
        self.op(eng, lambda e: e.tensor_reduce(out=o, in_=a, axis=axis, op=op), reads=_bufs(in_), writes=_bufs(out))

    def recip(self, out, in_):
        o, a = _ap(out), _ap(in_)
        self.op("dve", lambda e: e.reciprocal(out=o, in_=a), reads=_bufs(in_), writes=_bufs(out))

    def dma(self, eng, out, in_, sem=None, accum_w=False):
        o, a = _ap(out), _ap(in_)
        if sem is None:
            sb = out if (isinstance(out, T) and getattr(out.buf, "sb", False)) else in_
            sem = sb.buf.name
        self.op(eng, lambda e: e.dma_start(out=o, in_=a), reads=_bufs(in_), writes=_bufs(out), dma=sem, accum_w=accum_w)

    def barrier(self):
        toks = [(self.esem[e], self.ecnt[e]) for e in self.esem if self.ecnt[e] > 0]
        toks += [(rec[0], rec[1]) for name, rec in self.dsems.items() if rec[1] > 0 and name not in self.bg]
        for eng in self.ENG:
            waits = []
            wd = self.waited[eng]
            for sem, val in toks:
                if wd.get(id(sem), 0) < val:
                    wd[id(sem)] = val
                    waits.append((sem, val))
            if waits:
                self.q[eng].append((waits, None, None))
        self.free_dsems.extend(r for n_, r in self.dsems.items() if n_ not in self.bg)
        self.dsems = {n_: r for n_, r in self.dsems.items() if n_ in self.bg}

    def emit(self):
        nc = self.nc
        block = self.es.enter_context(nc.Block())

        def run(engname):
            def f(e):
                for waits, fn, inc in self.q[engname]:
                    for sem, val in waits:
                        e.wait_ge(sem, val)
                    if fn is not None:
                        ins = fn(e)
                        if inc is not None:
                            ins.then_inc(inc[0], inc[1])
            return f

        block.tensor(run("pe"))
        block.scalar(run("act"))
        block.vector(run("dve"))
        block.gpsimd(run("pool"))
        block.sync(run("sp"))


class Mem:
    def __init__(self, nc, es, P, sbuf_words):
        self.P = P
        self.sb = es.enter_context(nc.sbuf_tensor("arena", [128, sbuf_words], F32))
        self.ps = es.enter_context(nc.psum_tensor("psum", [128, 4096], F32))
        self.words = sbuf_words
        self.off = 0
        self.keep = 0

    def reset(self):
        self.off = self.keep

    def alloc(self, free_shape, dt=F32, name=None, parts=128):
        n = 1
        for s in free_shape:
            n *= s
        sz = 4 if dt in (F32, I32) else 2
        words = (n * sz + 3) // 4
        words = (words + 7) // 8 * 8
        assert self.off + words <= self.words, ("SBUF arena overflow", name, self.off, words, self.words)
        ap = self.sb[0:parts, self.off:self.off + words]
        self.off += words
        if dt != F32:
            ap = ap.bitcast(dt)
        ap = ap[:, 0:n]
        if len(free_shape) == 2:
            ap = ap.rearrange("p (a b) -> p a b", a=free_shape[0], b=free_shape[1])
        elif len(free_shape) == 3:
            ap = ap.rearrange("p (a b c) -> p a b c", a=free_shape[0], b=free_shape[1], c=free_shape[2])
        b = self.P.buf(name)
        b.sb = True
        return T(ap, b)

    def psum(self, col0, ncols, name=None, dt=F32):
        ap = self.ps[:, col0:col0 + ncols]
        if dt != F32:
            ap = ap.bitcast(dt)
        b = self.P.buf(name)
        return T(ap, b)


def dram(nc, P, name, shape, dt, kind="Internal"):
    t = nc.dram_tensor(name, list(shape), dt, kind=kind)
    return T(t.ap(), P.buf(name))


def emit_consts(P, M, io):
    idb = M.alloc([128], BF16, "idb")
    idf = M.alloc([128], F32, "idf")
    P.dma("sp", idf, io["ident"])
    P.copy("dve", idb, idf)
    return {"idb": idb}


def emit_mod(P, M, io, l, need_a, need_b):
    outA = {c: M.alloc([8], F32, "modA%d" % c) for c in need_a}
    outB = {c: M.alloc([1024], F32, "modB%d" % c) for c in need_b}
    keep = M.off
    cc = M.alloc([8], F32, "c_col")
    P.dma("sp", cc, io["c_a"])
    cond = M.alloc([8], F32, "cond")
    P.act(cond, cc, AF.Silu)
    crep = M.alloc([8, 128], F32, "cond_rep")
    P.copy("dve", crep, cond.unsq(2).bcast([128, 8, 128]))
    adab_a = M.alloc([48], F32, "adab_a")
    P.dma("sp", adab_a, io["ada_b_a"][l])
    wbuf = [M.alloc([8, 1024], F32, "adaw%d" % i) for i in range(2)]
    psA = M.psum(0, 8, "psA")
    psB = M.psum(512, 1024, "psB")
    chunks = sorted(set(need_a) | set(need_b))
    for i, c in enumerate(chunks):
        w = wbuf[i % 2]
        P.dma("sp", w, io["ada_w"][l][:, c * 1024:(c + 1) * 1024].rearrange("(kc p) n -> p kc n", p=128))
        if c in need_a:
            for fc in range(8):
                for kc in range(8):
                    P.mm(psA[:, fc:fc + 1], w[:, kc, fc * 128:(fc + 1) * 128], cond[:, kc:kc + 1],
                         start=(kc == 0), stop=(kc == 7))
            P.tt("dve", outA[c], psA, adab_a[:, c * 8:(c + 1) * 8], ALU.add)
        if c in need_b:
            for n in range(2):
                for kc in range(8):
                    P.mm(psB[:, n * 512:(n + 1) * 512], crep[:, kc, :], w[:, kc, n * 512:(n + 1) * 512],
                         start=(kc == 0), stop=(kc == 7))
            bb = outB[c]
            P.dma("sp", bb, io["ada_b"][l:l + 1, c * 1024:(c + 1) * 1024].pbc())
            P.tt("dve", bb, psB, bb, ALU.add)
    P.barrier()
    M.off = keep
    return outA, outB


def emit_rstd(P, ss, out, n, tmp=None):
    P.act(out, ss, AF.Sqrt, scale=1.0 / n, bias=EPS)
    P.recip(out, out)


def emit_l0_mixer(P, M, io, x_in, x_out, ntile_all=NT, ntile_own=NTO, dbg=None):
    nc = P.nc
    M.reset()
    cst = emit_consts(P, M, io)
    idb = cst["idb"]
    modA, modB = emit_mod(P, M, io, 0, need_a=[0, 1], need_b=[2])
    shift_a, scale_a, gate_b = modA[0], modA[1], modB[2]
    nm_a = M.alloc([8], F32, "nm_a")
    P.dma("sp", nm_a, io["norm_mix_a"][0])
    gs_a = M.alloc([8], F32, "gs_a")
    P.stt("dve", gs_a, scale_a, 1.0, nm_a, ALU.add, ALU.mult)

    Wp = M.alloc([8, AB_IN], BF16, "Wp")
    bias_bc = M.alloc([AB_IN], F32, "bias_bc")
    Wqb = M.alloc([4, 768], BF16, "Wqb")
    Wkvb = M.alloc([2, 1024], BF16, "Wkvb")
    g_mq = M.alloc([96], F32, "g_mq")
    g_mk = M.alloc([96], F32, "g_mk")
    g_gq = M.alloc([64], F32, "g_gq")
    g_gk = M.alloc([64], F32, "g_gk")
    keep_off = M.off

    shrep = M.alloc([8, 128], BF16, "shift_rep")
    P.copy("dve", shrep, shift_a.unsq(2).bcast([128, 8, 128]))
    Wo = M.alloc([8, AB_IN], BF16, "Wo")
    stg = [M.alloc([AB_IN], F32, "wstg%d" % i) for i in range(2)]
    for kc in range(8):
        s = stg[kc % 2]
        P.dma("sp", s, io["ab_w_in"][kc * 128:(kc + 1) * 128, :])
        P.act(Wp[:, kc, :], s, AF.Copy, scale=gs_a[:, kc:kc + 1])
        P.copy("pool", Wo[:, kc, :], s)
    for n0 in range(0, AB_IN, 512):
        n1 = min(AB_IN, n0 + 512)
        ps = M.psum((n0 // 512) * 512, n1 - n0, "psbias%d" % n0)
        for kc in range(8):
            P.mm(ps, shrep[:, kc, :], Wo[:, kc, n0:n1], start=(kc == 0), stop=(kc == 7))
        P.copy("dve", bias_bc[:, n0:n1], ps)
    qan = M.alloc([4], F32, "qan")
    P.dma("sp", qan, io["ab_q_a_norm_a"])
    kvan = M.alloc([2], F32, "kvan")
    P.dma("sp", kvan, io["ab_kv_a_norm_a"])
    for kc in range(4):
        s = stg[kc % 2]
        P.dma("sp", s[:, 0:768], io["ab_w_qb"][kc * 128:(kc + 1) * 128, :])
        P.act(Wqb[:, kc, :], s[:, 0:768], AF.Copy, scale=qan[:, kc:kc + 1])
    for kc in range(2):
        s = stg[kc % 2]
        P.dma("sp", s[:, 0:1024], io["ab_w_kvb"][kc * 128:(kc + 1) * 128, :])
        P.act(Wkvb[:, kc, :], s[:, 0:1024], AF.Copy, scale=kvan[:, kc:kc + 1])
    P.dma("sp", g_mq, io["ab_mla_qn"].pbc())
    P.ts("dve", g_mq, g_mq, 1.0 / math.sqrt(96.0), None, ALU.mult)
    P.dma("sp", g_mk, io["ab_mla_kn"].pbc())
    P.dma("sp", g_gq, io["ab_gqa_qn"].pbc())
    P.ts("dve", g_gq, g_gq, 0.125, None, ALU.mult)
    P.dma("sp", g_gk, io["ab_gqa_kn"].pbc())
    P.barrier()
    M.off = keep_off

    QTm = dram(nc, P, "QTm", [8, 96, SH], BF16)
    QTg = dram(nc, P, "QTg", [8, 64, SH], BF16)
    KTm = dram(nc, P, "KTm", [8, 96, S], BF16)
    KTg = dram(nc, P, "KTg", [2, 64, S], BF16)
    Vm = dram(nc, P, "Vm", [8, 128, NT, 128], BF16)
    Vg = dram(nc, P, "Vg", [2, 128, NT, 128], BF16)
    AO = dram(nc, P, "AO", [1024, SH], BF16)

    off_proj = M.off
    xt = [M.alloc([1024], F32, "xt%d" % i) for i in range(2)]
    rt = [M.alloc([192], F32, "rt%d" % i) for i in range(2)]
    junk = M.alloc([1024], BF16, "junk")
    xb = M.alloc([1024], BF16, "xb")
    xT = M.alloc([1024], BF16, "xT")
    st = M.alloc([16], F32, "stats")
    cq = M.alloc([512], F32, "cq")
    ckvpe = M.alloc([288], F32, "ckvpe")
    qg = M.alloc([8, 64], F32, "qg")
    kvg = M.alloc([256], F32, "kvg")
    cqb = M.alloc([512], BF16, "cqb")
    cqT = M.alloc([512], BF16, "cqT")
    ckvb = M.alloc([256], BF16, "ckvb")
    ckvT = M.alloc([256], BF16, "ckvT")
    q = M.alloc([8, 96], F32, "q")
    sq = M.alloc([1024], F32, "sq")
    ssh = M.alloc([8], F32, "ssh")
    rsh = M.alloc([8], F32, "rsh")
    qpe = M.alloc([8, 32], F32, "qpe")
    t1 = M.alloc([8, 64], F32, "t1")
    t2 = M.alloc([8, 64], F32, "t2")
    qf = M.alloc([8, 96], BF16, "qf")
    qgf = M.alloc([8, 64], BF16, "qgf")
    kv = M.alloc([8, 128], F32, "kv")
    kf = M.alloc([8, 96], BF16, "kf")
    kpe = M.alloc([32], F32, "kpe")
    kgn = M.alloc([2, 64], F32, "kgn")
    kgf = M.alloc([2, 64], BF16, "kgf")
    QTs = [M.alloc([8, 512], BF16, "QTs%d" % i, parts=96) for i in range(2)]
    QTgs = [M.alloc([8, 512], BF16, "QTgs%d" % i, parts=64) for i in range(2)]
    KTs = [M.alloc([8, 512], BF16, "KTs%d" % i, parts=96) for i in range(2)]
    KTgs = [M.alloc([2, 512], BF16, "KTgs%d" % i, parts=64) for i in range(2)]
    Vs = [M.alloc([8, 8, 128], BF16, "Vs%d" % i) for i in range(2)]
    Vgs = [M.alloc([8, 2, 128], BF16, "Vgs%d" % i) for i in range(2)]
    for i in range(2):
        P.memset("pool", Vs[i], 1.0)
        P.memset("pool", Vgs[i], 1.0)

    psT = M.psum(0, 512, "psT", BF16)
    psA = M.psum(512, 512, "psA")
    psBk = M.psum(1024, 512, "psB")
    psC = M.psum(1536, 512, "psC")
    psD = M.psum(2048, 512, "psD")
    psQ = M.psum(2560, 1024, "psQ")
    psO = M.psum(3584, 512, "psO", BF16)

    def head_norm_rope(src3, nh, d, gain, rope_w, cosT, ssinT, dst_bf, name):
        P.tt("pool", sq[:, 0:nh * d].rearrange("p (h d) -> p h d", h=nh), src3, src3, ALU.mult)
        P.reduce("dve", ssh[:, 0:nh], sq[:, 0:nh * d].rearrange("p (h d) -> p h d", h=nh))
        emit_rstd(P, ssh[:, 0:nh], rsh[:, 0:nh], d)
        P.tt("dve", src3, src3, rsh[:, 0:nh].unsq(2).bcast([128, nh, d]), ALU.mult)
        return

    def ld_proj(t):
        if t < ntile_all:
            P.dma("sp", xt[t % 2], x_in[t * 128:(t + 1) * 128, :])
            P.dma("sp", rt[t % 2], io["rt"][t * 128:(t + 1) * 128, :])

    ld_proj(0)
    for t in range(ntile_all):
        own = t < ntile_own
        x_t = xt[t % 2]
        r_t = rt[t % 2]
        ld_proj(t + 1)
        P.act(junk, x_t, AF.Square, accum=st[:, 0:1])
        emit_rstd(P, st[:, 0:1], st[:, 1:2], 1024)
        rstd = st[:, 1:2]
        P.copy("pool", xb, x_t)
        for kc in range(8):
            P.tr(psT[:, kc * 128:(kc + 1) * 128], xb[:, kc * 128:(kc + 1) * 128], idb)
        P.copy("act", xT, psT)
        segs = []
        if own:
            segs += [(psA, 0, 512, cq), (psC, 800, 1312, qg.rearrange("p h d -> p (h d)"))]
        segs += [(psBk, 512, 800, ckvpe), (psD, 1312, 1568, kvg)]
        for ps, c0, c1, dst in segs:
            for kc in range(8):
                P.mm(ps[:, 0:c1 - c0], xT[:, kc * 128:(kc + 1) * 128], Wp[:, kc, c0:c1],
                     start=(kc == 0), stop=(kc == 7))
        for ps, c0, c1, dst in segs:
            P.stt("dve", dst, ps[:, 0:c1 - c0], rstd, bias_bc[:, c0:c1], ALU.mult, ALU.add)
        slot4 = (t // 4) % 2
        j4 = t % 4
        cosm, ssinm = r_t[:, 0:32], r_t[:, 32:64]
        cosa, ssina = r_t[:, 64:128], r_t[:, 128:192]
        if own:
            P.act(junk[:, 0:512], cq, AF.Square, accum=st[:, 2:3])
            emit_rstd(P, st[:, 2:3], st[:, 3:4], 512)
            P.copy("pool", cqb, cq)
            for kc in range(4):
                P.tr(psT[:, kc * 128:(kc + 1) * 128], cqb[:, kc * 128:(kc + 1) * 128], idb)
            P.copy("act", cqT, psT[:, 0:512])
            for n0, n1 in ((0, 512), (512, 768)):
                for kc in range(4):
                    P.mm(psQ[:, n0:n1], cqT[:, kc * 128:(kc + 1) * 128], Wqb[:, kc, n0:n1],
                         start=(kc == 0), stop=(kc == 3))
            qflat = q.rearrange("p h d -> p (h d)")
            P.ts("dve", qflat, psQ[:, 0:768], st[:, 3:4], None, ALU.mult)
            head_norm_rope(q, 8, 96, None, None, None, None, None, "q")
            P.tt("pool", qf[:, :, 0:64], q[:, :, 0:64], g_mq[:, 0:64].unsq(1).bcast([128, 8, 64]), ALU.mult)
            P.tt("dve", qpe, q[:, :, 64:96], g_mq[:, 64:96].unsq(1).bcast([128, 8, 32]), ALU.mult)
            P.tt("pool", t1[:, :, 0:32], qpe, cosm.unsq(1).bcast([128, 8, 32]), ALU.mult)
            P.tt("dve", t2[:, :, 0:16], qpe[:, :, 16:32], ssinm[:, 0:16].unsq(1).bcast([128, 8, 16]), ALU.mult)
            P.tt("dve", t2[:, :, 16:32], qpe[:, :, 0:16], ssinm[:, 16:32].unsq(1).bcast([128, 8, 16]), ALU.mult)
            P.tt("dve", qf[:, :, 64:96], t1[:, :, 0:32], t2[:, :, 0:32], ALU.add)
            for h in range(8):
                P.tr(psO[0:96, h * 128:(h + 1) * 128], qf[:, h, :], idb)
            P.copy("act", QTs[slot4][:, :, j4 * 128:(j4 + 1) * 128],
                   psO[0:96, :].rearrange("p (h t) -> p h t", h=8))
            head_norm_rope(qg, 8, 64, None, None, None, None, None, "qg")
            P.tt("pool", qg, qg, g_gq.unsq(1).bcast([128, 8, 64]), ALU.mult)
            P.tt("pool", t1, qg, cosa.unsq(1).bcast([128, 8, 64]), ALU.mult)
            qg5 = qg.rearrange("p h (b f e) -> p h b f e", b=2, f=2)
            t25 = t2.rearrange("p h (b f e) -> p h b f e", b=2, f=2)
            ss5 = ssina.rearrange("p (b f e) -> p b f e", b=2, f=2)
            for f in range(2):
                for bb in range(2):
                    P.tt("dve", t25[:, :, bb, f, :], qg5[:, :, bb, 1 - f, :],
                         ss5[:, bb, f, :].unsq(1).bcast([128, 8, 16]), ALU.mult)
            P.tt("dve", qgf, t1, t2, ALU.add)
            for h in range(8):
                P.tr(psO[0:64, h * 128:(h + 1) * 128], qgf[:, h, :], idb)
            P.copy("act", QTgs[slot4][:, :, j4 * 128:(j4 + 1) * 128],
                   psO[0:64, :].rearrange("p (h t) -> p h t", h=8))
        ckv = ckvpe[:, 0:256]
        P.act(junk[:, 0:256], ckv, AF.Square, accum=st[:, 4:5])
        emit_rstd(P, st[:, 4:5], st[:, 5:6], 256)
        P.copy("pool", ckvb, ckv)
        for kc in range(2):
            P.tr(psT[:, kc * 128:(kc + 1) * 128], ckvb[:, kc * 128:(kc + 1) * 128], idb)
        P.copy("act", ckvT, psT[:, 0:256])
        for n0, n1 in ((0, 512), (512, 1024)):
            for kc in range(2):
                P.mm(psQ[:, n0:n1], ckvT[:, kc * 128:(kc + 1) * 128], Wkvb[:, kc, n0:n1],
                     start=(kc == 0), stop=(kc == 1))
        kvflat = kv.rearrange("p h d -> p (h d)")
        P.ts("dve", kvflat, psQ, st[:, 5:6], None, ALU.mult)
        slot16 = (t // 8) % 2
        j16 = t % 8
        P.copy("pool", Vs[slot16][:, j16, :, 0:64], kv[:, :, 64:128])
        sq3 = sq[:, 0:512].rearrange("p (h d) -> p h d", h=8)
        P.tt("pool", sq3, kv[:, :, 0:64], kv[:, :, 0:64], ALU.mult)
        P.reduce("dve", ssh, sq3)
        P.act(junk[:, 0:32], ckvpe[:, 256:288], AF.Square, accum=st[:, 6:7])
        P.ts("dve", ssh, ssh, st[:, 6:7], None, ALU.add)
        emit_rstd(P, ssh, rsh, 96)
        P.tt("dve", kv[:, :, 0:64], kv[:, :, 0:64], rsh.unsq(2).bcast([128, 8, 64]), ALU.mult)
        P.tt("pool", kf[:, :, 0:64], kv[:, :, 0:64], g_mk[:, 0:64].unsq(1).bcast([128, 8, 64]), ALU.mult)
        P.tt("dve", kpe, ckvpe[:, 256:288], g_mk[:, 64:96], ALU.mult)
        tk1 = t1[:, 0, 0:32]
        tk2 = t2[:, 0, 0:32]
        P.tt("dve", tk1, kpe, cosm, ALU.mult)
        P.tt("dve", tk2[:, 0:16], kpe[:, 16:32], ssinm[:, 0:16], ALU.mult)
        P.tt("dve", tk2[:, 16:32], kpe[:, 0:16], ssinm[:, 16:32], ALU.mult)
        P.tt("dve", kpe, tk1, tk2, ALU.add)
        P.tt("dve", kf[:, :, 64:96], kpe.unsq(1).bcast([128, 8, 32]), rsh.unsq(2).bcast([128, 8, 32]), ALU.mult)
        for h in range(8):
            P.tr(psO[0:96, h * 128:(h + 1) * 128], kf[:, h, :], idb)
        P.copy("act", KTs[slot4][:, :, j4 * 128:(j4 + 1) * 128],
               psO[0:96, :].rearrange("p (h t) -> p h t", h=8))
        kg = kvg[:, 0:128].rearrange("p (h d) -> p h d", h=2)
        P.copy("pool", Vgs[slot16][:, j16, :, 0:64], kvg[:, 128:256].rearrange("p (h d) -> p h d", h=2))
        sqg = sq[:, 0:128].rearrange("p (h d) -> p h d", h=2)
        P.tt("pool", sqg, kg, kg, ALU.mult)
        P.reduce("dve", ssh[:, 0:2], sqg)
        emit_rstd(P, ssh[:, 0:2], rsh[:, 0:2], 64)
        P.tt("dve", kgn, kg, rsh[:, 0:2].unsq(2).bcast([128, 2, 64]), ALU.mult)
        P.tt("dve", kgn, kgn, g_gk.unsq(1).bcast([128, 2, 64]), ALU.mult)
        tg1 = t1[:, 0:2, :]
        tg2 = t2[:, 0:2, :]
        P.tt("pool", tg1, kgn, cosa.unsq(1).bcast([128, 2, 64]), ALU.mult)
        kg5 = kgn.rearrange("p h (b f e) -> p h b f e", b=2, f=2)
        tg25 = tg2.rearrange("p h (b f e) -> p h b f e", b=2, f=2)
        ss5 = ssina.rearrange("p (b f e) -> p b f e", b=2, f=2)
        for f in range(2):
            for bb in range(2):
                P.tt("dve", tg25[:, :, bb, f, :], kg5[:, :, bb, 1 - f, :],
                     ss5[:, bb, f, :].unsq(1).bcast([128, 2, 16]), ALU.mult)
        P.tt("dve", kgf, tg1, tg2, ALU.add)
        for h in range(2):
            P.tr(psO[0:64, h * 128:(h + 1) * 128], kgf[:, h, :], idb)
        P.copy("act", KTgs[slot4][:, :, j4 * 128:(j4 + 1) * 128],
               psO[0:64, 0:256].rearrange("p (h t) -> p h t", h=2))
        if j4 == 3:
            g4 = t // 4
            c0 = g4 * 512
            if own:
                P.dma("sp", QTm[:, :, c0:c0 + 512].rearrange("h p t -> p h t"), QTs[slot4], accum_w=True)
                P.dma("sp", QTg[:, :, c0:c0 + 512].rearrange("h p t -> p h t"), QTgs[slot4], accum_w=True)
            P.dma("sp", KTm[:, :, c0:c0 + 512].rearrange("h p t -> p h t"), KTs[slot4], accum_w=True)
            P.dma("sp", KTg[:, :, c0:c0 + 512].rearrange("h p t -> p h t"), KTgs[slot4], accum_w=True)
        if j16 == 7 or t == ntile_all - 1:
            g16 = t // 8
            nt16 = j16 + 1
            for hh in range(8):
                P.dma("sp", Vm[hh][:, g16 * 8:g16 * 8 + nt16, :], Vs[slot16][:, 0:nt16, hh, :], accum_w=True)
            for hh in range(2):
                P.dma("sp", Vg[hh][:, g16 * 8:g16 * 8 + nt16, :], Vgs[slot16][:, 0:nt16, hh, :], accum_w=True)
    if dbg is not None:
        dbg["QTm"], dbg["QTg"], dbg["KTm"], dbg["KTg"], dbg["Vm"], dbg["Vg"] = QTm, QTg, KTm, KTg, Vm, Vg
    P.barrier()
    M.off = off_proj

    nkt = ntile_all
    nqt = ntile_own // 4
    KT = [M.alloc([nkt * 128], BF16, "KT%d" % i) for i in range(2)]
    Vh = [M.alloc([nkt, 128], BF16, "Vh%d" % i) for i in range(2)]
    QT = [M.alloc([nqt * 512], BF16, "QT%d" % i) for i in range(2)]
    for i in range(2):
        P.memset("pool", KT[i][64:128, :], 0.0)
        P.memset("pool", QT[i][64:128, :], 0.0)
    OT = [M.alloc([nqt * 512], BF16, "OT%d" % i, parts=64) for i in range(2)]
    Pb = [M.alloc([1536], BF16, "Pb%d" % i) for i in range(2)]
    rl = M.alloc([512], F32, "rl", parts=64)
    Sg = [M.psum(0, 1536, "Sg0"), M.psum(1536, 1536, "Sg1")]
    Oa = [M.psum(3072, 512, "Oa0"), M.psum(3584, 512, "Oa1")]
    groups = []
    k0 = 0
    while k0 < nkt:
        n = 3 if (nkt - k0) not in (2, 4) else 2
        n = min(n, nkt - k0)
        groups.append(list(range(k0, k0 + n)))
        k0 += n

    def load_head(h):
        s = h % 2
        if h < 8:
            P.dma("sp", KT[s][0:96], KTm[h][:, 0:nkt * 128])
            P.dma("sp", Vh[s], Vm[h][:, 0:nkt])
            P.dma("sp", QT[s][0:96], QTm[h][:, 0:nqt * 512])
        else:
            if h < 10:
                P.memset("pool", QT[s][64:128, :], 0.0)
            P.dma("sp", KT[s][0:64], KTg[(h - 8) // 4][:, 0:nkt * 128])
            P.dma("sp", Vh[s], Vg[(h - 8) // 4][:, 0:nkt])
            P.dma("sp", QT[s][0:64], QTg[h - 8][:, 0:nqt * 512])

    tasks = []
    for h in range(16):
        for qt in range(nqt):
            for gi, g in enumerate(groups):
                tasks.append((h, qt, gi, g))

    def qk(i):
        h, qt, gi, g = tasks[i]
        s = h % 2
        d = 96 if h < 8 else 64
        for j, kt in enumerate(g):
            P.mm(Sg[i % 2][:, j * 512:(j + 1) * 512], KT[s][:, kt * 128:(kt + 1) * 128],
                 QT[s][:, qt * 512:(qt + 1) * 512])

    def ex_pv(i):
        h, qt, gi, g = tasks[i]
        s = h % 2
        n = len(g)
        P.act(Pb[i % 2][:, 0:n * 512], Sg[i % 2][:, 0:n * 512], AF.Exp)
        oa = Oa[(h * nqt + qt) % 2]
        for j, kt in enumerate(g):
            P.mm(oa, Vh[s][:, kt, :], Pb[i % 2][:, j * 512:(j + 1) * 512],
                 start=(kt == 0), stop=(kt == nkt - 1))
        if gi == len(groups) - 1:
            P.recip(rl, oa[64:128, :])
            P.tt("dve", OT[s][:, qt * 512:(qt + 1) * 512], oa[0:64, :], rl, ALU.mult)
            if qt == nqt - 1:
                P.dma("sp", AO[h * 64:(h + 1) * 64, 0:nqt * 512], OT[s])
                if h + 2 < 16:
                    load_head(h + 2)

    load_head(0)
    load_head(1)
    for i in range(len(tasks) + 1):
        if i < len(tasks):
            qk(i)
        if i >= 1:
            ex_pv(i - 1)
    P.barrier()
    M.off = off_proj

    Wout = M.alloc([8, 1024], BF16, "Wout")
    wst = [M.alloc([1024], F32, "wost%d" % i) for i in range(2)]
    for kc in range(8):
        P.dma("sp", wst[kc % 2], io["ab_w_out"][kc * 128:(kc + 1) * 128, :])
        P.tt("dve", Wout[:, kc, :], wst[kc % 2], gate_b, ALU.mult)
    AOs = [M.alloc([8, 512], BF16, "AOs%d" % i) for i in range(2)]
    xo = [M.alloc([1024], F32, "xo%d" % i) for i in range(4)]
    ys = [M.psum(0, 1024, "y0"), M.psum(1024, 1024, "y1")]

    def ld_ao(g):
        if g * 4 < ntile_own:
            P.dma("sp", AOs[g % 2], AO[:, g * 512:(g + 1) * 512].rearrange("(fc p) t -> p fc t", p=128))

    def ld_x(t):
        if t < ntile_own:
            P.dma("sp", xo[t % 4], x_in[t * 128:(t + 1) * 128, :])

    ld_ao(0)
    ld_x(0)
    ld_x(1)
    for t in range(ntile_own):
        g4 = t // 4
        j4 = t % 4
        a = AOs[g4 % 2]
        if j4 == 0:
            ld_ao(g4 + 1)
        ld_x(t + 2)
        x_t = xo[t % 4]
        y = ys[t % 2]
        for n in range(2):
            for fc in range(8):
                P.mm(y[:, n * 512:(n + 1) * 512], a[:, fc, j4 * 128:(j4 + 1) * 128],
                     Wout[:, fc, n * 512:(n + 1) * 512], start=(fc == 0), stop=(fc == 7))
        P.tt("dve", x_t, x_t, y, ALU.add)
        P.dma("sp", x_out[t * 128:(t + 1) * 128, :], x_t, accum_w=True)
    P.barrier()


def rope_tables_np(pos, dim):
    inv = (np.float32(10000.0) ** (-np.arange(0, dim, 2, dtype=np.float32) / np.float32(dim))).astype(np.float32)
    ang = pos.astype(np.float32)[:, None] * inv[None, :]
    ang = np.concatenate([ang, ang], axis=-1)
    return np.cos(ang).astype(np.float32), np.sin(ang).astype(np.float32)


def signed_sin(sin):
    h = sin.shape[-1] // 2
    return np.concatenate([-sin[..., :h], sin[..., h:]], axis=-1)


def make_rt(half):
    pos = (np.arange(S) + half * SH) % S
    cm, sm = rope_tables_np(pos, 32)
    row, col = pos // 64, pos % 64
    rc, rs = rope_tables_np(row, 32)
    cc, cs = rope_tables_np(col, 32)
    return np.concatenate([cm, signed_sin(sm), rc, cc, signed_sin(rs), signed_sin(cs)], axis=-1).astype(np.float32)


def a_layout(v):
    return np.ascontiguousarray(np.asarray(v, np.float32).reshape(-1, 128).T)


def declare_inputs(nc, P, specs):
    io = {}
    for name, (shape, dt) in specs.items():
        io[name] = dram(nc, P, name, shape, dt, kind="ExternalInput")
    return io


SBUF_WORDS = 48 * 1024


def build_l0_mixer(ntile_all=NT, ntile_own=NTO, debug=False):
    nc = bass.Bass("TRN2", target_bir_lowering=False)
    es = ExitStack()
    with es:
        P = Prog(nc, es)
        M = Mem(nc, es, P, SBUF_WORDS)
        specs = {
            "x": ([S, D], F32), "ident": ([128, 128], F32), "c_a": ([128, 8], F32),
            "ada_w": ([2, D, 6 * D], F32), "ada_b": ([2, 6 * D], F32), "ada_b_a": ([2, 128, 48], F32),
            "norm_mix_a": ([2, 128, 8], F32), "rt": ([S, 192], F32),
            "ab_w_in": ([D, AB_IN], F32), "ab_q_a_norm_a": ([128, 4], F32), "ab_w_qb": ([512, 768], F32),
            "ab_kv_a_norm_a": ([128, 2], F32), "ab_w_kvb": ([256, 1024], F32),
            "ab_mla_qn": ([1, 96], F32), "ab_mla_kn": ([1, 96], F32), "ab_gqa_qn": ([1, 64], F32),
            "ab_gqa_kn": ([1, 64], F32), "ab_w_out": ([D, D], F32),
        }
        io = declare_inputs(nc, P, specs)
        x_out = dram(nc, P, "x1", [SH, D], F32, kind="ExternalOutput")
        dbg = {} if debug else None
        emit_l0_mixer(P, M, io, io["x"], x_out, ntile_all, ntile_own, dbg)
        if debug:
            for k in ("QTm", "KTm", "Vm", "QTg", "KTg", "Vg"):
                src = dbg[k]
                o = dram(nc, P, "dbg_" + k, list(src.shape), BF16, kind="ExternalOutput")
                P.dma("sp", o, src, sem="dbgout")
        P.barrier()
        P.emit()
    return nc


def host_inputs_l0(inputs, core):
    b, half = core // 2, core % 2
    x = np.asarray(inputs["x"][b], np.float32)
    xr = np.concatenate([x[half * SH:(half + 1) * SH], x[(1 - half) * SH:(2 - half) * SH]], axis=0)
    return {
        "x": np.ascontiguousarray(xr),
        "ident": np.eye(128, dtype=np.float32),
        "c_a": a_layout(inputs["c"][b]),
        "ada_w": np.asarray(inputs["ada_w"], np.float32),
        "ada_b": np.asarray(inputs["ada_b"], np.float32),
        "ada_b_a": np.stack([a_layout(inputs["ada_b"][l]) for l in range(2)]),
        "norm_mix_a": np.stack([a_layout(inputs["norm_mix"][l]) for l in range(2)]),
        "rt": make_rt(half),
        "ab_w_in": np.asarray(inputs["ab_w_in"][0], np.float32),
        "ab_q_a_norm_a": a_layout(inputs["ab_q_a_norm"][0]),
        "ab_w_qb": np.asarray(inputs["ab_w_qb"][0], np.float32),
        "ab_kv_a_norm_a": a_layout(inputs["ab_kv_a_norm"][0]),
        "ab_w_kvb": np.asarray(inputs["ab_w_kvb"][0], np.float32),
        "ab_mla_qn": np.asarray(inputs["ab_mla_qn"], np.float32).reshape(1, 96),
        "ab_mla_kn": np.asarray(inputs["ab_mla_kn"], np.float32).reshape(1, 96),
        "ab_gqa_qn": np.asarray(inputs["ab_gqa_qn"], np.float32).reshape(1, 64),
        "ab_gqa_kn": np.asarray(inputs["ab_gqa_kn"], np.float32).reshape(1, 64),
        "ab_w_out": np.asarray(inputs["ab_w_out"][0], np.float32),
    }


CAPB = 8
CAP = CAPB * 128
NSLOT = 32 * CAP
TRASH = NSLOT


def _idma(P, out, in_, out_off=None, in_off=None, sem=None, accum_w=False, bounds=None):
    o, a = _ap(out), _ap(in_)
    oo = bass.IndirectOffsetOnAxis(ap=_ap(out_off), axis=0) if out_off is not None else None
    io_ = bass.IndirectOffsetOnAxis(ap=_ap(in_off), axis=0) if in_off is not None else None
    if sem is None:
        sb = out if (isinstance(out, T) and out.buf.sb) else in_
        sem = sb.buf.name
    P.op("pool", lambda e: e.indirect_dma_start(out=o, out_offset=oo, in_=a, in_offset=io_,
                                                bounds_check=bounds, oob_is_err=False),
         reads=_bufs(in_, out_off, in_off), writes=_bufs(out), dma=sem, accum_w=accum_w)


def emit_moe(P, M, io, l, x_in, x_out, ntile=NTO, dbg=None, wl=None):
    nc = P.nc
    wl = l if wl is None else wl
    M.reset()
    cst = emit_consts(P, M, io)
    idb = cst["idb"]
    modA, modB = emit_mod(P, M, io, l, need_a=[], need_b=[3, 4, 5])
    shift_b, scale_b, gate_b = modB[3], modB[4], modB[5]
    gs_b = scale_b
    nf = M.alloc([1024], F32, "nf_b")
    P.dma("sp", nf, io["norm_ffn"][l:l + 1, :].pbc())
    P.stt("dve", gs_b, scale_b, 1.0, nf, ALU.add, ALU.mult)
    wr = M.alloc([8, 36], F32, "wr")
    P.dma("sp", wr, io["moe_wr"][wl].rearrange("(kc p) n -> p kc n", p=128))
    whi = M.alloc([8, 36], BF16, "whi")
    wlo = M.alloc([8, 36], BF16, "wlo")
    P.copy("dve", whi, wr)
    P.tt("dve", wlo, wr, whi, ALU.subtract)
    rbias = M.alloc([36], F32, "rbias")
    P.dma("sp", rbias, io["moe_rb"][wl:wl + 1, :].pbc())
    trif = M.alloc([128], F32, "trif")
    P.dma("sp", trif, io["tri"])
    tri = M.alloc([128], BF16, "tri")
    P.copy("dve", tri, trif)
    ones = M.alloc([128], BF16, "ones")
    P.memset("dve", ones, 1.0)
    ebase = M.alloc([32], F32, "ebase")
    P.dma("sp", ebase, io["ebase"].pbc())
    tokid = M.alloc([ntile + 8], I32, "tokid")
    P.dma("sp", tokid[:, 0:ntile], io["tokid"][:, 0:ntile])
    LG = M.alloc([ntile, 36], F32, "LG")
    gates = M.alloc([ntile, 2], F32, "gates")
    slots_i = M.alloc([ntile + 4, 2], I32, "slots_i")
    keep_off = M.off

    H2 = dram(nc, P, "H2_%d" % l, [SH, D], BF16)
    SLOT_T = dram(nc, P, "SLOT_T%d" % l, [NSLOT + 128, 1], I32)
    Y = dram(nc, P, "Y_%d" % l, [NSLOT + 128, D], F32)

    zt = M.alloc([1024], F32, "zt")
    P.memset("dve", zt, 0.0)
    P.dma("sp", SLOT_T.rearrange("(p j) o -> p (j o)", p=128), zt[:, 0:(NSLOT + 128) // 128].bitcast(I32))
    P.dma("sp", Y[NSLOT:NSLOT + 128, :], zt)

    xt = [M.alloc([1024], F32, "mxt%d" % i) for i in range(4)]
    junk2 = [M.alloc([1024], BF16, "mjunk%d" % i) for i in range(2)]
    st2 = [M.alloc([8], F32, "mst%d" % i) for i in range(2)]
    h22 = [M.alloc([1024], F32, "h2_%d" % i) for i in range(2)]
    hi = [M.alloc([1024], BF16, "hi%d" % i) for i in range(2)]
    lo2 = [M.alloc([1024], BF16, "lo%d" % i) for i in range(2)]
    hT2 = [M.alloc([1024], BF16, "hT%d" % i) for i in range(2)]
    lT2 = [M.alloc([1024], BF16, "lT%d" % i) for i in range(2)]
    psT = [M.psum(0, 512, "mpsT0", BF16), M.psum(512, 512, "mpsT1", BF16)]
    psL2 = [M.psum(1024, 36, "psL0"), M.psum(1536, 36, "psL1")]

    def ld_mx(t):
        if t < ntile:
            P.dma("sp", xt[t % 4], x_in[t * 128:(t + 1) * 128, :])

    ld_mx(0)
    ld_mx(1)
    for t in range(ntile):
        ld_mx(t + 2)
        x_t = xt[t % 4]
        junk, st, h2, lo, hT, lT, psL = junk2[t % 2], st2[t % 2], h22[t % 2], lo2[t % 2], hT2[t % 2], lT2[t % 2], psL2[t % 2]
        P.act(junk, x_t, AF.Square, accum=st[:, 0:1])
        emit_rstd(P, st[:, 0:1], st[:, 1:2], 1024)
        P.stt("dve", h2, x_t, st[:, 1:2], gs_b, ALU.mult, ALU.mult)
        P.tt("pool", h2, h2, shift_b, ALU.add)
        hi_t = hi[t % 2]
        P.copy("act", hi_t, h2)
        P.tt("dve", lo, h2, hi_t, ALU.subtract)
        P.dma("sp", H2[t * 128:(t + 1) * 128, :], hi_t, accum_w=True)
        for kc in range(8):
            P.tr(psT[0][:, kc * 128:(kc + 1) * 128], hi_t[:, kc * 128:(kc + 1) * 128], idb)
        P.copy("act", hT, psT[0])
        for kc in range(8):
            P.tr(psT[1][:, kc * 128:(kc + 1) * 128], lo[:, kc * 128:(kc + 1) * 128], idb)
        P.copy("act", lT, psT[1])
        n = 0
        for a, w in ((hT, whi), (hT, wlo), (lT, whi)):
            for kc in range(8):
                P.mm(psL, a[:, kc * 128:(kc + 1) * 128], w[:, kc, :], start=(n == 0), stop=(n == 23))
                n += 1
        P.tt("dve", LG[:, t, :], psL, rbias, ALU.add)
    nt = ntile
    w4 = M.alloc([nt, 4], F32, "w4")
    ohg = M.alloc([nt, 4], F32, "ohg")
    c1 = M.alloc([nt], F32, "c1")
    c2 = M.alloc([nt], F32, "c2")
    c3 = M.alloc([nt], F32, "c3")
    pgt = M.alloc([nt], F32, "pgt")
    el = M.alloc([nt, 8], F32, "el")
    el2 = M.alloc([nt, 8], F32, "el2")
    w8 = M.alloc([nt, 8], F32, "w8")
    oh1 = M.alloc([nt, 8], F32, "oh1")
    oh2 = M.alloc([nt, 8], F32, "oh2")
    A1 = M.alloc([nt, 4, 8], F32, "A1")
    A2 = M.alloc([nt, 4, 8], F32, "A2")
    Ab = M.alloc([nt, 32], BF16, "Ab")
    Acum = M.alloc([nt + 1, 32], BF16, "Acum")
    pos = M.alloc([nt, 32], F32, "pos")
    w32 = M.alloc([nt, 32], F32, "w32")
    lg4 = LG[:, :, 0:4]
    P.reduce("dve", c1, lg4, op=ALU.max)
    P.tt("dve", ohg, lg4, c1.unsq(2).bcast([128, nt, 4]), ALU.is_equal)
    P.tt("dve", w4, lg4, c1.unsq(2).bcast([128, nt, 4]), ALU.subtract)
    P.act(w4, w4, AF.Exp)
    P.reduce("dve", c2, w4)
    P.recip(pgt, c2)
    for g in range(4):
        src = LG[:, :, 4 + 8 * g:12 + 8 * g]
        og = ohg[:, :, g:g + 1].bcast([128, nt, 8])
        if g == 0:
            P.tt("dve", el, src, og, ALU.mult)
        else:
            P.tt("dve", w8, src, og, ALU.mult)
            P.tt("dve", el, el, w8, ALU.add)
    P.reduce("dve", c1, el, op=ALU.max)
    P.tt("dve", oh1, el, c1.unsq(2).bcast([128, nt, 8]), ALU.is_equal)
    P.stt("dve", el2, oh1, -1e30, el, ALU.mult, ALU.add)
    P.reduce("dve", c2, el2, op=ALU.max)
    P.tt("dve", oh2, el2, c2.unsq(2).bcast([128, nt, 8]), ALU.is_equal)
    P.tt("dve", c3, c2, c1, ALU.subtract)
    P.act(c3, c3, AF.Exp)
    P.ts("dve", c3, c3, 1.0, None, ALU.add)
    P.recip(c3, c3)
    P.tt("dve", gates[:, :, 0], pgt, c3, ALU.mult)
    P.tt("dve", gates[:, :, 1], pgt, gates[:, :, 0], ALU.subtract)
    P.tt("dve", A1, ohg.unsq(3).bcast([128, nt, 4, 8]), oh1.unsq(2).bcast([128, nt, 4, 8]), ALU.mult)
    P.tt("dve", A2, ohg.unsq(3).bcast([128, nt, 4, 8]), oh2.unsq(2).bcast([128, nt, 4, 8]), ALU.mult)
    A1f = A1.rearrange("p t g e -> p t (g e)")
    A2f = A2.rearrange("p t g e -> p t (g e)")
    P.tt("dve", Ab, A1f, A2f, ALU.add)
    P.memset("dve", Acum[:, 0, :], 0.0)
    for t in range(nt):
        P.tt("dve", Acum[:, t + 1, :], Acum[:, t, :], Ab[:, t, :], ALU.add)
    psP = M.psum(2048, nt * 32, "psP")
    for t in range(nt):
        P.mm(psP[:, t * 32:(t + 1) * 32], tri, Ab[:, t, :], start=True, stop=False)
        P.mm(psP[:, t * 32:(t + 1) * 32], ones, Acum[:, t, :], start=False, stop=True)
    P.copy("dve", pos.rearrange("p t e -> p (t e)"), psP)
    for k, Ak in enumerate((A1f, A2f)):
        P.tt("dve", w32, Ak, pos, ALU.mult)
        P.reduce("dve", c1, w32)
        P.tt("dve", w32, Ak, ebase.unsq(1).bcast([128, nt, 32]), ALU.mult)
        P.reduce("dve", c2, w32)
        P.ts("dve", c3, c1, CAP - 0.5, None, ALU.is_lt)
        P.tt("dve", c2, c2, c1, ALU.add)
        P.ts("dve", c2, c2, float(TRASH), None, ALU.subtract)
        P.tt("dve", c2, c2, c3, ALU.mult)
        P.ts("dve", c2, c2, float(TRASH), None, ALU.add)
        P.tt("dve", gates[:, :, k], gates[:, :, k], c3, ALU.mult)
        P.copy("dve", slots_i[:, 0:nt, k], c2)
    istg = [M.alloc([8], I32, "istg%d" % i) for i in range(4)]
    for t in range(nt):
        for k in range(2):
            ist = istg[(2 * t + k) % 4]
            P.copy("pool", ist[:, 0:1], slots_i[:, t, k:k + 1])
            _idma(P, SLOT_T, tokid[:, t:t + 1], out_off=ist[:, 0:1], sem=ist.buf.name, accum_w=True)
    if dbg is not None:
        dbg["LG"], dbg["gates"], dbg["slots_i"], dbg["pos"] = LG, gates, slots_i[:, 0:nt, :], pos
    P.barrier()
    M.off = keep_off

    NH = CAP // 512
    Wup = [M.alloc([8, 1024], BF16, "Wup%d" % i) for i in range(2)]
    Wdn = [M.alloc([4, 1024], BF16, "Wdn%d" % i) for i in range(2)]
    idx = [M.alloc([CAPB], I32, "idx%d" % i) for i in range(2)]
    Xg = [M.alloc([CAPB, 1024], BF16, "Xg%d" % i) for i in range(2)]
    XT = M.alloc([8, CAP], BF16, "XT")
    actT = M.alloc([4, CAP], BF16, "actT")
    sg = [M.alloc([512], F32, "sg%d" % i) for i in range(2)]
    Ysb = [M.alloc([1024], F32, "Ysb%d" % i) for i in range(2)]
    psXh = [M.psum(0, 256, "psXa", BF16), M.psum(3584, 256, "psXb", BF16)]
    psU = [(M.psum(512, 512, "psG0"), M.psum(1024, 512, "psU0")),
           (M.psum(1536, 512, "psG1"), M.psum(2048, 512, "psU1"))]
    psYh = [M.psum(2560, 512, "psY0"), M.psum(3072, 512, "psY1")]
    Yv = Y[0:NSLOT, :].rearrange("(e p j) d -> e p j d", e=32, p=128)
    SLv = SLOT_T[0:NSLOT, :].rearrange("(e p j) o -> e p (j o)", e=32, p=128)

    def load_expert(e):
        s = e % 2
        P.dma("pool", Wup[s], io["moe_w_up"][wl, e].rearrange("(kc p) n -> p kc n", p=128))
        P.dma("pool", Wdn[s], io["moe_w_down"][wl, e].rearrange("(kc p) n -> p kc n", p=128))
        P.dma("sp", idx[s], SLv[e])
        for j in range(CAPB):
            _idma(P, Xg[s][:, j, :], H2, in_off=idx[s][:, j:j + 1], sem="Xg%d" % s, accum_w=True)

    load_expert(0)
    load_expert(1)
    cnt = 0
    for e in range(32):
        s = e % 2
        for j in range(CAPB):
            for hx in range(2):
                for k4 in range(4):
                    kc = hx * 4 + k4
                    P.tr(psXh[hx][:, k4 * 128:(k4 + 1) * 128], Xg[s][:, j, kc * 128:(kc + 1) * 128], idb)
                P.copy("act" if hx == 0 else "dve", XT[:, hx * 4:(hx + 1) * 4, j * 128:(j + 1) * 128],
                       psXh[hx].rearrange("p (k t) -> p k t", k=4))
        for hf in range(NH):
            cs = slice(hf * 512, (hf + 1) * 512)
            for hc in range(4):
                pg_, pu_ = psU[cnt % 2]
                for kc in range(8):
                    P.mm(pg_, Wup[s][:, kc, hc * 128:(hc + 1) * 128], XT[:, kc, cs], start=(kc == 0), stop=(kc == 7))
                for kc in range(8):
                    P.mm(pu_, Wup[s][:, kc, 512 + hc * 128:512 + (hc + 1) * 128], XT[:, kc, cs],
                         start=(kc == 0), stop=(kc == 7))
                P.act(sg[cnt % 2], pg_, AF.Silu)
                P.tt("dve", actT[:, hc, cs], sg[cnt % 2], pu_, ALU.mult)
                cnt += 1
        for j in range(CAPB):
            ysb = Ysb[j % 2]
            for n in range(2):
                for hc in range(4):
                    P.mm(psYh[n], actT[:, hc, j * 128:(j + 1) * 128],
                         Wdn[s][:, hc, n * 512:(n + 1) * 512], start=(hc == 0), stop=(hc == 3))
                P.copy("act" if n == 0 else "dve", ysb[:, n * 512:(n + 1) * 512], psYh[n])
            P.dma("sp", Yv[e][:, j, :], ysb, accum_w=True)
        if e + 2 < 32:
            load_expert(e + 2)
    P.barrier()
    M.off = keep_off

    xo = [M.alloc([1024], F32, "cxo%d" % i) for i in range(3)]
    y1 = [M.alloc([1024], F32, "cy1%d" % i) for i in range(3)]
    y2 = [M.alloc([1024], F32, "cy2%d" % i) for i in range(3)]
    cidx = [M.alloc([8], I32, "cidx%d" % i) for i in range(3)]

    def ld_c(t):
        if t < ntile:
            s_ = t % 3
            P.dma("sp", xo[s_], x_in[t * 128:(t + 1) * 128, :])
            P.copy("pool", cidx[s_][:, 0:1], slots_i[:, t, 0:1])
            P.copy("pool", cidx[s_][:, 1:2], slots_i[:, t, 1:2])
            _idma(P, y1[s_], Y, in_off=cidx[s_][:, 0:1])
            _idma(P, y2[s_], Y, in_off=cidx[s_][:, 1:2])

    ld_c(0)
    ld_c(1)
    for t in range(ntile):
        s = t % 3
        P.ts("dve", y1[s], y1[s], gates[:, t, 0:1], None, ALU.mult)
        P.stt("dve", y1[s], y2[s], gates[:, t, 1:2], y1[s], ALU.mult, ALU.add)
        P.tt("dve", y1[s], y1[s], gate_b, ALU.mult)
        P.tt("dve", xo[s], xo[s], y1[s], ALU.add)
        P.dma("sp", x_out[t * 128:(t + 1) * 128, :], xo[s], accum_w=True)
        ld_c(t + 2)
    P.barrier()


MOE_SPECS = {
    "x": ([SH, D], F32), "ident": ([128, 128], F32), "c_a": ([128, 8], F32),
    "ada_w": ([2, D, 6 * D], F32), "ada_b": ([2, 6 * D], F32), "ada_b_a": ([2, 128, 48], F32),
    "norm_ffn": ([2, D], F32), "moe_wr": ([1, D, 36], F32), "moe_rb": ([1, 36], F32),
    "tri": ([128, 128], F32), "ebase": ([1, 32], F32), "tokid": ([128, NTO], I32),
    "moe_w_up": ([1, 32, D, D], F32), "moe_w_down": ([1, 32, 512, D], F32),
}


def build_moe(l, ntile=NTO, debug=False):
    nc = bass.Bass("TRN2", target_bir_lowering=False)
    es = ExitStack()
    with es:
        P = Prog(nc, es)
        M = Mem(nc, es, P, SBUF_WORDS)
        io = declare_inputs(nc, P, MOE_SPECS)
        x_out = dram(nc, P, "x2", [SH, D], F32, kind="ExternalOutput")
        dbg = {} if debug else None
        emit_moe(P, M, io, l, io["x"], x_out, ntile, dbg, wl=0)
        if debug:
            for k, dt in (("LG", F32), ("gates", F32), ("slots_i", I32), ("pos", F32)):
                src = dbg[k]
                shp = list(src.shape)
                o = dram(nc, P, "dbg_" + k, shp, dt, kind="ExternalOutput")
                P.dma("sp", o, src, sem="dbgout")
        P.barrier()
        P.emit()
    return nc


def host_inputs_moe(inputs, core, x_own, l):
    b = core // 2
    wr = np.concatenate([inputs["moe_w_group"][l],
                         np.transpose(inputs["moe_w_expert"][l], (1, 0, 2)).reshape(D, 32)], axis=1)[None].astype(np.float32)
    rb = np.concatenate([inputs["moe_b_group"][l], inputs["moe_b_expert"][l].reshape(32)])[None].astype(np.float32)
    return {
        "x": np.ascontiguousarray(x_own, dtype=np.float32),
        "ident": np.eye(128, dtype=np.float32),
        "c_a": a_layout(inputs["c"][b]),
        "ada_w": np.asarray(inputs["ada_w"], np.float32),
        "ada_b": np.asarray(inputs["ada_b"], np.float32),
        "ada_b_a": np.stack([a_layout(inputs["ada_b"][k]) for k in range(2)]),
        "norm_ffn": np.asarray(inputs["norm_ffn"], np.float32),
        "moe_wr": wr, "moe_rb": rb,
        "tri": np.triu(np.ones((128, 128), np.float32), 1),
        "ebase": (np.arange(32, dtype=np.float32) * CAP).reshape(1, 32),
        "tokid": (np.arange(NTO)[None, :] * 128 + np.arange(128)[:, None]).astype(np.int32),
        "moe_w_up": np.asarray(inputs["moe_w_up"][l:l + 1], np.float32),
        "moe_w_down": np.asarray(inputs["moe_w_down"][l:l + 1], np.float32),
    }


C_IN = 3072
LAMBDA_INIT1 = 0.8 - 0.6 * math.exp(-0.3 * 1)
GW = 1280
HKW = 1152


def t5_bucket_np(rel):
    rel = np.asarray(rel, np.int64)
    nb = 16
    max_exact = 8
    ret = np.where(rel > 0, nb, 0)
    n = np.abs(rel)
    nf = np.maximum(n, 1).astype(np.float32)
    large = max_exact + (np.log(nf / np.float32(max_exact)) / np.float32(math.log(128 / max_exact))
                         * np.float32(nb - max_exact)).astype(np.int32)
    large = np.minimum(large, nb - 1)
    return ret + np.where(n < max_exact, n, large)


def make_bias_onehots(half):
    m = np.arange(GW)
    r_own = 639 - m
    d32 = 512 - half * 8192
    d63 = 8064 - half * 8192
    mm_ = np.arange(640)
    r32 = 127 - mm_ + d32
    r63 = 127 - mm_ + d63
    rel = np.concatenate([r_own, r32, r63])
    bk = t5_bucket_np(rel)
    oh = (bk[None, :] == np.arange(32)[:, None]).astype(np.float32)
    sel = np.zeros((32, 3), np.float32)
    sel[15, 0] = 1.0
    sel[31, 1] = 1.0
    sel[31 if half == 0 else 15, 2] = 1.0
    return oh, sel


def emit_l1_mixer(P, M, io, x_in, x_out, ntile_all=NT, ntile_own=NTO, dbg=None, tile_hook=None):
    nc = P.nc
    xin = x_in if callable(x_in) else (lambda t: x_in[t * 128:(t + 1) * 128, :])
    M.reset()
    cst = emit_consts(P, M, io)
    idb = cst["idb"]
    modA, modB = emit_mod(P, M, io, 1, need_a=[0, 1], need_b=[2])
    shift_a, scale_a, gate_b = modA[0], modA[1], modB[2]
    nm_a = M.alloc([8], F32, "nm_a")
    P.dma("sp", nm_a, io["norm_mix_a"][1])
    gs_a = M.alloc([8], F32, "gs_a")
    P.stt("dve", gs_a, scale_a, 1.0, nm_a, ALU.add, ALU.mult)
    g_q = M.alloc([2, 64], F32, "g_q1")
    g_k = M.alloc([2, 64], F32, "g_k1")
    neglam = M.alloc([1], F32, "neglam")
    sublnc = M.alloc([1], F32, "sublnc")
    bcol = M.alloc([3, 8], F32, "bcol")
    Hk = M.alloc([8, HKW], BF16, "Hk")
    Hx = [M.alloc([8, 512], BF16, "Hx%d" % i) for i in range(2)]
    Jb = M.alloc([128], BF16, "Jb")
    ones = M.alloc([128], BF16, "ones1")
    keep_attn = M.off
    Wp = M.alloc([8, C_IN], BF16, "Wp1")
    bias_bc = M.alloc([C_IN], F32, "bias_bc1")
    keep_off = M.off
    P.memset("dve", ones, 1.0)

    jf = M.alloc([128], F32, "jf")
    P.dma("sp", jf, io["jmat"])
    P.copy("dve", Jb, jf)
    P.dma("sp", g_q.rearrange("p c d -> p (c d)"), io["c_qn"].pbc())
    P.ts("dve", g_q, g_q, 0.125, None, ALU.mult)
    P.dma("sp", g_k.rearrange("p c d -> p (c d)"), io["c_kn"].pbc())
    lamv = M.alloc([4, 64], F32, "lamv")
    P.dma("sp", lamv.rearrange("p a d -> p (a d)"), io["c_lam"].pbc())
    lw = M.alloc([2, 64], F32, "lw")
    P.tt("dve", lw[:, 0, :], lamv[:, 0, :], lamv[:, 1, :], ALU.mult)
    P.tt("dve", lw[:, 1, :], lamv[:, 2, :], lamv[:, 3, :], ALU.mult)
    ls = M.alloc([2], F32, "ls")
    P.reduce("dve", ls, lw)
    P.act(ls, ls, AF.Exp)
    P.tt("dve", neglam, ls[:, 1:2], ls[:, 0:1], ALU.subtract)
    P.ts("dve", neglam, neglam, -LAMBDA_INIT1, None, ALU.add)
    P.dma("sp", sublnc, io["c_subln_a"])
    P.ts("dve", sublnc, sublnc, 1.0 - LAMBDA_INIT1, None, ALU.mult)
    rb = M.alloc([8], F32, "rb", parts=32)
    P.dma("sp", rb, io["rel_bias"])
    oh = M.alloc([GW + 1280], F32, "oh", parts=32)
    P.dma("sp", oh, io["bias_oh"])
    sel = M.alloc([3], F32, "sel", parts=32)
    P.dma("sp", sel, io["bias_sel"])
    selrep = M.alloc([3, 128], F32, "selrep", parts=32)
    P.copy("dve", selrep, sel.unsq(2).bcast([32, 3, 128]))
    psc = M.psum(0, 24, "psc")
    for i in range(3):
        P.mm(psc[:, i * 8:(i + 1) * 8], selrep[:, i, :], rb)
    P.copy("dve", bcol.rearrange("p a h -> p (a h)"), psc)
    GT = GW + 1280
    Gd = dram(nc, P, "Gd", [8, GT], F32)
    gsb = M.alloc([GT], F32, "gsb", parts=8)
    for c0 in range(0, GT, 512):
        c1 = min(GT, c0 + 512)
        psg = M.psum(512 + (c0 // 512) * 512, c1 - c0, "psg%d" % c0)
        P.mm(psg[0:8, :], rb, oh[:, c0:c1])
        P.copy("dve", gsb[:, c0:c1], psg[0:8, :])
    P.dma("sp", Gd, gsb)
    hkf = M.alloc([HKW], F32, "hkf")
    for h in range(8):
        src = T(bass.AP(Gd.ap.tensor, h * GT, [[1, 128], [1, HKW]]), Gd.buf)
        P.dma("sp", hkf, src)
        P.copy("dve", Hk[:, h, :], hkf)
        for xi in range(2):
            src = T(bass.AP(Gd.ap.tensor, h * GT + GW + xi * 640, [[1, 128], [1, 512]]), Gd.buf)
            P.dma("sp", hkf[:, 0:512], src)
            P.copy("dve", Hx[xi][:, h, :], hkf[:, 0:512])
    P.barrier()
    M.off = keep_off
    shrep = M.alloc([8, 128], BF16, "shift_rep1")
    P.copy("dve", shrep, shift_a.unsq(2).bcast([128, 8, 128]))
    Wo = M.alloc([8, C_IN], BF16, "Wo1")
    stg = [M.alloc([C_IN], F32, "w1stg%d" % i) for i in range(2)]
    for kc in range(8):
        s = stg[kc % 2]
        P.dma("sp", s, io["c_w_in"][kc * 128:(kc + 1) * 128, :])
        P.act(Wp[:, kc, :], s, AF.Copy, scale=gs_a[:, kc:kc + 1])
        P.copy("pool", Wo[:, kc, :], s)
    for n0 in range(0, C_IN, 512):
        ps = M.psum((n0 // 512) * 512, 512, "ps1bias%d" % n0)
        for kc in range(8):
            P.mm(ps, shrep[:, kc, :], Wo[:, kc, n0:n0 + 512], start=(kc == 0), stop=(kc == 7))
        P.copy("dve", bias_bc[:, n0:n0 + 512], ps)
    P.barrier()
    M.off = keep_off

    QT1 = dram(nc, P, "QT1", [8, 128, SH], BF16)
    KT1 = dram(nc, P, "KT1", [8, 128, S], BF16)
    V1 = dram(nc, P, "V1", [8, 128, NT, 128], BF16)
    AO = dram(nc, P, "AO1", [1024, SH], BF16)

    xt = [M.alloc([1024], F32, "x1t%d" % i) for i in range(2)]
    junk = M.alloc([1024], BF16, "junk1")
    xb = M.alloc([1024], BF16, "xb1")
    xT = M.alloc([1024], BF16, "xT1")
    st = M.alloc([8], F32, "stats1")
    qk = M.alloc([16, 64], F32, "qk1")
    sq = M.alloc([16, 64], F32, "sq1")
    ssh = M.alloc([16], F32, "ssh1")
    rsh = M.alloc([16], F32, "rsh1")
    qkf = M.alloc([16, 64], BF16, "qkf1")
    QTs = [M.alloc([8, 512], BF16, "Q1s%d" % i) for i in range(2)]
    KTs = [M.alloc([8, 512], BF16, "K1s%d" % i) for i in range(2)]
    Vs = [M.alloc([8, 8, 128], BF16, "V1s%d" % i) for i in range(2)]
    psT = M.psum(0, 512, "ps1T", BF16)
    psP = [M.psum(512 * (1 + i), 512, "ps1P%d" % i) for i in range(6)]
    psO = M.psum(3584, 512, "ps1O", BF16)
    for t in range(ntile_all):
        own = t < ntile_own
        if tile_hook is not None:
            tile_hook(t)
        x_t = xt[t % 2]
        if t == 0:
            P.dma("sp", xt[0], xin(0))
        if t + 1 < ntile_all:
            P.dma("sp", xt[(t + 1) % 2], xin(t + 1))
        P.act(junk, x_t, AF.Square, accum=st[:, 0:1])
        emit_rstd(P, st[:, 0:1], st[:, 1:2], 1024)
        rstd = st[:, 1:2]
        P.copy("pool", xb, x_t)
        for kc in range(8):
            P.tr(psT[:, kc * 128:(kc + 1) * 128], xb[:, kc * 128:(kc + 1) * 128], idb)
        P.copy("act", xT, psT)
        cols = ([0, 512] if own else []) + [1024, 1536, 2048, 2560]
        for i, c0 in enumerate(cols):
            for kc in range(8):
                P.mm(psP[i], xT[:, kc * 128:(kc + 1) * 128], Wp[:, kc, c0:c0 + 512], start=(kc == 0), stop=(kc == 7))
        slot4, j4 = (t // 4) % 2, t % 4
        slot8, j8 = (t // 8) % 2, t % 8
        bi = 0
        for which in (["q"] if own else []) + ["k"]:
            c0 = 0 if which == "q" else 1024
            gain = g_q if which == "q" else g_k
            qflat = qk.rearrange("p h d -> p (h d)")
            for hf in range(2):
                P.stt("dve", qflat[:, hf * 512:(hf + 1) * 512], psP[bi], rstd,
                      bias_bc[:, c0 + hf * 512:c0 + (hf + 1) * 512], ALU.mult, ALU.add)
                bi += 1
            P.tt("pool", sq, qk, qk, ALU.mult)
            P.reduce("dve", ssh, sq)
            emit_rstd(P, ssh, rsh, 64)
            P.tt("dve", qk, qk, rsh.unsq(2).bcast([128, 16, 64]), ALU.mult)
            P.tt("pool", qkf.rearrange("p (h c) d -> p h c d", c=2), qk.rearrange("p (h c) d -> p h c d", c=2),
                 gain.unsq(1).bcast([128, 8, 2, 64]), ALU.mult)
            for h in range(8):
                P.tr(psO[:, h * 128:(h + 1) * 128], qkf[:, 2 * h:2 * h + 2, :].rearrange("p c d -> p (c d)"), idb)
            dst = (QTs if which == "q" else KTs)[slot4]
            P.copy("act", dst[:, :, j4 * 128:(j4 + 1) * 128], psO.rearrange("p (h t) -> p h t", h=8))
        vdst = Vs[slot8][:, j8, :, :].rearrange("p h d -> p (h d)")
        for hf in range(2):
            P.stt("dve", vdst[:, hf * 512:(hf + 1) * 512], psP[bi], rstd,
                  bias_bc[:, 2048 + hf * 512:2048 + (hf + 1) * 512], ALU.mult, ALU.add)
            bi += 1
        if j4 == 3:
            c0 = (t // 4) * 512
            if own:
                P.dma("sp", QT1[:, :, c0:c0 + 512].rearrange("h p t -> p h t"), QTs[slot4], accum_w=True)
            P.dma("sp", KT1[:, :, c0:c0 + 512].rearrange("h p t -> p h t"), KTs[slot4], accum_w=True)
        if j8 == 7 or t == ntile_all - 1:
            g8 = t // 8
            n8 = j8 + 1
            for hh in range(8):
                P.dma("sp", V1[hh][:, g8 * 8:g8 * 8 + n8, :], Vs[slot8][:, 0:n8, hh, :], accum_w=True)
    if dbg is not None:
        dbg["QT1"], dbg["KT1"], dbg["V1"] = QT1, KT1, V1
    P.barrier()
    M.off = keep_attn
    off_proj = keep_attn

    nkt = ntile_all
    nqt = ntile_own // 4
    nko = ntile_own
    KT = [M.alloc([nkt * 128], BF16, "K1T%d" % i) for i in range(2)]
    Vh = [M.alloc([nkt, 128], BF16, "V1h%d" % i) for i in range(2)]
    QT = [M.alloc([nqt * 512], BF16, "Q1T%d" % i) for i in range(2)]
    OT = [M.alloc([nqt * 512], BF16, "O1T%d" % i) for i in range(2)]
    QTz = [[M.alloc([nqt * 512], BF16, "Q1z%d_%d" % (i, c)) for c in range(2)] for i in range(2)]
    for i in range(2):
        for c in range(2):
            P.memset("pool", QTz[i][c], 0.0)
    GSZ = 2
    Pb = [M.alloc([GSZ * 512], BF16, "P1b%d" % i) for i in range(2)]
    Psum = [M.alloc([512], BF16, "P1sum%d" % i) for i in range(2)]
    rl = [M.alloc([512], F32, "rl1_%d" % i) for i in range(2)]
    o0s = M.alloc([512], F32, "o0s")
    o1s = M.alloc([512], F32, "o1s")
    sqb = M.alloc([512], BF16, "sqb")
    rsd = M.alloc([512], F32, "rsd")
    Sg = [M.psum(0, GSZ * 512, "S1g0"), M.psum(1024, GSZ * 512, "S1g1")]
    Oa = [M.psum(2048, 512, "O1a0"), M.psum(3072, 512, "O1a1")]
    La = [M.psum(2560, 512, "L1a0"), M.psum(3584, 512, "L1a1")]

    def seglist(qt):
        segs = []
        lo, hi = 4 * qt - 1, 4 * qt + 4
        left = [k for k in range(0, max(lo, 0))]
        band = [k for k in range(max(lo, 0), min(hi, nko - 1) + 1)]
        right = [k for k in range(min(hi, nko - 1) + 1, nko)]
        other = list(range(nko, nkt))
        cross = []
        if nkt > nko:
            if qt == nqt - 1 and nko in other:
                other.remove(nko)
                cross.append((nko, 0))
            if qt == 0 and (nkt - 1) in other:
                other.remove(nkt - 1)
                cross.append((nkt - 1, 1))
        if left:
            segs.append(("c", 0, left))
        if band:
            segs.append(("b", None, [(k, "own", 512 - (k * 128 - qt * 512)) for k in band]))
        if cross:
            segs.append(("b", None, [(k, xi, 0) for k, xi in cross]))
        if right:
            segs.append(("c", 1, right))
        if other:
            segs.append(("c", 2, other))
        return segs

    tasks = []
    for h in range(8):
        for qt in range(nqt):
            for c in range(2):
                gl = []
                for kind, arg, lst in seglist(qt):
                    for i in range(0, len(lst), GSZ):
                        gl.append((kind, arg, lst[i:i + GSZ]))
                for gi, (kind, arg, lst) in enumerate(gl):
                    tasks.append((h, qt, c, gi, len(gl), kind, arg, lst))

    def load_head(h):
        s = h % 2
        P.dma("sp", KT[s], KT1[h][:, 0:nkt * 128])
        P.dma("sp", Vh[s], V1[h][:, 0:nkt])
        P.dma("sp", QT[s], QT1[h][:, 0:nqt * 512])
        for c in range(2):
            P.copy("pool", QTz[s][c][c * 64:(c + 1) * 64, :], QT[s][c * 64:(c + 1) * 64, :])

    def qk_mm(i):
        h, qt, c, gi, ng, kind, arg, lst = tasks[i]
        s = h % 2
        pr = slice(c * 64, (c + 1) * 64)
        for j, item in enumerate(lst):
            kt = item if kind == "c" else item[0]
            P.mm(Sg[i % 2][:, j * 512:(j + 1) * 512], KT[s][:, kt * 128:(kt + 1) * 128],
                 QTz[s][c][:, qt * 512:(qt + 1) * 512], start=True, stop=(kind == "c"))
            if kind == "b":
                _, tab, off = item
                src = Hk[:, h, off:off + 512] if tab == "own" else Hx[tab][:, h, :]
                P.mm(Sg[i % 2][:, j * 512:(j + 1) * 512], Jb, src, start=False, stop=True)

    pend = {}
    deferred = []

    def ex_pv(i):
        h, qt, c, gi, ng, kind, arg, lst = tasks[i]
        s = h % 2
        n = len(lst)
        pb = Pb[i % 2]
        if kind == "c":
            P.act(pb[:, 0:n * 512], Sg[i % 2][:, 0:n * 512], AF.Exp, bias=bcol[:, arg, h:h + 1])
        else:
            P.act(pb[:, 0:n * 512], Sg[i % 2][:, 0:n * 512], AF.Exp)
        if n == 1:
            lsrc = pb[:, 0:512]
        else:
            lsrc = Psum[i % 2]
            P.tt("dve", lsrc, pb[:, 0:512], pb[:, 512:1024], ALU.add)
        oa, la = Oa[c], La[c]
        for j, item in enumerate(lst):
            kt = item if kind == "c" else item[0]
            pj = pb[:, j * 512:(j + 1) * 512]
            P.mm(oa, Vh[s][:, kt, :], pj, start=(gi == 0 and j == 0), stop=(gi == ng - 1 and j == n - 1))
        if gi > 0:
            P.mm(la, ones, pend["lsrc"], start=(gi == 1), stop=False)
        pend["lsrc"] = lsrc
        if gi == ng - 1:
            P.mm(la, ones, lsrc, start=(gi == 0), stop=True)

            def norm(c=c, oa=oa, la=la):
                P.act(rl[c], la, AF.Ln)
                P.act(rl[c], rl[c], AF.Exp, scale=-1.0)
                if c == 0:
                    P.tt("dve", o0s, oa, rl[0], ALU.mult)
                else:
                    P.tt("dve", o1s, oa, rl[1], ALU.mult)
                    P.stt("dve", o1s, o1s, neglam[:, 0:1], o0s, ALU.mult, ALU.add)
                    P.tt("pool", sqb, o1s, o1s, ALU.mult)
            deferred.append((i + 3, norm))
            if c == 1:
                def fin(h=h, qt=qt, s=s, la=la):
                    P.mm(la, ones, sqb)
                    P.act(rsd, la, AF.Ln, scale=1.0 / 128.0, bias=EPS)
                    P.act(rsd, rsd, AF.Exp, scale=-0.5)
                    P.tt("dve", o1s, o1s, rsd, ALU.mult)
                    P.ts("dve", OT[s][:, qt * 512:(qt + 1) * 512], o1s, sublnc[:, 0:1], None, ALU.mult)
                    if qt == nqt - 1:
                        P.dma("sp", AO[h * 128:(h + 1) * 128, 0:nqt * 512], OT[s])
                        if h + 2 < 8:
                            load_head(h + 2)
                deferred.append((i + 11, fin))

    def run_deferred(i, flush=False):
        while deferred and (flush or deferred[0][0] <= i):
            deferred.pop(0)[1]()

    load_head(0)
    load_head(1)
    for i in range(len(tasks) + 1):
        if i < len(tasks):
            qk_mm(i)
        if i >= 1:
            ex_pv(i - 1)
            run_deferred(i - 1)
    run_deferred(0, flush=True)
    P.barrier()
    M.off = off_proj

    Wout = M.alloc([8, 1024], BF16, "Wout1")
    wst = [M.alloc([1024], F32, "wo1st%d" % i) for i in range(2)]
    for kc in range(8):
        P.dma("sp", wst[kc % 2], io["c_w_out"][kc * 128:(kc + 1) * 128, :])
        P.tt("dve", Wout[:, kc, :], wst[kc % 2], gate_b, ALU.mult)
    AOs = [M.alloc([8, 512], BF16, "AO1s%d" % i) for i in range(2)]
    xo = [M.alloc([1024], F32, "x1o%d" % i) for i in range(4)]
    ys = [M.psum(0, 1024, "y10"), M.psum(1024, 1024, "y11")]

    def ld_ao(g):
        if g * 4 < ntile_own:
            P.dma("sp", AOs[g % 2], AO[:, g * 512:(g + 1) * 512].rearrange("(fc p) t -> p fc t", p=128))

    def ld_x(t):
        if t < ntile_own:
            P.dma("sp", xo[t % 4], xin(t))

    ld_ao(0)
    ld_x(0)
    ld_x(1)
    for t in range(ntile_own):
        g4, j4 = t // 4, t % 4
        a = AOs[g4 % 2]
        if j4 == 0:
            ld_ao(g4 + 1)
        ld_x(t + 2)
        x_t = xo[t % 4]
        y = ys[t % 2]
        for n in range(2):
            for fc in range(8):
                P.mm(y[:, n * 512:(n + 1) * 512], a[:, fc, j4 * 128:(j4 + 1) * 128],
                     Wout[:, fc, n * 512:(n + 1) * 512], start=(fc == 0), stop=(fc == 7))
        P.tt("dve", x_t, x_t, y, ALU.add)
        P.dma("sp", x_out[t * 128:(t + 1) * 128, :], x_t, accum_w=True)
    P.barrier()


L1_SPECS = {
    "x": ([S, D], F32), "ident": ([128, 128], F32), "jmat": ([128, 128], F32), "c_a": ([128, 8], F32),
    "ada_w": ([2, D, 6 * D], F32), "ada_b": ([2, 6 * D], F32), "ada_b_a": ([2, 128, 48], F32),
    "norm_mix_a": ([2, 128, 8], F32), "rel_bias": ([32, 8], F32),
    "bias_oh": ([32, GW + 1280], F32), "bias_sel": ([32, 3], F32),
    "c_w_in": ([D, C_IN], F32), "c_qn": ([1, 128], F32), "c_kn": ([1, 128], F32), "c_lam": ([1, 256], F32),
    "c_subln_a": ([128, 1], F32), "c_w_out": ([D, D], F32),
}


def build_l1_mixer(ntile_all=NT, ntile_own=NTO, debug=False):
    nc = bass.Bass("TRN2", target_bir_lowering=False)
    es = ExitStack()
    with es:
        P = Prog(nc, es)
        M = Mem(nc, es, P, SBUF_WORDS)
        io = declare_inputs(nc, P, L1_SPECS)
        x_out = dram(nc, P, "x1", [SH, D], F32, kind="ExternalOutput")
        dbg = {} if debug else None
        emit_l1_mixer(P, M, io, io["x"], x_out, ntile_all, ntile_own, dbg)
        if debug:
            for k in ("QT1", "KT1", "V1"):
                src = dbg[k]
                o = dram(nc, P, "dbg_" + k, list(src.shape), BF16, kind="ExternalOutput")
                P.dma("sp", o, src, sem="dbgout")
        P.barrier()
        P.emit()
    return nc


def host_inputs_l1(inputs, core, x_rot):
    b, half = core // 2, core % 2
    oh, sel = make_bias_onehots(half)
    return {
        "x": np.ascontiguousarray(x_rot, dtype=np.float32),
        "ident": np.eye(128, dtype=np.float32),
        "jmat": np.ascontiguousarray(np.eye(128, dtype=np.float32)[::-1]),
        "c_a": a_layout(inputs["c"][b]),
        "ada_w": np.asarray(inputs["ada_w"], np.float32),
        "ada_b": np.asarray(inputs["ada_b"], np.float32),
        "ada_b_a": np.stack([a_layout(inputs["ada_b"][l]) for l in range(2)]),
        "norm_mix_a": np.stack([a_layout(inputs["norm_mix"][l]) for l in range(2)]),
        "rel_bias": np.asarray(inputs["rel_bias"], np.float32),
        "bias_oh": oh, "bias_sel": sel,
        "c_w_in": np.asarray(inputs["c_w_in"][0], np.float32),
        "c_qn": np.asarray(inputs["c_qn"][0], np.float32).reshape(1, 128),
        "c_kn": np.asarray(inputs["c_kn"][0], np.float32).reshape(1, 128),
        "c_lam": np.concatenate([inputs["c_lam_q1"][0], inputs["c_lam_k1"][0],
                                 inputs["c_lam_q2"][0], inputs["c_lam_k2"][0]]).astype(np.float32).reshape(1, 256),
        "c_subln_a": np.asarray(inputs["c_subln"][0], np.float32).reshape(128, 1),
        "c_w_out": np.asarray(inputs["c_w_out"][0], np.float32),
    }


def fused_specs():
    sp = {}
    sp.update({
        "x": ([S, D], F32), "ident": ([128, 128], F32), "c_a": ([128, 8], F32),
        "ada_w": ([2, D, 6 * D], F32), "ada_b": ([2, 6 * D], F32), "ada_b_a": ([2, 128, 48], F32),
        "norm_mix_a": ([2, 128, 8], F32), "rt": ([S, 192], F32),
        "ab_w_in": ([D, AB_IN], F32), "ab_q_a_norm_a": ([128, 4], F32), "ab_w_qb": ([512, 768], F32),
        "ab_kv_a_norm_a": ([128, 2], F32), "ab_w_kvb": ([256, 1024], F32),
        "ab_mla_qn": ([1, 96], F32), "ab_mla_kn": ([1, 96], F32), "ab_gqa_qn": ([1, 64], F32),
        "ab_gqa_kn": ([1, 64], F32), "ab_w_out": ([D, D], F32),
    })
    sp.update({k: v for k, v in L1_SPECS.items() if k != "x"})
    sp.update({
        "norm_ffn": ([2, D], F32), "moe_wr": ([2, D, 36], F32), "moe_rb": ([2, 36], F32),
        "tri": ([128, 128], F32), "ebase": ([1, 32], F32), "tokid": ([128, NTO], I32),
        "moe_w_up": ([2, 32, D, D], F32), "moe_w_down": ([2, 32, 512, D], F32),
        "other_off": ([1, 1], I32),
    })
    return sp


def build_fused(na=NT, no=NTO, parts=(1, 1, 1, 1, 1)):
    nc = bass.Bass("TRN2", target_bir_lowering=False)
    es = ExitStack()
    with es:
        P = Prog(nc, es)
        M = Mem(nc, es, P, SBUF_WORDS)
        reg = es.enter_context(nc.gpsimd.register("r_off"))
        io = declare_inputs(nc, P, fused_specs())
        out = dram(nc, P, "out", [SH, D], F32, kind="ExternalOutput")
        XA = dram(nc, P, "XA", [SH, D], F32)
        XBf = dram(nc, P, "XBf", [S, D], F32)
        XG = dram(nc, P, "XG", [S, D], F32)
        XC = dram(nc, P, "XC", [SH, D], F32)
        if parts[0]:
            emit_l0_mixer(P, M, io, io["x"], XA, na, no)
        if parts[1]:
            emit_moe(P, M, io, 0, XA, XBf[0:SH, :], no)
        XB_own = T(XBf.ap[0:SH, :], XBf.buf)
        XB_oth = T(XBf.ap[SH:S, :], P.buf("XB_oth"))
        M.reset()
        osb = M.alloc([8], I32, "osb", parts=1)
        P.dma("pool", osb[:, 0:1], io["other_off"])
        o1 = osb.ap[0:1, 0:1]
        P.op("pool", lambda e: e.reg_load(reg, o1), reads=[osb.buf], noinc=True)
        CH = 256
        NCH = SH // CH
        P.bg.add("ccsem")
        for ci in range(NCH):
            in_ap = XBf.ap[ci * CH:(ci + 1) * CH, :]
            out_ap = XG.ap[ci * 2 * CH:(ci + 1) * 2 * CH, :]
            P.op("pool", (lambda e, a=in_ap, b=out_ap: e.collective_compute(
                "AllGather", ALU.bypass, replica_groups=[[0, 1], [2, 3], [4, 5], [6, 7]], ins=[a], outs=[b])),
                reads=[XBf.buf], writes=[XG.buf], dma="ccsem", dma_inc=1, accum_w=True)
        P.barrier()

        def xrows(t):
            if t < NTO:
                return XB_own[t * 128:(t + 1) * 128, :]
            return XB_oth[(t - NTO) * 128:(t - NTO + 1) * 128, :]

        def hook(t):
            if t == min(16, no - 1):
                src = T(bass.AP(XG.ap.tensor, reg, [[2 * CH * D, NCH], [D, CH], [1, D]]), XG.buf)
                P.dma("pool", XB_oth.rearrange("(c r) d -> c r d", c=NCH), src, sem="xchg", accum_w=True)
                P.bg.discard("ccsem")

        if parts[3]:
            emit_l1_mixer(P, M, io, xrows, XC, na, no, tile_hook=hook)
        if parts[4]:
            emit_moe(P, M, io, 1, XC, out, no)
        P.barrier()
        P.emit()
    return nc


def host_inputs_fused(inputs, core, shared):
    b, half = core // 2, core % 2
    oh, sel = make_bias_onehots(half)
    d = dict(shared)
    d.update({
        "x": np.ascontiguousarray(_rot(np.asarray(inputs["x"][b], np.float32), half)),
        "c_a": a_layout(inputs["c"][b]),
        "rt": make_rt(half),
        "bias_oh": oh, "bias_sel": sel,
        "other_off": np.array([[(1 - half) * 256 * D]], np.int32),
    })
    return d


def host_shared(inputs):
    f = lambda a: np.ascontiguousarray(np.asarray(a, np.float32))
    wr = np.stack([np.concatenate([inputs["moe_w_group"][l],
                                   np.transpose(inputs["moe_w_expert"][l], (1, 0, 2)).reshape(D, 32)], axis=1)
                   for l in range(2)]).astype(np.float32)
    rb = np.stack([np.concatenate([inputs["moe_b_group"][l], inputs["moe_b_expert"][l].reshape(32)])
                   for l in range(2)]).astype(np.float32)
    return {
        "ident": np.eye(128, dtype=np.float32),
        "jmat": np.ascontiguousarray(np.eye(128, dtype=np.float32)[::-1]),
        "ada_w": f(inputs["ada_w"]), "ada_b": f(inputs["ada_b"]),
        "ada_b_a": np.stack([a_layout(inputs["ada_b"][l]) for l in range(2)]),
        "norm_mix_a": np.stack([a_layout(inputs["norm_mix"][l]) for l in range(2)]),
        "ab_w_in": f(inputs["ab_w_in"][0]), "ab_q_a_norm_a": a_layout(inputs["ab_q_a_norm"][0]),
        "ab_w_qb": f(inputs["ab_w_qb"][0]), "ab_kv_a_norm_a": a_layout(inputs["ab_kv_a_norm"][0]),
        "ab_w_kvb": f(inputs["ab_w_kvb"][0]),
        "ab_mla_qn": f(inputs["ab_mla_qn"]).reshape(1, 96), "ab_mla_kn": f(inputs["ab_mla_kn"]).reshape(1, 96),
        "ab_gqa_qn": f(inputs["ab_gqa_qn"]).reshape(1, 64), "ab_gqa_kn": f(inputs["ab_gqa_kn"]).reshape(1, 64),
        "ab_w_out": f(inputs["ab_w_out"][0]),
        "rel_bias": f(inputs["rel_bias"]),
        "c_w_in": f(inputs["c_w_in"][0]),
        "c_qn": f(inputs["c_qn"][0]).reshape(1, 128), "c_kn": f(inputs["c_kn"][0]).reshape(1, 128),
        "c_lam": np.concatenate([inputs["c_lam_q1"][0], inputs["c_lam_k1"][0],
                                 inputs["c_lam_q2"][0], inputs["c_lam_k2"][0]]).astype(np.float32).reshape(1, 256),
        "c_subln_a": f(inputs["c_subln"][0]).reshape(128, 1),
        "c_w_out": f(inputs["c_w_out"][0]),
        "norm_ffn": f(inputs["norm_ffn"]), "moe_wr": wr, "moe_rb": rb,
        "tri": np.triu(np.ones((128, 128), np.float32), 1),
        "ebase": (np.arange(32, dtype=np.float32) * CAP).reshape(1, 32),
        "tokid": (np.arange(NTO)[None, :] * 128 + np.arange(128)[:, None]).astype(np.int32),
        "moe_w_up": f(inputs["moe_w_up"]), "moe_w_down": f(inputs["moe_w_down"]),
    }


_PROGS = {}


def _prog(name, fn):
    if name not in _PROGS:
        _PROGS[name] = fn()
    return _PROGS[name]


def _rot(xb, half):
    return np.concatenate([xb[half * SH:(half + 1) * SH], xb[(1 - half) * SH:(2 - half) * SH]], axis=0)


def kernel_unfused(**inputs):
    inputs = {k: np.asarray(v) for k, v in inputs.items()}
    cores = list(range(8))
    nc = _prog("l0", lambda: build_l0_mixer())
    res = run_bass_kernel_spmd(nc, [host_inputs_l0(inputs, c) for c in cores], core_ids=cores)
    x1 = [np.asarray(r["x1"]) for r in res.results]
    nc = _prog("moe0", lambda: build_moe(0))
    res = run_bass_kernel_spmd(nc, [host_inputs_moe(inputs, c, x1[c], 0) for c in cores], core_ids=cores)
    x2 = [np.asarray(r["x2"]) for r in res.results]
    xb = [np.concatenate([x2[2 * b], x2[2 * b + 1]], axis=0) for b in range(4)]
    nc = _prog("l1", lambda: build_l1_mixer())
    res = run_bass_kernel_spmd(nc, [host_inputs_l1(inputs, c, _rot(xb[c // 2], c % 2)) for c in cores], core_ids=cores)
    x3 = [np.asarray(r["x1"]) for r in res.results]
    nc = _prog("moe1", lambda: build_moe(1))
    res = run_bass_kernel_spmd(nc, [host_inputs_moe(inputs, c, x3[c], 1) for c in cores], core_ids=cores)
    x4 = [np.asarray(r["x2"]) for r in res.results]
    out = np.stack([np.concatenate([x4[2 * b], x4[2 * b + 1]], axis=0) for b in range(4)]).astype(np.float32)
    return out


def kernel(**inputs):
    inputs = {k: np.asarray(v) for k, v in inputs.items()}
    cores = list(range(8))
    nc = _prog("fused", build_fused)
    shared = host_shared(inputs)
    in_maps = [host_inputs_fused(inputs, c, shared) for c in cores]
    res = run_bass_kernel_spmd(nc, in_maps, core_ids=cores)
    xs = [np.asarray(r["out"]) for r in res.results]
    return np.stack([np.concatenate([xs[2 * b], xs[2 * b + 1]], axis=0) for b in range(4)]).astype(np.float32)
```

```python
import math
from contextlib import ExitStack

import numpy as np
import concourse.bass as bass
import concourse.mybir as mybir
from concourse.bass_utils import run_bass_kernel_spmd

F32 = mybir.dt.float32
BF16 = mybir.dt.bfloat16
I32 = mybir.dt.int32
ALU = mybir.AluOpType
AF = mybir.ActivationFunctionType
AX = mybir.AxisListType

D = 1024
S = 8192
SH = 4096
NT = 64
NTO = 32
EPS = 1e-6
AB_IN = 1568


class Buf:
    __slots__ = ("name", "w", "r", "sb")

    def __init__(self, name):
        self.name = name
        self.w = {}
        self.r = {}
        self.sb = False


class T:
    __slots__ = ("ap", "buf")

    def __init__(self, ap, buf):
        self.ap = ap
        self.buf = buf

    def __getitem__(self, k):
        return T(self.ap[k], self.buf)

    def rearrange(self, s, **kw):
        return T(self.ap.rearrange(s, **kw), self.buf)

    def bitcast(self, dt):
        return T(self.ap.bitcast(dt), self.buf)

    def bcast(self, shape):
        return T(self.ap.to_broadcast(list(shape)), self.buf)

    def unsq(self, ax):
        return T(self.ap.unsqueeze(ax), self.buf)

    def pbc(self, n=128):
        return T(self.ap.partition_broadcast(n), self.buf)

    @property
    def shape(self):
        return self.ap.shape


def _ap(x):
    return x.ap if isinstance(x, T) else x


def _bufs(*xs):
    out = []
    for x in xs:
        if isinstance(x, T) and x.buf not in out:
            out.append(x.buf)
    return out


class Prog:
    ENG = ("pe", "act", "dve", "pool", "sp")

    def __init__(self, nc, es):
        self.nc = nc
        self.es = es
        self.q = {e: [] for e in self.ENG}
        self.esem = {e: es.enter_context(nc.semaphore("s_" + e)) for e in ("pe", "act", "dve", "pool")}
        self.ecnt = {e: 0 for e in ("pe", "act", "dve", "pool")}
        self.waited = {e: {} for e in self.ENG}
        self.dsems = {}
        self.free_dsems = []
        self.bg = set()
        self.nsem = 0
        self.nbuf = 0

    def buf(self, name=None):
        self.nbuf += 1
        return Buf(name or ("b%d" % self.nbuf))

    def _dsem(self, name):
        if name not in self.dsems:
            if self.free_dsems:
                self.dsems[name] = self.free_dsems.pop()
            else:
                self.nsem += 1
                self.dsems[name] = [self.es.enter_context(self.nc.semaphore("d_%d" % self.nsem)), 0]
        return self.dsems[name]

    def op(self, eng, fn, reads=(), writes=(), dma=None, n=1, accum_w=False, dma_inc=16, noinc=False):
        need = {}

        def add(tok):
            sem, val, src = tok
            if src == "pe" and eng == "pe":
                return
            k = id(sem)
            if k not in need or need[k][1] < val:
                need[k] = (sem, val)

        for b in reads:
            for tok in b.w.values():
                add(tok)
        for b in writes:
            if not accum_w:
                for tok in b.w.values():
                    add(tok)
            for tok in b.r.values():
                add(tok)
        waits = []
        wd = self.waited[eng]
        for k, (sem, val) in need.items():
            if wd.get(k, 0) < val:
                wd[k] = val
                waits.append((sem, val))
        if dma is not None:
            rec = self._dsem(dma)
            rec[1] += dma_inc * n
            tok = (rec[0], rec[1], "dma")
            inc = (rec[0], dma_inc)
        else:
            self.ecnt[eng] += 1
            tok = (self.esem[eng], self.ecnt[eng], eng)
            inc = (self.esem[eng], 1)
        if noinc:
            self.ecnt[eng] -= 1
            self.q[eng].append((waits, fn, None))
            return None
        self.q[eng].append((waits, fn, inc))
        k = id(tok[0])
        for b in reads:
            b.r[k] = tok
        for b in writes:
            if accum_w:
                b.w[k] = tok
            else:
                b.w = {k: tok}
            b.r = {}
        return tok

    def mm(self, out, lhsT, rhs, start=True, stop=True, extra_r=()):
        o, a, b = _ap(out), _ap(lhsT), _ap(rhs)
        self.op("pe", lambda e: e.matmul(o, lhsT=a, rhs=b, start=start, stop=stop),
                reads=_bufs(lhsT, rhs) + list(extra_r), writes=_bufs(out))

    def tr(self, out, in_, ident):
        o, a, i = _ap(out), _ap(in_), _ap(ident)
        self.op("pe", lambda e: e.transpose(o, a, i), reads=_bufs(in_, ident), writes=_bufs(out))

    def act(self, out, in_, func, bias=None, scale=None, accum=None):
        o, a = _ap(out), _ap(in_)
        kw = {}
        if bias is not None:
            kw["bias"] = _ap(bias)
        if scale is not None:
            kw["scale"] = _ap(scale)
        if accum is not None:
            kw["accum_out"] = _ap(accum)
        self.op("act", lambda e: e.activation(out=o, in_=a, func=func, **kw),
                reads=_bufs(in_, bias, scale), writes=_bufs(out, accum))

    def tt(self, eng, out, in0, in1, op):
        o, a, b = _ap(out), _ap(in0), _ap(in1)
        self.op(eng, lambda e: e.tensor_tensor(out=o, in0=a, in1=b, op=op),
                reads=_bufs(in0, in1), writes=_bufs(out))

    def ts(self, eng, out, in0, s1, s2, op0, op1=None):
        o, a, x1, x2 = _ap(out), _ap(in0), _ap(s1), _ap(s2)
        if op1 is None:
            self.op(eng, lambda e: e.tensor_scalar(out=o, in0=a, scalar1=x1, scalar2=None, op0=op0),
                    reads=_bufs(in0, s1), writes=_bufs(out))
        else:
            self.op(eng, lambda e: e.tensor_scalar(out=o, in0=a, scalar1=x1, scalar2=x2, op0=op0, op1=op1),
                    reads=_bufs(in0, s1, s2), writes=_bufs(out))

    def stt(self, eng, out, in0, scalar, in1, op0, op1):
        o, a, s, b = _ap(out), _ap(in0), _ap(scalar), _ap(in1)
        self.op(eng, lambda e: e.scalar_tensor_tensor(out=o, in0=a, scalar=s, in1=b, op0=op0, op1=op1),
                reads=_bufs(in0, scalar, in1), writes=_bufs(out))

    def copy(self, eng, out, in_):
        o, a = _ap(out), _ap(in_)
        if eng == "act":
            self.op(eng, lambda e: e.activation(out=o, in_=a, func=AF.Copy), reads=_bufs(in_), writes=_bufs(out))
        else:
            self.op(eng, lambda e: e.tensor_copy(out=o, in_=a), reads=_bufs(in_), writes=_bufs(out))

    def memset(self, eng, out, val):
        o = _ap(out)
        self.op(eng, lambda e: e.memset(o, val), writes=_bufs(out))

    def reduce(self, eng, out, in_, op=ALU.add, axis=AX.X):
        o, a = _ap(out), _ap(in_)
        self.op(eng, lambda e: e.tensor_reduce(out=o, in_=a, axis=axis, op=op), reads=_bufs(in_), writes=_bufs(out))

    def recip(self, out, in_):
        o, a = _ap(out), _ap(in_)
        self.op("dve", lambda e: e.reciprocal(out=o, in_=a), reads=_bufs(in_), writes=_bufs(out))

    def dma(self, eng, out, in_, sem=None, accum_w=False):
        o, a = _ap(out), _ap(in_)
        if sem is None:
            sb = out if (isinstance(out, T) and getattr(out.buf, "sb", False)) else in_
            sem = sb.buf.name
        self.op(eng, lambda e: e.dma_start(out=o, in_=a), reads=_bufs(in_), writes=_bufs(out), dma=sem, accum_w=accum_w)

    def barrier(self):
        toks = [(self.esem[e], self.ecnt[e]) for e in self.esem if self.ecnt[e] > 0]
        toks += [(rec[0], rec[1]) for name, rec in self.dsems.items() if rec[1] > 0 and name not in self.bg]
        for eng in self.ENG:
            waits = []
            wd = self.waited[eng]
            for sem, val in toks:
                if wd.get(id(sem), 0) < val:
                    wd[id(sem)] = val
                    waits.append((sem, val))
            if waits:
                self.q[eng].append((waits, None, None))
        self.free_dsems.extend(r for n_, r in self.dsems.items() if n_ not in self.bg)
        self.dsems = {n_: r for n_, r in self.dsems.items() if n_ in self.bg}

    def emit(self):
        nc = self.nc
        block = self.es.enter_context(nc.Block())

        def run(engname):
            def f(e):
                for waits, fn, inc in self.q[engname]:
                    for sem, val in waits:
                        e.wait_ge(sem, val)
                    if fn is not None:
                        ins = fn(e)
                        if inc is not None:
                            ins.then_inc(inc[0], inc[1])
            return f

        block.tensor(run("pe"))
        block.scalar(run("act"))
        block.vector(run("dve"))
        block.gpsimd(run("pool"))
        block.sync(run("sp"))


class Mem:
    def __init__(self, nc, es, P, sbuf_words):
        self.P = P
        self.sb = es.enter_context(nc.sbuf_tensor("arena", [128, sbuf_words], F32))
        self.ps = es.enter_context(nc.psum_tensor("psum", [128, 4096], F32))
        self.words = sbuf_words
        self.off = 0
        self.keep = 0

    def reset(self):
        self.off = self.keep

    def alloc(self, free_shape, dt=F32, name=None, parts=128):
        n = 1
        for s in free_shape:
            n *= s
        sz = 4 if dt in (F32, I32) else 2
        words = (n * sz + 3) // 4
        words = (words + 7) // 8 * 8
        assert self.off + words <= self.words, ("SBUF arena overflow", name, self.off, words, self.words)
        ap = self.sb[0:parts, self.off:self.off + words]
        self.off += words
        if dt != F32:
            ap = ap.bitcast(dt)
        ap = ap[:, 0:n]
        if len(free_shape) == 2:
            ap = ap.rearrange("p (a b) -> p a b", a=free_shape[0], b=free_shape[1])
        elif len(free_shape) == 3:
            ap = ap.rearrange("p (a b c) -> p a b c", a=free_shape[0], b=free_shape[1], c=free_shape[2])
        b = self.P.buf(name)
        b.sb = True
        return T(ap, b)

    def psum(self, col0, ncols, name=None, dt=F32):
        ap = self.ps[:, col0:col0 + ncols]
        if dt != F32:
            ap = ap.bitcast(dt)
        b = self.P.buf(name)
        return T(ap, b)


def dram(nc, P, name, shape, dt, kind="Internal"):
    t = nc.dram_tensor(name, list(shape), dt, kind=kind)
    return T(t.ap(), P.buf(name))


def emit_consts(P, M, io):
    idb = M.alloc([128], BF16, "idb")
    idf = M.alloc([128], F32, "idf")
    P.dma("sp", idf, io["ident"])
    P.copy("dve", idb, idf)
    return {"idb": idb}


def emit_mod(P, M, io, l, need_a, need_b):
    outA = {c: M.alloc([8], F32, "modA%d" % c) for c in need_a}
    outB = {c: M.alloc([1024], F32, "modB%d" % c) for c in need_b}
    keep = M.off
    cc = M.alloc([8], F32, "c_col")
    P.dma("sp", cc, io["c_a"])
    cond = M.alloc([8], F32, "cond")
    P.act(cond, cc, AF.Silu)
    crep = M.alloc([8, 128], F32, "cond_rep")
    P.copy("dve", crep, cond.unsq(2).bcast([128, 8, 128]))
    adab_a = M.alloc([48], F32, "adab_a")
    P.dma("sp", adab_a, io["ada_b_a"][l])
    wbuf = [M.alloc([8, 1024], F32, "adaw%d" % i) for i in range(2)]
    psA = M.psum(0, 8, "psA")
    psB = M.psum(512, 1024, "psB")
    chunks = sorted(set(need_a) | set(need_b))
    for i, c in enumerate(chunks):
        w = wbuf[i % 2]
        P.dma("sp", w, io["ada_w"][l][:, c * 1024:(c + 1) * 1024].rearrange("(kc p) n -> p kc n", p=128))
        if c in need_a:
            for fc in range(8):
                for kc in range(8):
                    P.mm(psA[:, fc:fc + 1], w[:, kc, fc * 128:(fc + 1) * 128], cond[:, kc:kc + 1],
                         start=(kc == 0), stop=(kc == 7))
            P.tt("dve", outA[c], psA, adab_a[:, c * 8:(c + 1) * 8], ALU.add)
        if c in need_b:
            for n in range(2):
                for kc in range(8):
                    P.mm(psB[:, n * 512:(n + 1) * 512], crep[:, kc, :], w[:, kc, n * 512:(n + 1) * 512],
                         start=(kc == 0), stop=(kc == 7))
            bb = outB[c]
            P.dma("sp", bb, io["ada_b"][l:l + 1, c * 1024:(c + 1) * 1024].pbc())
            P.tt("dve", bb, psB, bb, ALU.add)
    P.barrier()
    M.off = keep
    return outA, outB


def emit_rstd(P, ss, out, n, tmp=None):
    P.act(out, ss, AF.Ln, scale=1.0 / n, bias=EPS)
    P.act(out, out, AF.Exp, scale=-0.5)


def emit_l0_mixer(P, M, io, x_in, x_out, ntile_all=NT, ntile_own=NTO, dbg=None):
    nc = P.nc
    M.reset()
    cst = emit_consts(P, M, io)
    idb = cst["idb"]
    modA, modB = emit_mod(P, M, io, 0, need_a=[0, 1], need_b=[2])
    shift_a, scale_a, gate_b = modA[0], modA[1], modB[2]
    nm_a = M.alloc([8], F32, "nm_a")
    P.dma("sp", nm_a, io["norm_mix_a"][0])
    gs_a = M.alloc([8], F32, "gs_a")
    P.stt("dve", gs_a, scale_a, 1.0, nm_a, ALU.add, ALU.mult)

    Wp = M.alloc([8, AB_IN], BF16, "Wp")
    bias_bc = M.alloc([AB_IN], F32, "bias_bc")
    Wqb = M.alloc([4, 768], BF16, "Wqb")
    Wkvb = M.alloc([2, 1024], BF16, "Wkvb")
    g_mq = M.alloc([96], F32, "g_mq")
    g_mk = M.alloc([96], F32, "g_mk")
    g_gq = M.alloc([64], F32, "g_gq")
    g_gk = M.alloc([64], F32, "g_gk")
    keep_off = M.off

    shrep = M.alloc([8, 128], BF16, "shift_rep")
    P.copy("dve", shrep, shift_a.unsq(2).bcast([128, 8, 128]))
    Wo = M.alloc([8, AB_IN], BF16, "Wo")
    stg = [M.alloc([AB_IN], F32, "wstg%d" % i) for i in range(2)]
    for kc in range(8):
        s = stg[kc % 2]
        P.dma("sp", s, io["ab_w_in"][kc * 128:(kc + 1) * 128, :])
        P.act(Wp[:, kc, :], s, AF.Copy, scale=gs_a[:, kc:kc + 1])
        P.copy("pool", Wo[:, kc, :], s)
    for n0 in range(0, AB_IN, 512):
        n1 = min(AB_IN, n0 + 512)
        ps = M.psum((n0 // 512) * 512, n1 - n0, "psbias%d" % n0)
        for kc in range(8):
            P.mm(ps, shrep[:, kc, :], Wo[:, kc, n0:n1], start=(kc == 0), stop=(kc == 7))
        P.copy("dve", bias_bc[:, n0:n1], ps)
    qan = M.alloc([4], F32, "qan")
    P.dma("sp", qan, io["ab_q_a_norm_a"])
    kvan = M.alloc([2], F32, "kvan")
    P.dma("sp", kvan, io["ab_kv_a_norm_a"])
    for kc in range(4):
        s = stg[kc % 2]
        P.dma("sp", s[:, 0:768], io["ab_w_qb"][kc * 128:(kc + 1) * 128, :])
        P.act(Wqb[:, kc, :], s[:, 0:768], AF.Copy, scale=qan[:, kc:kc + 1])
    for kc in range(2):
        s = stg[kc % 2]
        P.dma("sp", s[:, 0:1024], io["ab_w_kvb"][kc * 128:(kc + 1) * 128, :])
        P.act(Wkvb[:, kc, :], s[:, 0:1024], AF.Copy, scale=kvan[:, kc:kc + 1])
    P.dma("sp", g_mq, io["ab_mla_qn"].pbc())
    P.ts("dve", g_mq, g_mq, 1.0 / math.sqrt(96.0), None, ALU.mult)
    P.dma("sp", g_mk, io["ab_mla_kn"].pbc())
    P.dma("sp", g_gq, io["ab_gqa_qn"].pbc())
    P.ts("dve", g_gq, g_gq, 0.125, None, ALU.mult)
    P.dma("sp", g_gk, io["ab_gqa_kn"].pbc())
    P.barrier()
    M.off = keep_off

    QTm = dram(nc, P, "QTm", [8, 96, SH], BF16)
    QTg = dram(nc, P, "QTg", [8, 64, SH], BF16)
    KTm = dram(nc, P, "KTm", [8, 96, S], BF16)
    KTg = dram(nc, P, "KTg", [2, 64, S], BF16)
    Vm = dram(nc, P, "Vm", [8, 128, NT, 128], BF16)
    Vg = dram(nc, P, "Vg", [2, 128, NT, 128], BF16)
    AO = dram(nc, P, "AO", [1024, SH], BF16)

    off_proj = M.off
    xt = [M.alloc([1024], F32, "xt%d" % i) for i in range(2)]
    rt = [M.alloc([192], F32, "rt%d" % i) for i in range(2)]
    junk = M.alloc([1024], BF16, "junk")
    xb = M.alloc([1024], BF16, "xb")
    xT = M.alloc([1024], BF16, "xT")
    st = M.alloc([16], F32, "stats")
    cq = M.alloc([512], F32, "cq")
    ckvpe = M.alloc([288], F32, "ckvpe")
    qg = M.alloc([8, 64], F32, "qg")
    kvg = M.alloc([256], F32, "kvg")
    cqb = M.alloc([512], BF16, "cqb")
    cqT = M.alloc([512], BF16, "cqT")
    ckvb = M.alloc([256], BF16, "ckvb")
    ckvT = M.alloc([256], BF16, "ckvT")
    q = M.alloc([8, 96], F32, "q")
    sq = M.alloc([1024], F32, "sq")
    ssh = M.alloc([8], F32, "ssh")
    rsh = M.alloc([8], F32, "rsh")
    qpe = M.alloc([8, 32], F32, "qpe")
    t1 = M.alloc([8, 64], F32, "t1")
    t2 = M.alloc([8, 64], F32, "t2")
    qf = M.alloc([8, 96], BF16, "qf")
    qgf = M.alloc([8, 64], BF16, "qgf")
    kv = M.alloc([8, 128], F32, "kv")
    kf = M.alloc([8, 96], BF16, "kf")
    kpe = M.alloc([32], F32, "kpe")
    kgn = M.alloc([2, 64], F32, "kgn")
    kgf = M.alloc([2, 64], BF16, "kgf")
    QTs = [M.alloc([8, 512], BF16, "QTs%d" % i, parts=96) for i in range(2)]
    QTgs = [M.alloc([8, 512], BF16, "QTgs%d" % i, parts=64) for i in range(2)]
    KTs = [M.alloc([8, 512], BF16, "KTs%d" % i, parts=96) for i in range(2)]
    KTgs = [M.alloc([2, 512], BF16, "KTgs%d" % i, parts=64) for i in range(2)]
    Vs = [M.alloc([8, 8, 128], BF16, "Vs%d" % i) for i in range(2)]
    Vgs = [M.alloc([8, 2, 128], BF16, "Vgs%d" % i) for i in range(2)]
    for i in range(2):
        P.memset("pool", Vs[i], 1.0)
        P.memset("pool", Vgs[i], 1.0)

    psT = M.psum(0, 512, "psT", BF16)
    psA = M.psum(512, 512, "psA")
    psBk = M.psum(1024, 512, "psB")
    psC = M.psum(1536, 512, "psC")
    psD = M.psum(2048, 512, "psD")
    psQ = M.psum(2560, 1024, "psQ")
    psO = M.psum(3584, 512, "psO", BF16)

    def head_norm_rope(src3, nh, d, gain, rope_w, cosT, ssinT, dst_bf, name):
        P.tt("pool", sq[:, 0:nh * d].rearrange("p (h d) -> p h d", h=nh), src3, src3, ALU.mult)
        P.reduce("dve", ssh[:, 0:nh], sq[:, 0:nh * d].rearrange("p (h d) -> p h d", h=nh))
        emit_rstd(P, ssh[:, 0:nh], rsh[:, 0:nh], d)
        P.tt("dve", src3, src3, rsh[:, 0:nh].unsq(2).bcast([128, nh, d]), ALU.mult)
        return

    def ld_proj(t):
        if t < ntile_all:
            P.dma("sp", xt[t % 2], x_in[t * 128:(t + 1) * 128, :])
            P.dma("sp", rt[t % 2], io["rt"][t * 128:(t + 1) * 128, :])

    ld_proj(0)
    for t in range(ntile_all):
        own = t < ntile_own
        x_t = xt[t % 2]
        r_t = rt[t % 2]
        ld_proj(t + 1)
        P.act(junk, x_t, AF.Square, accum=st[:, 0:1])
        emit_rstd(P, st[:, 0:1], st[:, 1:2], 1024)
        rstd = st[:, 1:2]
        P.copy("pool", xb, x_t)
        for kc in range(8):
            P.tr(psT[:, kc * 128:(kc + 1) * 128], xb[:, kc * 128:(kc + 1) * 128], idb)
        P.copy("act", xT, psT)
        segs = []
        if own:
            segs += [(psA, 0, 512, cq), (psC, 800, 1312, qg.rearrange("p h d -> p (h d)"))]
        segs += [(psBk, 512, 800, ckvpe), (psD, 1312, 1568, kvg)]
        for ps, c0, c1, dst in segs:
            for kc in range(8):
                P.mm(ps[:, 0:c1 - c0], xT[:, kc * 128:(kc + 1) * 128], Wp[:, kc, c0:c1],
                     start=(kc == 0), stop=(kc == 7))
        for ps, c0, c1, dst in segs:
            P.stt("dve", dst, ps[:, 0:c1 - c0], rstd, bias_bc[:, c0:c1], ALU.mult, ALU.add)
        slot4 = (t // 4) % 2
        j4 = t % 4
        cosm, ssinm = r_t[:, 0:32], r_t[:, 32:64]
        cosa, ssina = r_t[:, 64:128], r_t[:, 128:192]
        if own:
            P.act(junk[:, 0:512], cq, AF.Square, accum=st[:, 2:3])
            emit_rstd(P, st[:, 2:3], st[:, 3:4], 512)
            P.copy("pool", cqb, cq)
            for kc in range(4):
                P.tr(psT[:, kc * 128:(kc + 1) * 128], cqb[:, kc * 128:(kc + 1) * 128], idb)
            P.copy("act", cqT, psT[:, 0:512])
            for n0, n1 in ((0, 512), (512, 768)):
                for kc in range(4):
                    P.mm(psQ[:, n0:n1], cqT[:, kc * 128:(kc + 1) * 128], Wqb[:, kc, n0:n1],
                         start=(kc == 0), stop=(kc == 3))
            qflat = q.rearrange("p h d -> p (h d)")
            P.ts("dve", qflat, psQ[:, 0:768], st[:, 3:4], None, ALU.mult)
            head_norm_rope(q, 8, 96, None, None, None, None, None, "q")
            P.tt("pool", qf[:, :, 0:64], q[:, :, 0:64], g_mq[:, 0:64].unsq(1).bcast([128, 8, 64]), ALU.mult)
            P.tt("dve", qpe, q[:, :, 64:96], g_mq[:, 64:96].unsq(1).bcast([128, 8, 32]), ALU.mult)
            P.tt("pool", t1[:, :, 0:32], qpe, cosm.unsq(1).bcast([128, 8, 32]), ALU.mult)
            P.tt("dve", t2[:, :, 0:16], qpe[:, :, 16:32], ssinm[:, 0:16].unsq(1).bcast([128, 8, 16]), ALU.mult)
            P.tt("dve", t2[:, :, 16:32], qpe[:, :, 0:16], ssinm[:, 16:32].unsq(1).bcast([128, 8, 16]), ALU.mult)
            P.tt("dve", qf[:, :, 64:96], t1[:, :, 0:32], t2[:, :, 0:32], ALU.add)
            for h in range(8):
                P.tr(psO[0:96, h * 128:(h + 1) * 128], qf[:, h, :], idb)
            P.copy("act", QTs[slot4][:, :, j4 * 128:(j4 + 1) * 128],
                   psO[0:96, :].rearrange("p (h t) -> p h t", h=8))
            head_norm_rope(qg, 8, 64, None, None, None, None, None, "qg")
            P.tt("pool", qg, qg, g_gq.unsq(1).bcast([128, 8, 64]), ALU.mult)
            P.tt("pool", t1, qg, cosa.unsq(1).bcast([128, 8, 64]), ALU.mult)
            qg5 = qg.rearrange("p h (b f e) -> p h b f e", b=2, f=2)
            t25 = t2.rearrange("p h (b f e) -> p h b f e", b=2, f=2)
            ss5 = ssina.rearrange("p (b f e) -> p b f e", b=2, f=2)
            for f in range(2):
                for bb in range(2):
                    P.tt("dve", t25[:, :, bb, f, :], qg5[:, :, bb, 1 - f, :],
                         ss5[:, bb, f, :].unsq(1).bcast([128, 8, 16]), ALU.mult)
            P.tt("dve", qgf, t1, t2, ALU.add)
            for h in range(8):
                P.tr(psO[0:64, h * 128:(h + 1) * 128], qgf[:, h, :], idb)
            P.copy("act", QTgs[slot4][:, :, j4 * 128:(j4 + 1) * 128],
                   psO[0:64, :].rearrange("p (h t) -> p h t", h=8))
        ckv = ckvpe[:, 0:256]
        P.act(junk[:, 0:256], ckv, AF.Square, accum=st[:, 4:5])
        emit_rstd(P, st[:, 4:5], st[:, 5:6], 256)
        P.copy("pool", ckvb, ckv)
        for kc in range(2):
            P.tr(psT[:, kc * 128:(kc + 1) * 128], ckvb[:, kc * 128:(kc + 1) * 128], idb)
        P.copy("act", ckvT, psT[:, 0:256])
        for n0, n1 in ((0, 512), (512, 1024)):
            for kc in range(2):
                P.mm(psQ[:, n0:n1], ckvT[:, kc * 128:(kc + 1) * 128], Wkvb[:, kc, n0:n1],
                     start=(kc == 0), stop=(kc == 1))
        kvflat = kv.rearrange("p h d -> p (h d)")
        P.ts("dve", kvflat, psQ, st[:, 5:6], None, ALU.mult)
        slot16 = (t // 8) % 2
        j16 = t % 8
        P.copy("pool", Vs[slot16][:, j16, :, 0:64], kv[:, :, 64:128])
        sq3 = sq[:, 0:512].rearrange("p (h d) -> p h d", h=8)
        P.tt("pool", sq3, kv[:, :, 0:64], kv[:, :, 0:64], ALU.mult)
        P.reduce("dve", ssh, sq3)
        P.act(junk[:, 0:32], ckvpe[:, 256:288], AF.Square, accum=st[:, 6:7])
        P.ts("dve", ssh, ssh, st[:, 6:7], None, ALU.add)
        emit_rstd(P, ssh, rsh, 96)
        P.tt("dve", kv[:, :, 0:64], kv[:, :, 0:64], rsh.unsq(2).bcast([128, 8, 64]), ALU.mult)
        P.tt("pool", kf[:, :, 0:64], kv[:, :, 0:64], g_mk[:, 0:64].unsq(1).bcast([128, 8, 64]), ALU.mult)
        P.tt("dve", kpe, ckvpe[:, 256:288], g_mk[:, 64:96], ALU.mult)
        tk1 = t1[:, 0, 0:32]
        tk2 = t2[:, 0, 0:32]
        P.tt("dve", tk1, kpe, cosm, ALU.mult)
        P.tt("dve", tk2[:, 0:16], kpe[:, 16:32], ssinm[:, 0:16], ALU.mult)
        P.tt("dve", tk2[:, 16:32], kpe[:, 0:16], ssinm[:, 16:32], ALU.mult)
        P.tt("dve", kpe, tk1, tk2, ALU.add)
        P.tt("dve", kf[:, :, 64:96], kpe.unsq(1).bcast([128, 8, 32]), rsh.unsq(2).bcast([128, 8, 32]), ALU.mult)
        for h in range(8):
            P.tr(psO[0:96, h * 128:(h + 1) * 128], kf[:, h, :], idb)
        P.copy("act", KTs[slot4][:, :, j4 * 128:(j4 + 1) * 128],
               psO[0:96, :].rearrange("p (h t) -> p h t", h=8))
        kg = kvg[:, 0:128].rearrange("p (h d) -> p h d", h=2)
        P.copy("pool", Vgs[slot16][:, j16, :, 0:64], kvg[:, 128:256].rearrange("p (h d) -> p h d", h=2))
        sqg = sq[:, 0:128].rearrange("p (h d) -> p h d", h=2)
        P.tt("pool", sqg, kg, kg, ALU.mult)
        P.reduce("dve", ssh[:, 0:2], sqg)
        emit_rstd(P, ssh[:, 0:2], rsh[:, 0:2], 64)
        P.tt("dve", kgn, kg, rsh[:, 0:2].unsq(2).bcast([128, 2, 64]), ALU.mult)
        P.tt("dve", kgn, kgn, g_gk.unsq(1).bcast([128, 2, 64]), ALU.mult)
        tg1 = t1[:, 0:2, :]
        tg2 = t2[:, 0:2, :]
        P.tt("pool", tg1, kgn, cosa.unsq(1).bcast([128, 2, 64]), ALU.mult)
        kg5 = kgn.rearrange("p h (b f e) -> p h b f e", b=2, f=2)
        tg25 = tg2.rearrange("p h (b f e) -> p h b f e", b=2, f=2)
        ss5 = ssina.rearrange("p (b f e) -> p b f e", b=2, f=2)
        for f in range(2):
            for bb in range(2):
                P.tt("dve", tg25[:, :, bb, f, :], kg5[:, :, bb, 1 - f, :],
                     ss5[:, bb, f, :].unsq(1).bcast([128, 2, 16]), ALU.mult)
        P.tt("dve", kgf, tg1, tg2, ALU.add)
        for h in range(2):
            P.tr(psO[0:64, h * 128:(h + 1) * 128], kgf[:, h, :], idb)
        P.copy("act", KTgs[slot4][:, :, j4 * 128:(j4 + 1) * 128],
               psO[0:64, 0:256].rearrange("p (h t) -> p h t", h=2))
        if j4 == 3:
            g4 = t // 4
            c0 = g4 * 512
            if own:
                P.dma("sp", QTm[:, :, c0:c0 + 512].rearrange("h p t -> p h t"), QTs[slot4], accum_w=True)
                P.dma("sp", QTg[:, :, c0:c0 + 512].rearrange("h p t -> p h t"), QTgs[slot4], accum_w=True)
            P.dma("sp", KTm[:, :, c0:c0 + 512].rearrange("h p t -> p h t"), KTs[slot4], accum_w=True)
            P.dma("sp", KTg[:, :, c0:c0 + 512].rearrange("h p t -> p h t"), KTgs[slot4], accum_w=True)
        if j16 == 7 or t == ntile_all - 1:
            g16 = t // 8
            nt16 = j16 + 1
            for hh in range(8):
                P.dma("sp", Vm[hh][:, g16 * 8:g16 * 8 + nt16, :], Vs[slot16][:, 0:nt16, hh, :], accum_w=True)
            for hh in range(2):
                P.dma("sp", Vg[hh][:, g16 * 8:g16 * 8 + nt16, :], Vgs[slot16][:, 0:nt16, hh, :], accum_w=True)
    if dbg is not None:
        dbg["QTm"], dbg["QTg"], dbg["KTm"], dbg["KTg"], dbg["Vm"], dbg["Vg"] = QTm, QTg, KTm, KTg, Vm, Vg
    P.barrier()
    M.off = off_proj

    nkt = ntile_all
    nqt = ntile_own // 4
    KT = [M.alloc([nkt * 128], BF16, "KT%d" % i, parts=96) for i in range(2)]
    Vh = [M.alloc([nkt, 128], BF16, "Vh%d" % i) for i in range(2)]
    QT = [M.alloc([nqt * 512], BF16, "QT%d" % i, parts=96) for i in range(2)]
    OT = [M.alloc([nqt * 512], BF16, "OT%d" % i, parts=64) for i in range(2)]
    Pb = [M.alloc([1536], BF16, "Pb%d" % i) for i in range(2)]
    rl = M.alloc([512], F32, "rl", parts=64)
    Sg = [M.psum(0, 1536, "Sg0"), M.psum(1536, 1536, "Sg1")]
    Oa = [M.psum(3072, 512, "Oa0"), M.psum(3584, 512, "Oa1")]
    groups = []
    k0 = 0
    while k0 < nkt:
        n = 3 if (nkt - k0) not in (2, 4) else 2
        n = min(n, nkt - k0)
        groups.append(list(range(k0, k0 + n)))
        k0 += n

    def load_head(h):
        s = h % 2
        if h < 8:
            P.dma("sp", KT[s], KTm[h][:, 0:nkt * 128])
            P.dma("sp", Vh[s], Vm[h][:, 0:nkt])
            P.dma("sp", QT[s], QTm[h][:, 0:nqt * 512])
        else:
            P.dma("sp", KT[s][0:64], KTg[(h - 8) // 4][:, 0:nkt * 128])
            P.dma("sp", Vh[s], Vg[(h - 8) // 4][:, 0:nkt])
            P.dma("sp", QT[s][0:64], QTg[h - 8][:, 0:nqt * 512])

    tasks = []
    for h in range(16):
        for qt in range(nqt):
            for gi, g in enumerate(groups):
                tasks.append((h, qt, gi, g))

    def qk(i):
        h, qt, gi, g = tasks[i]
        s = h % 2
        d = 96 if h < 8 else 64
        for j, kt in enumerate(g):
            P.mm(Sg[i % 2][:, j * 512:(j + 1) * 512], KT[s][0:d, kt * 128:(kt + 1) * 128],
                 QT[s][0:d, qt * 512:(qt + 1) * 512])

    def ex_pv(i):
        h, qt, gi, g = tasks[i]
        s = h % 2
        n = len(g)
        P.act(Pb[i % 2][:, 0:n * 512], Sg[i % 2][:, 0:n * 512], AF.Exp)
        oa = Oa[(h * nqt + qt) % 2]
        for j, kt in enumerate(g):
            P.mm(oa, Vh[s][:, kt, :], Pb[i % 2][:, j * 512:(j + 1) * 512],
                 start=(kt == 0), stop=(kt == nkt - 1))
        if gi == len(groups) - 1:
            P.recip(rl, oa[64:128, :])
            P.tt("dve", OT[s][:, qt * 512:(qt + 1) * 512], oa[0:64, :], rl, ALU.mult)
            if qt == nqt - 1:
                P.dma("sp", AO[h * 64:(h + 1) * 64, 0:nqt * 512], OT[s])
                if h + 2 < 16:
                    load_head(h + 2)

    load_head(0)
    load_head(1)
    for i in range(len(tasks) + 1):
        if i < len(tasks):
            qk(i)
        if i >= 1:
            ex_pv(i - 1)
    P.barrier()
    M.off = off_proj

    Wout = M.alloc([8, 1024], BF16, "Wout")
    wst = [M.alloc([1024], F32, "wost%d" % i) for i in range(2)]
    for kc in range(8):
        P.dma("sp", wst[kc % 2], io["ab_w_out"][kc * 128:(kc + 1) * 128, :])
        P.tt("dve", Wout[:, kc, :], wst[kc % 2], gate_b, ALU.mult)
    AOs = [M.alloc([8, 512], BF16, "AOs%d" % i) for i in range(2)]
    xo = [M.alloc([1024], F32, "xo%d" % i) for i in range(4)]
    ys = [M.psum(0, 1024, "y0"), M.psum(1024, 1024, "y1")]

    def ld_ao(g):
        if g * 4 < ntile_own:
            P.dma("sp", AOs[g % 2], AO[:, g * 512:(g + 1) * 512].rearrange("(fc p) t -> p fc t", p=128))

    def ld_x(t):
        if t < ntile_own:
            P.dma("sp", xo[t % 4], x_in[t * 128:(t + 1) * 128, :])

    ld_ao(0)
    ld_x(0)
    ld_x(1)
    for t in range(ntile_own):
        g4 = t // 4
        j4 = t % 4
        a = AOs[g4 % 2]
        if j4 == 0:
            ld_ao(g4 + 1)
        ld_x(t + 2)
        x_t = xo[t % 4]
        y = ys[t % 2]
        for n in range(2):
            for fc in range(8):
                P.mm(y[:, n * 512:(n + 1) * 512], a[:, fc, j4 * 128:(j4 + 1) * 128],
                     Wout[:, fc, n * 512:(n + 1) * 512], start=(fc == 0), stop=(fc == 7))
        P.tt("dve", x_t, x_t, y, ALU.add)
        P.dma("sp", x_out[t * 128:(t + 1) * 128, :], x_t, accum_w=True)
    P.barrier()


def rope_tables_np(pos, dim):
    inv = (np.float32(10000.0) ** (-np.arange(0, dim, 2, dtype=np.float32) / np.float32(dim))).astype(np.float32)
    ang = pos.astype(np.float32)[:, None] * inv[None, :]
    ang = np.concatenate([ang, ang], axis=-1)
    return np.cos(ang).astype(np.float32), np.sin(ang).astype(np.float32)


def signed_sin(sin):
    h = sin.shape[-1] // 2
    return np.concatenate([-sin[..., :h], sin[..., h:]], axis=-1)


def make_rt(half):
    pos = (np.arange(S) + half * SH) % S
    cm, sm = rope_tables_np(pos, 32)
    row, col = pos // 64, pos % 64
    rc, rs = rope_tables_np(row, 32)
    cc, cs = rope_tables_np(col, 32)
    return np.concatenate([cm, signed_sin(sm), rc, cc, signed_sin(rs), signed_sin(cs)], axis=-1).astype(np.float32)


def a_layout(v):
    return np.ascontiguousarray(np.asarray(v, np.float32).reshape(-1, 128).T)


def declare_inputs(nc, P, specs):
    io = {}
    for name, (shape, dt) in specs.items():
        io[name] = dram(nc, P, name, shape, dt, kind="ExternalInput")
    return io


SBUF_WORDS = 48 * 1024


def build_l0_mixer(ntile_all=NT, ntile_own=NTO, debug=False):
    nc = bass.Bass("TRN2", target_bir_lowering=False)
    es = ExitStack()
    with es:
        P = Prog(nc, es)
        M = Mem(nc, es, P, SBUF_WORDS)
        specs = {
            "x": ([S, D], F32), "ident": ([128, 128], F32), "c_a": ([128, 8], F32),
            "ada_w": ([2, D, 6 * D], F32), "ada_b": ([2, 6 * D], F32), "ada_b_a": ([2, 128, 48], F32),
            "norm_mix_a": ([2, 128, 8], F32), "rt": ([S, 192], F32),
            "ab_w_in": ([D, AB_IN], F32), "ab_q_a_norm_a": ([128, 4], F32), "ab_w_qb": ([512, 768], F32),
            "ab_kv_a_norm_a": ([128, 2], F32), "ab_w_kvb": ([256, 1024], F32),
            "ab_mla_qn": ([1, 96], F32), "ab_mla_kn": ([1, 96], F32), "ab_gqa_qn": ([1, 64], F32),
            "ab_gqa_kn": ([1, 64], F32), "ab_w_out": ([D, D], F32),
        }
        io = declare_inputs(nc, P, specs)
        x_out = dram(nc, P, "x1", [SH, D], F32, kind="ExternalOutput")
        dbg = {} if debug else None
        emit_l0_mixer(P, M, io, io["x"], x_out, ntile_all, ntile_own, dbg)
        if debug:
            for k in ("QTm", "KTm", "Vm", "QTg", "KTg", "Vg"):
                src = dbg[k]
                o = dram(nc, P, "dbg_" + k, list(src.shape), BF16, kind="ExternalOutput")
                P.dma("sp", o, src, sem="dbgout")
        P.barrier()
        P.emit()
    return nc


def host_inputs_l0(inputs, core):
    b, half = core // 2, core % 2
    x = np.asarray(inputs["x"][b], np.float32)
    xr = np.concatenate([x[half * SH:(half + 1) * SH], x[(1 - half) * SH:(2 - half) * SH]], axis=0)
    return {
        "x": np.ascontiguousarray(xr),
        "ident": np.eye(128, dtype=np.float32),
        "c_a": a_layout(inputs["c"][b]),
        "ada_w": np.asarray(inputs["ada_w"], np.float32),
        "ada_b": np.asarray(inputs["ada_b"], np.float32),
        "ada_b_a": np.stack([a_layout(inputs["ada_b"][l]) for l in range(2)]),
        "norm_mix_a": np.stack([a_layout(inputs["norm_mix"][l]) for l in range(2)]),
        "rt": make_rt(half),
        "ab_w_in": np.asarray(inputs["ab_w_in"][0], np.float32),
        "ab_q_a_norm_a": a_layout(inputs["ab_q_a_norm"][0]),
        "ab_w_qb": np.asarray(inputs["ab_w_qb"][0], np.float32),
        "ab_kv_a_norm_a": a_layout(inputs["ab_kv_a_norm"][0]),
        "ab_w_kvb": np.asarray(inputs["ab_w_kvb"][0], np.float32),
        "ab_mla_qn": np.asarray(inputs["ab_mla_qn"], np.float32).reshape(1, 96),
        "ab_mla_kn": np.asarray(inputs["ab_mla_kn"], np.float32).reshape(1, 96),
        "ab_gqa_qn": np.asarray(inputs["ab_gqa_qn"], np.float32).reshape(1, 64),
        "ab_gqa_kn": np.asarray(inputs["ab_gqa_kn"], np.float32).reshape(1, 64),
        "ab_w_out": np.asarray(inputs["ab_w_out"][0], np.float32),
    }


CAPB = 8
CAP = CAPB * 128
NSLOT = 32 * CAP
TRASH = NSLOT


def _idma(P, out, in_, out_off=None, in_off=None, sem=None, accum_w=False, bounds=None):
    o, a = _ap(out), _ap(in_)
    oo = bass.IndirectOffsetOnAxis(ap=_ap(out_off), axis=0) if out_off is not None else None
    io_ = bass.IndirectOffsetOnAxis(ap=_ap(in_off), axis=0) if in_off is not None else None
    if sem is None:
        sb = out if (isinstance(out, T) and out.buf.sb) else in_
        sem = sb.buf.name
    P.op("pool", lambda e: e.indirect_dma_start(out=o, out_offset=oo, in_=a, in_offset=io_,
                                                bounds_check=bounds, oob_is_err=False),
         reads=_bufs(in_, out_off, in_off), writes=_bufs(out), dma=sem, accum_w=accum_w)


def emit_moe(P, M, io, l, x_in, x_out, ntile=NTO, dbg=None, wl=None):
    nc = P.nc
    wl = l if wl is None else wl
    M.reset()
    cst = emit_consts(P, M, io)
    idb = cst["idb"]
    modA, modB = emit_mod(P, M, io, l, need_a=[], need_b=[3, 4, 5])
    shift_b, scale_b, gate_b = modB[3], modB[4], modB[5]
    gs_b = scale_b
    nf = M.alloc([1024], F32, "nf_b")
    P.dma("sp", nf, io["norm_ffn"][l:l + 1, :].pbc())
    P.stt("dve", gs_b, scale_b, 1.0, nf, ALU.add, ALU.mult)
    wr = M.alloc([8, 36], F32, "wr")
    P.dma("sp", wr, io["moe_wr"][wl].rearrange("(kc p) n -> p kc n", p=128))
    whi = M.alloc([8, 36], BF16, "whi")
    wlo = M.alloc([8, 36], BF16, "wlo")
    P.copy("dve", whi, wr)
    P.tt("dve", wlo, wr, whi, ALU.subtract)
    rbias = M.alloc([36], F32, "rbias")
    P.dma("sp", rbias, io["moe_rb"][wl:wl + 1, :].pbc())
    trif = M.alloc([128], F32, "trif")
    P.dma("sp", trif, io["tri"])
    tri = M.alloc([128], BF16, "tri")
    P.copy("dve", tri, trif)
    ones = M.alloc([128], BF16, "ones")
    P.memset("dve", ones, 1.0)
    ebase = M.alloc([32], F32, "ebase")
    P.dma("sp", ebase, io["ebase"].pbc())
    tokid = M.alloc([ntile + 8], I32, "tokid")
    P.dma("sp", tokid[:, 0:ntile], io["tokid"][:, 0:ntile])
    LG = M.alloc([ntile, 36], F32, "LG")
    gates = M.alloc([ntile, 2], F32, "gates")
    slots_i = M.alloc([ntile + 4, 2], I32, "slots_i")
    keep_off = M.off

    H2 = dram(nc, P, "H2_%d" % l, [SH, D], BF16)
    SLOT_T = dram(nc, P, "SLOT_T%d" % l, [NSLOT + 128, 1], I32)
    Y = dram(nc, P, "Y_%d" % l, [NSLOT + 128, D], F32)

    zt = M.alloc([1024], F32, "zt")
    P.memset("dve", zt, 0.0)
    P.dma("sp", SLOT_T.rearrange("(p j) o -> p (j o)", p=128), zt[:, 0:(NSLOT + 128) // 128].bitcast(I32))
    P.dma("sp", Y[NSLOT:NSLOT + 128, :], zt)

    xt = [M.alloc([1024], F32, "mxt%d" % i) for i in range(4)]
    junk2 = [M.alloc([1024], BF16, "mjunk%d" % i) for i in range(2)]
    st2 = [M.alloc([8], F32, "mst%d" % i) for i in range(2)]
    h22 = [M.alloc([1024], F32, "h2_%d" % i) for i in range(2)]
    hi = [M.alloc([1024], BF16, "hi%d" % i) for i in range(2)]
    lo2 = [M.alloc([1024], BF16, "lo%d" % i) for i in range(2)]
    hT2 = [M.alloc([1024], BF16, "hT%d" % i) for i in range(2)]
    lT2 = [M.alloc([1024], BF16, "lT%d" % i) for i in range(2)]
    psT = [M.psum(0, 512, "mpsT0", BF16), M.psum(512, 512, "mpsT1", BF16)]
    psL2 = [M.psum(1024, 36, "psL0"), M.psum(1536, 36, "psL1")]

    def ld_mx(t):
        if t < ntile:
            P.dma("sp", xt[t % 4], x_in[t * 128:(t + 1) * 128, :])

    ld_mx(0)
    ld_mx(1)
    for t in range(ntile):
        ld_mx(t + 2)
        x_t = xt[t % 4]
        junk, st, h2, lo, hT, lT, psL = junk2[t % 2], st2[t % 2], h22[t % 2], lo2[t % 2], hT2[t % 2], lT2[t % 2], psL2[t % 2]
        P.act(junk, x_t, AF.Square, accum=st[:, 0:1])
        emit_rstd(P, st[:, 0:1], st[:, 1:2], 1024)
        P.stt("dve", h2, x_t, st[:, 1:2], gs_b, ALU.mult, ALU.mult)
        P.tt("pool", h2, h2, shift_b, ALU.add)
        hi_t = hi[t % 2]
        P.copy("act", hi_t, h2)
        P.tt("dve", lo, h2, hi_t, ALU.subtract)
        P.dma("sp", H2[t * 128:(t + 1) * 128, :], hi_t, accum_w=True)
        for kc in range(8):
            P.tr(psT[0][:, kc * 128:(kc + 1) * 128], hi_t[:, kc * 128:(kc + 1) * 128], idb)
        P.copy("act", hT, psT[0])
        for kc in range(8):
            P.tr(psT[1][:, kc * 128:(kc + 1) * 128], lo[:, kc * 128:(kc + 1) * 128], idb)
        P.copy("act", lT, psT[1])
        n = 0
        for a, w in ((hT, whi), (hT, wlo), (lT, whi)):
            for kc in range(8):
                P.mm(psL, a[:, kc * 128:(kc + 1) * 128], w[:, kc, :], start=(n == 0), stop=(n == 23))
                n += 1
        P.tt("dve", LG[:, t, :], psL, rbias, ALU.add)
    nt = ntile
    w4 = M.alloc([nt, 4], F32, "w4")
    ohg = M.alloc([nt, 4], F32, "ohg")
    c1 = M.alloc([nt], F32, "c1")
    c2 = M.alloc([nt], F32, "c2")
    c3 = M.alloc([nt], F32, "c3")
    pgt = M.alloc([nt], F32, "pgt")
    el = M.alloc([nt, 8], F32, "el")
    el2 = M.alloc([nt, 8], F32, "el2")
    w8 = M.alloc([nt, 8], F32, "w8")
    oh1 = M.alloc([nt, 8], F32, "oh1")
    oh2 = M.alloc([nt, 8], F32, "oh2")
    A1 = M.alloc([nt, 4, 8], F32, "A1")
    A2 = M.alloc([nt, 4, 8], F32, "A2")
    Ab = M.alloc([nt, 32], BF16, "Ab")
    Acum = M.alloc([nt + 1, 32], BF16, "Acum")
    pos = M.alloc([nt, 32], F32, "pos")
    w32 = M.alloc([nt, 32], F32, "w32")
    lg4 = LG[:, :, 0:4]
    P.reduce("dve", c1, lg4, op=ALU.max)
    P.tt("dve", ohg, lg4, c1.unsq(2).bcast([128, nt, 4]), ALU.is_equal)
    P.tt("dve", w4, lg4, c1.unsq(2).bcast([128, nt, 4]), ALU.subtract)
    P.act(w4, w4, AF.Exp)
    P.reduce("dve", c2, w4)
    P.recip(pgt, c2)
    for g in range(4):
        src = LG[:, :, 4 + 8 * g:12 + 8 * g]
        og = ohg[:, :, g:g + 1].bcast([128, nt, 8])
        if g == 0:
            P.tt("dve", el, src, og, ALU.mult)
        else:
            P.tt("dve", w8, src, og, ALU.mult)
            P.tt("dve", el, el, w8, ALU.add)
    P.reduce("dve", c1, el, op=ALU.max)
    P.tt("dve", oh1, el, c1.unsq(2).bcast([128, nt, 8]), ALU.is_equal)
    P.stt("dve", el2, oh1, -1e30, el, ALU.mult, ALU.add)
    P.reduce("dve", c2, el2, op=ALU.max)
    P.tt("dve", oh2, el2, c2.unsq(2).bcast([128, nt, 8]), ALU.is_equal)
    P.tt("dve", c3, c2, c1, ALU.subtract)
    P.act(c3, c3, AF.Exp)
    P.ts("dve", c3, c3, 1.0, None, ALU.add)
    P.recip(c3, c3)
    P.tt("dve", gates[:, :, 0], pgt, c3, ALU.mult)
    P.tt("dve", gates[:, :, 1], pgt, gates[:, :, 0], ALU.subtract)
    P.tt("dve", A1, ohg.unsq(3).bcast([128, nt, 4, 8]), oh1.unsq(2).bcast([128, nt, 4, 8]), ALU.mult)
    P.tt("dve", A2, ohg.unsq(3).bcast([128, nt, 4, 8]), oh2.unsq(2).bcast([128, nt, 4, 8]), ALU.mult)
    A1f = A1.rearrange("p t g e -> p t (g e)")
    A2f = A2.rearrange("p t g e -> p t (g e)")
    P.tt("dve", Ab, A1f, A2f, ALU.add)
    P.memset("dve", Acum[:, 0, :], 0.0)
    for t in range(nt):
        P.tt("dve", Acum[:, t + 1, :], Acum[:, t, :], Ab[:, t, :], ALU.add)
    psP = M.psum(2048, nt * 32, "psP")
    for t in range(nt):
        P.mm(psP[:, t * 32:(t + 1) * 32], tri, Ab[:, t, :], start=True, stop=False)
        P.mm(psP[:, t * 32:(t + 1) * 32], ones, Acum[:, t, :], start=False, stop=True)
    P.copy("dve", pos.rearrange("p t e -> p (t e)"), psP)
    for k, Ak in enumerate((A1f, A2f)):
        P.tt("dve", w32, Ak, pos, ALU.mult)
        P.reduce("dve", c1, w32)
        P.tt("dve", w32, Ak, ebase.unsq(1).bcast([128, nt, 32]), ALU.mult)
        P.reduce("dve", c2, w32)
        P.ts("dve", c3, c1, CAP - 0.5, None, ALU.is_lt)
        P.tt("dve", c2, c2, c1, ALU.add)
        P.ts("dve", c2, c2, float(TRASH), None, ALU.subtract)
        P.tt("dve", c2, c2, c3, ALU.mult)
        P.ts("dve", c2, c2, float(TRASH), None, ALU.add)
        P.tt("dve", gates[:, :, k], gates[:, :, k], c3, ALU.mult)
        P.copy("dve", slots_i[:, 0:nt, k], c2)
    istg = [M.alloc([8], I32, "istg%d" % i) for i in range(4)]
    for t in range(nt):
        for k in range(2):
            ist = istg[(2 * t + k) % 4]
            P.copy("pool", ist[:, 0:1], slots_i[:, t, k:k + 1])
            _idma(P, SLOT_T, tokid[:, t:t + 1], out_off=ist[:, 0:1], sem=ist.buf.name, accum_w=True)
    if dbg is not None:
        dbg["LG"], dbg["gates"], dbg["slots_i"], dbg["pos"] = LG, gates, slots_i[:, 0:nt, :], pos
    P.barrier()
    M.off = keep_off

    NH = CAP // 512
    Wup = [M.alloc([8, 1024], BF16, "Wup%d" % i) for i in range(2)]
    Wdn = [M.alloc([4, 1024], BF16, "Wdn%d" % i) for i in range(2)]
    idx = [M.alloc([CAPB], I32, "idx%d" % i) for i in range(2)]
    Xg = [M.alloc([CAPB, 1024], BF16, "Xg%d" % i) for i in range(2)]
    XT = M.alloc([8, CAP], BF16, "XT")
    actT = M.alloc([4, CAP], BF16, "actT")
    sg = [M.alloc([512], F32, "sg%d" % i) for i in range(2)]
    Ysb = [M.alloc([1024], F32, "Ysb%d" % i) for i in range(2)]
    psXh = [M.psum(0, 256, "psXa", BF16), M.psum(3584, 256, "psXb", BF16)]
    psU = [(M.psum(512, 512, "psG0"), M.psum(1024, 512, "psU0")),
           (M.psum(1536, 512, "psG1"), M.psum(2048, 512, "psU1"))]
    psYh = [M.psum(2560, 512, "psY0"), M.psum(3072, 512, "psY1")]
    Yv = Y[0:NSLOT, :].rearrange("(e p j) d -> e p j d", e=32, p=128)
    SLv = SLOT_T[0:NSLOT, :].rearrange("(e p j) o -> e p (j o)", e=32, p=128)

    def load_expert(e):
        s = e % 2
        P.dma("pool", Wup[s], io["moe_w_up"][wl, e].rearrange("(kc p) n -> p kc n", p=128))
        P.dma("pool", Wdn[s], io["moe_w_down"][wl, e].rearrange("(kc p) n -> p kc n", p=128))
        P.dma("sp", idx[s], SLv[e])
        for j in range(CAPB):
            _idma(P, Xg[s][:, j, :], H2, in_off=idx[s][:, j:j + 1], sem="Xg%d" % s, accum_w=True)

    load_expert(0)
    load_expert(1)
    cnt = 0
    for e in range(32):
        s = e % 2
        for j in range(CAPB):
            for hx in range(2):
                for k4 in range(4):
                    kc = hx * 4 + k4
                    P.tr(psXh[hx][:, k4 * 128:(k4 + 1) * 128], Xg[s][:, j, kc * 128:(kc + 1) * 128], idb)
                P.copy("act" if hx == 0 else "dve", XT[:, hx * 4:(hx + 1) * 4, j * 128:(j + 1) * 128],
                       psXh[hx].rearrange("p (k t) -> p k t", k=4))
        for hf in range(NH):
            cs = slice(hf * 512, (hf + 1) * 512)
            for hc in range(4):
                pg_, pu_ = psU[cnt % 2]
                for kc in range(8):
                    P.mm(pg_, Wup[s][:, kc, hc * 128:(hc + 1) * 128], XT[:, kc, cs], start=(kc == 0), stop=(kc == 7))
                for kc in range(8):
                    P.mm(pu_, Wup[s][:, kc, 512 + hc * 128:512 + (hc + 1) * 128], XT[:, kc, cs],
                         start=(kc == 0), stop=(kc == 7))
                P.act(sg[cnt % 2], pg_, AF.Silu)
                P.tt("dve", actT[:, hc, cs], sg[cnt % 2], pu_, ALU.mult)
                cnt += 1
        for j in range(CAPB):
            ysb = Ysb[j % 2]
            for n in range(2):
                for hc in range(4):
                    P.mm(psYh[n], actT[:, hc, j * 128:(j + 1) * 128],
                         Wdn[s][:, hc, n * 512:(n + 1) * 512], start=(hc == 0), stop=(hc == 3))
                P.copy("act" if n == 0 else "dve", ysb[:, n * 512:(n + 1) * 512], psYh[n])
            P.dma("sp", Yv[e][:, j, :], ysb, accum_w=True)
        if e + 2 < 32:
            load_expert(e + 2)
    P.barrier()
    M.off = keep_off

    xo = [M.alloc([1024], F32, "cxo%d" % i) for i in range(3)]
    y1 = [M.alloc([1024], F32, "cy1%d" % i) for i in range(3)]
    y2 = [M.alloc([1024], F32, "cy2%d" % i) for i in range(3)]
    cidx = [M.alloc([8], I32, "cidx%d" % i) for i in range(3)]

    def ld_c(t):
        if t < ntile:
            s_ = t % 3
            P.dma("sp", xo[s_], x_in[t * 128:(t + 1) * 128, :])
            P.copy("pool", cidx[s_][:, 0:1], slots_i[:, t, 0:1])
            P.copy("pool", cidx[s_][:, 1:2], slots_i[:, t, 1:2])
            _idma(P, y1[s_], Y, in_off=cidx[s_][:, 0:1])
            _idma(P, y2[s_], Y, in_off=cidx[s_][:, 1:2])

    ld_c(0)
    ld_c(1)
    for t in range(ntile):
        s = t % 3
        P.ts("dve", y1[s], y1[s], gates[:, t, 0:1], None, ALU.mult)
        P.stt("dve", y1[s], y2[s], gates[:, t, 1:2], y1[s], ALU.mult, ALU.add)
        P.tt("dve", y1[s], y1[s], gate_b, ALU.mult)
        P.tt("dve", xo[s], xo[s], y1[s], ALU.add)
        P.dma("sp", x_out[t * 128:(t + 1) * 128, :], xo[s], accum_w=True)
        ld_c(t + 2)
    P.barrier()


MOE_SPECS = {
    "x": ([SH, D], F32), "ident": ([128, 128], F32), "c_a": ([128, 8], F32),
    "ada_w": ([2, D, 6 * D], F32), "ada_b": ([2, 6 * D], F32), "ada_b_a": ([2, 128, 48], F32),
    "norm_ffn": ([2, D], F32), "moe_wr": ([1, D, 36], F32), "moe_rb": ([1, 36], F32),
    "tri": ([128, 128], F32), "ebase": ([1, 32], F32), "tokid": ([128, NTO], I32),
    "moe_w_up": ([1, 32, D, D], F32), "moe_w_down": ([1, 32, 512, D], F32),
}


def build_moe(l, ntile=NTO, debug=False):
    nc = bass.Bass("TRN2", target_bir_lowering=False)
    es = ExitStack()
    with es:
        P = Prog(nc, es)
        M = Mem(nc, es, P, SBUF_WORDS)
        io = declare_inputs(nc, P, MOE_SPECS)
        x_out = dram(nc, P, "x2", [SH, D], F32, kind="ExternalOutput")
        dbg = {} if debug else None
        emit_moe(P, M, io, l, io["x"], x_out, ntile, dbg, wl=0)
        if debug:
            for k, dt in (("LG", F32), ("gates", F32), ("slots_i", I32), ("pos", F32)):
                src = dbg[k]
                shp = list(src.shape)
                o = dram(nc, P, "dbg_" + k, shp, dt, kind="ExternalOutput")
                P.dma("sp", o, src, sem="dbgout")
        P.barrier()
        P.emit()
    return nc


def host_inputs_moe(inputs, core, x_own, l):
    b = core // 2
    wr = np.concatenate([inputs["moe_w_group"][l],
                         np.transpose(inputs["moe_w_expert"][l], (1, 0, 2)).reshape(D, 32)], axis=1)[None].astype(np.float32)
    rb = np.concatenate([inputs["moe_b_group"][l], inputs["moe_b_expert"][l].reshape(32)])[None].astype(np.float32)
    return {
        "x": np.ascontiguousarray(x_own, dtype=np.float32),
        "ident": np.eye(128, dtype=np.float32),
        "c_a": a_layout(inputs["c"][b]),
        "ada_w": np.asarray(inputs["ada_w"], np.float32),
        "ada_b": np.asarray(inputs["ada_b"], np.float32),
        "ada_b_a": np.stack([a_layout(inputs["ada_b"][k]) for k in range(2)]),
        "norm_ffn": np.asarray(inputs["norm_ffn"], np.float32),
        "moe_wr": wr, "moe_rb": rb,
        "tri": np.triu(np.ones((128, 128), np.float32), 1),
        "ebase": (np.arange(32, dtype=np.float32) * CAP).reshape(1, 32),
        "tokid": (np.arange(NTO)[None, :] * 128 + np.arange(128)[:, None]).astype(np.int32),
        "moe_w_up": np.asarray(inputs["moe_w_up"][l:l + 1], np.float32),
        "moe_w_down": np.asarray(inputs["moe_w_down"][l:l + 1], np.float32),
    }


C_IN = 3072
LAMBDA_INIT1 = 0.8 - 0.6 * math.exp(-0.3 * 1)
GW = 1280
HKW = 1152


def t5_bucket_np(rel):
    rel = np.asarray(rel, np.int64)
    nb = 16
    max_exact = 8
    ret = np.where(rel > 0, nb, 0)
    n = np.abs(rel)
    nf = np.maximum(n, 1).astype(np.float32)
    large = max_exact + (np.log(nf / np.float32(max_exact)) / np.float32(math.log(128 / max_exact))
                         * np.float32(nb - max_exact)).astype(np.int32)
    large = np.minimum(large, nb - 1)
    return ret + np.where(n < max_exact, n, large)


def make_bias_onehots(half):
    m = np.arange(GW)
    r_own = 639 - m
    d32 = 512 - half * 8192
    d63 = 8064 - half * 8192
    mm_ = np.arange(640)
    r32 = 127 - mm_ + d32
    r63 = 127 - mm_ + d63
    rel = np.concatenate([r_own, r32, r63])
    bk = t5_bucket_np(rel)
    oh = (bk[None, :] == np.arange(32)[:, None]).astype(np.float32)
    sel = np.zeros((32, 3), np.float32)
    sel[15, 0] = 1.0
    sel[31, 1] = 1.0
    sel[31 if half == 0 else 15, 2] = 1.0
    return oh, sel


def emit_l1_mixer(P, M, io, x_in, x_out, ntile_all=NT, ntile_own=NTO, dbg=None, tile_hook=None):
    nc = P.nc
    xin = x_in if callable(x_in) else (lambda t: x_in[t * 128:(t + 1) * 128, :])
    M.reset()
    cst = emit_consts(P, M, io)
    idb = cst["idb"]
    modA, modB = emit_mod(P, M, io, 1, need_a=[0, 1], need_b=[2])
    shift_a, scale_a, gate_b = modA[0], modA[1], modB[2]
    nm_a = M.alloc([8], F32, "nm_a")
    P.dma("sp", nm_a, io["norm_mix_a"][1])
    gs_a = M.alloc([8], F32, "gs_a")
    P.stt("dve", gs_a, scale_a, 1.0, nm_a, ALU.add, ALU.mult)
    g_q = M.alloc([2, 64], F32, "g_q1")
    g_k = M.alloc([2, 64], F32, "g_k1")
    neglam = M.alloc([1], F32, "neglam")
    sublnc = M.alloc([1], F32, "sublnc")
    bcol = M.alloc([3, 8], F32, "bcol")
    Hk = M.alloc([8, HKW], BF16, "Hk")
    Hx = [M.alloc([8, 512], BF16, "Hx%d" % i) for i in range(2)]
    Jb = M.alloc([128], BF16, "Jb")
    ones = M.alloc([128], BF16, "ones1")
    keep_attn = M.off
    Wp = M.alloc([8, C_IN], BF16, "Wp1")
    bias_bc = M.alloc([C_IN], F32, "bias_bc1")
    keep_off = M.off
    P.memset("dve", ones, 1.0)

    jf = M.alloc([128], F32, "jf")
    P.dma("sp", jf, io["jmat"])
    P.copy("dve", Jb, jf)
    P.dma("sp", g_q.rearrange("p c d -> p (c d)"), io["c_qn"].pbc())
    P.ts("dve", g_q, g_q, 0.125, None, ALU.mult)
    P.dma("sp", g_k.rearrange("p c d -> p (c d)"), io["c_kn"].pbc())
    lamv = M.alloc([4, 64], F32, "lamv")
    P.dma("sp", lamv.rearrange("p a d -> p (a d)"), io["c_lam"].pbc())
    lw = M.alloc([2, 64], F32, "lw")
    P.tt("dve", lw[:, 0, :], lamv[:, 0, :], lamv[:, 1, :], ALU.mult)
    P.tt("dve", lw[:, 1, :], lamv[:, 2, :], lamv[:, 3, :], ALU.mult)
    ls = M.alloc([2], F32, "ls")
    P.reduce("dve", ls, lw)
    P.act(ls, ls, AF.Exp)
    P.tt("dve", neglam, ls[:, 1:2], ls[:, 0:1], ALU.subtract)
    P.ts("dve", neglam, neglam, -LAMBDA_INIT1, None, ALU.add)
    P.dma("sp", sublnc, io["c_subln_a"])
    P.ts("dve", sublnc, sublnc, 1.0 - LAMBDA_INIT1, None, ALU.mult)
    rb = M.alloc([8], F32, "rb", parts=32)
    P.dma("sp", rb, io["rel_bias"])
    oh = M.alloc([GW + 1280], F32, "oh", parts=32)
    P.dma("sp", oh, io["bias_oh"])
    sel = M.alloc([3], F32, "sel", parts=32)
    P.dma("sp", sel, io["bias_sel"])
    selrep = M.alloc([3, 128], F32, "selrep", parts=32)
    P.copy("dve", selrep, sel.unsq(2).bcast([32, 3, 128]))
    psc = M.psum(0, 24, "psc")
    for i in range(3):
        P.mm(psc[:, i * 8:(i + 1) * 8], selrep[:, i, :], rb)
    P.copy("dve", bcol.rearrange("p a h -> p (a h)"), psc)
    GT = GW + 1280
    Gd = dram(nc, P, "Gd", [8, GT], F32)
    gsb = M.alloc([GT], F32, "gsb", parts=8)
    for c0 in range(0, GT, 512):
        c1 = min(GT, c0 + 512)
        psg = M.psum(512 + (c0 // 512) * 512, c1 - c0, "psg%d" % c0)
        P.mm(psg[0:8, :], rb, oh[:, c0:c1])
        P.copy("dve", gsb[:, c0:c1], psg[0:8, :])
    P.dma("sp", Gd, gsb)
    hkf = M.alloc([HKW], F32, "hkf")
    for h in range(8):
        src = T(bass.AP(Gd.ap.tensor, h * GT, [[1, 128], [1, HKW]]), Gd.buf)
        P.dma("sp", hkf, src)
        P.copy("dve", Hk[:, h, :], hkf)
        for xi in range(2):
            src = T(bass.AP(Gd.ap.tensor, h * GT + GW + xi * 640, [[1, 128], [1, 512]]), Gd.buf)
            P.dma("sp", hkf[:, 0:512], src)
            P.copy("dve", Hx[xi][:, h, :], hkf[:, 0:512])
    P.barrier()
    M.off = keep_off
    shrep = M.alloc([8, 128], BF16, "shift_rep1")
    P.copy("dve", shrep, shift_a.unsq(2).bcast([128, 8, 128]))
    Wo = M.alloc([8, C_IN], BF16, "Wo1")
    stg = [M.alloc([C_IN], F32, "w1stg%d" % i) for i in range(2)]
    for kc in range(8):
        s = stg[kc % 2]
        P.dma("sp", s, io["c_w_in"][kc * 128:(kc + 1) * 128, :])
        P.act(Wp[:, kc, :], s, AF.Copy, scale=gs_a[:, kc:kc + 1])
        P.copy("pool", Wo[:, kc, :], s)
    for n0 in range(0, C_IN, 512):
        ps = M.psum((n0 // 512) * 512, 512, "ps1bias%d" % n0)
        for kc in range(8):
            P.mm(ps, shrep[:, kc, :], Wo[:, kc, n0:n0 + 512], start=(kc == 0), stop=(kc == 7))
        P.copy("dve", bias_bc[:, n0:n0 + 512], ps)
    P.barrier()
    M.off = keep_off

    QT1 = dram(nc, P, "QT1", [8, 128, SH], BF16)
    KT1 = dram(nc, P, "KT1", [8, 128, S], BF16)
    V1 = dram(nc, P, "V1", [8, 128, NT, 128], BF16)
    AO = dram(nc, P, "AO1", [1024, SH], BF16)

    xt = [M.alloc([1024], F32, "x1t%d" % i) for i in range(2)]
    junk = M.alloc([1024], BF16, "junk1")
    xb = M.alloc([1024], BF16, "xb1")
    xT = M.alloc([1024], BF16, "xT1")
    st = M.alloc([8], F32, "stats1")
    qk = M.alloc([16, 64], F32, "qk1")
    sq = M.alloc([16, 64], F32, "sq1")
    ssh = M.alloc([16], F32, "ssh1")
    rsh = M.alloc([16], F32, "rsh1")
    qkf = M.alloc([16, 64], BF16, "qkf1")
    QTs = [M.alloc([8, 512], BF16, "Q1s%d" % i) for i in range(2)]
    KTs = [M.alloc([8, 512], BF16, "K1s%d" % i) for i in range(2)]
    Vs = [M.alloc([8, 8, 128], BF16, "V1s%d" % i) for i in range(2)]
    psT = M.psum(0, 512, "ps1T", BF16)
    psP = [M.psum(512 * (1 + i), 512, "ps1P%d" % i) for i in range(6)]
    psO = M.psum(3584, 512, "ps1O", BF16)
    for t in range(ntile_all):
        own = t < ntile_own
        if tile_hook is not None:
            tile_hook(t)
        x_t = xt[t % 2]
        if t == 0:
            P.dma("sp", xt[0], xin(0))
        if t + 1 < ntile_all:
            P.dma("sp", xt[(t + 1) % 2], xin(t + 1))
        P.act(junk, x_t, AF.Square, accum=st[:, 0:1])
        emit_rstd(P, st[:, 0:1], st[:, 1:2], 1024)
        rstd = st[:, 1:2]
        P.copy("pool", xb, x_t)
        for kc in range(8):
            P.tr(psT[:, kc * 128:(kc + 1) * 128], xb[:, kc * 128:(kc + 1) * 128], idb)
        P.copy("act", xT, psT)
        cols = ([0, 512] if own else []) + [1024, 1536, 2048, 2560]
        for i, c0 in enumerate(cols):
            for kc in range(8):
                P.mm(psP[i], xT[:, kc * 128:(kc + 1) * 128], Wp[:, kc, c0:c0 + 512], start=(kc == 0), stop=(kc == 7))
        slot4, j4 = (t // 4) % 2, t % 4
        slot8, j8 = (t // 8) % 2, t % 8
        bi = 0
        for which in (["q"] if own else []) + ["k"]:
            c0 = 0 if which == "q" else 1024
            gain = g_q if which == "q" else g_k
            qflat = qk.rearrange("p h d -> p (h d)")
            for hf in range(2):
                P.stt("dve", qflat[:, hf * 512:(hf + 1) * 512], psP[bi], rstd,
                      bias_bc[:, c0 + hf * 512:c0 + (hf + 1) * 512], ALU.mult, ALU.add)
                bi += 1
            P.tt("pool", sq, qk, qk, ALU.mult)
            P.reduce("dve", ssh, sq)
            emit_rstd(P, ssh, rsh, 64)
            P.tt("dve", qk, qk, rsh.unsq(2).bcast([128, 16, 64]), ALU.mult)
            P.tt("pool", qkf.rearrange("p (h c) d -> p h c d", c=2), qk.rearrange("p (h c) d -> p h c d", c=2),
                 gain.unsq(1).bcast([128, 8, 2, 64]), ALU.mult)
            for h in range(8):
                P.tr(psO[:, h * 128:(h + 1) * 128], qkf[:, 2 * h:2 * h + 2, :].rearrange("p c d -> p (c d)"), idb)
            dst = (QTs if which == "q" else KTs)[slot4]
            P.copy("act", dst[:, :, j4 * 128:(j4 + 1) * 128], psO.rearrange("p (h t) -> p h t", h=8))
        vdst = Vs[slot8][:, j8, :, :].rearrange("p h d -> p (h d)")
        for hf in range(2):
            P.stt("dve", vdst[:, hf * 512:(hf + 1) * 512], psP[bi], rstd,
                  bias_bc[:, 2048 + hf * 512:2048 + (hf + 1) * 512], ALU.mult, ALU.add)
            bi += 1
        if j4 == 3:
            c0 = (t // 4) * 512
            if own:
                P.dma("sp", QT1[:, :, c0:c0 + 512].rearrange("h p t -> p h t"), QTs[slot4], accum_w=True)
            P.dma("sp", KT1[:, :, c0:c0 + 512].rearrange("h p t -> p h t"), KTs[slot4], accum_w=True)
        if j8 == 7 or t == ntile_all - 1:
            g8 = t // 8
            n8 = j8 + 1
            for hh in range(8):
                P.dma("sp", V1[hh][:, g8 * 8:g8 * 8 + n8, :], Vs[slot8][:, 0:n8, hh, :], accum_w=True)
    if dbg is not None:
        dbg["QT1"], dbg["KT1"], dbg["V1"] = QT1, KT1, V1
    P.barrier()
    M.off = keep_attn
    off_proj = keep_attn

    nkt = ntile_all
    nqt = ntile_own // 4
    nko = ntile_own
    KT = [M.alloc([nkt * 128], BF16, "K1T%d" % i) for i in range(2)]
    Vh = [M.alloc([nkt, 128], BF16, "V1h%d" % i) for i in range(2)]
    QT = [M.alloc([nqt * 512], BF16, "Q1T%d" % i) for i in range(2)]
    OT = [M.alloc([nqt * 512], BF16, "O1T%d" % i) for i in range(2)]
    QTz = [[M.alloc([nqt * 512], BF16, "Q1z%d_%d" % (i, c)) for c in range(2)] for i in range(2)]
    for i in range(2):
        for c in range(2):
            P.memset("pool", QTz[i][c], 0.0)
    GSZ = 2
    Pb = [M.alloc([GSZ * 512], BF16, "P1b%d" % i) for i in range(2)]
    Psum = [M.alloc([512], BF16, "P1sum%d" % i) for i in range(2)]
    rl = [M.alloc([512], F32, "rl1_%d" % i) for i in range(2)]
    o0s = M.alloc([512], F32, "o0s")
    o1s = M.alloc([512], F32, "o1s")
    sqb = M.alloc([512], BF16, "sqb")
    rsd = M.alloc([512], F32, "rsd")
    Sg = [M.psum(0, GSZ * 512, "S1g0"), M.psum(1024, GSZ * 512, "S1g1")]
    Oa = [M.psum(2048, 512, "O1a0"), M.psum(3072, 512, "O1a1")]
    La = [M.psum(2560, 512, "L1a0"), M.psum(3584, 512, "L1a1")]

    def seglist(qt):
        segs = []
        lo, hi = 4 * qt - 1, 4 * qt + 4
        left = [k for k in range(0, max(lo, 0))]
        band = [k for k in range(max(lo, 0), min(hi, nko - 1) + 1)]
        right = [k for k in range(min(hi, nko - 1) + 1, nko)]
        other = list(range(nko, nkt))
        cross = []
        if nkt > nko:
            if qt == nqt - 1 and nko in other:
                other.remove(nko)
                cross.append((nko, 0))
            if qt == 0 and (nkt - 1) in other:
                other.remove(nkt - 1)
                cross.append((nkt - 1, 1))
        if left:
            segs.append(("c", 0, left))
        if band:
            segs.append(("b", None, [(k, "own", 512 - (k * 128 - qt * 512)) for k in band]))
        if cross:
            segs.append(("b", None, [(k, xi, 0) for k, xi in cross]))
        if right:
            segs.append(("c", 1, right))
        if other:
            segs.append(("c", 2, other))
        return segs

    tasks = []
    for h in range(8):
        for qt in range(nqt):
            for c in range(2):
                gl = []
                for kind, arg, lst in seglist(qt):
                    for i in range(0, len(lst), GSZ):
                        gl.append((kind, arg, lst[i:i + GSZ]))
                for gi, (kind, arg, lst) in enumerate(gl):
                    tasks.append((h, qt, c, gi, len(gl), kind, arg, lst))

    def load_head(h):
        s = h % 2
        P.dma("sp", KT[s], KT1[h][:, 0:nkt * 128])
        P.dma("sp", Vh[s], V1[h][:, 0:nkt])
        P.dma("sp", QT[s], QT1[h][:, 0:nqt * 512])
        for c in range(2):
            P.copy("pool", QTz[s][c][c * 64:(c + 1) * 64, :], QT[s][c * 64:(c + 1) * 64, :])

    def qk_mm(i):
        h, qt, c, gi, ng, kind, arg, lst = tasks[i]
        s = h % 2
        pr = slice(c * 64, (c + 1) * 64)
        for j, item in enumerate(lst):
            kt = item if kind == "c" else item[0]
            P.mm(Sg[i % 2][:, j * 512:(j + 1) * 512], KT[s][:, kt * 128:(kt + 1) * 128],
                 QTz[s][c][:, qt * 512:(qt + 1) * 512], start=True, stop=(kind == "c"))
            if kind == "b":
                _, tab, off = item
                src = Hk[:, h, off:off + 512] if tab == "own" else Hx[tab][:, h, :]
                P.mm(Sg[i % 2][:, j * 512:(j + 1) * 512], Jb, src, start=False, stop=True)

    pend = {}
    deferred = []

    def ex_pv(i):
        h, qt, c, gi, ng, kind, arg, lst = tasks[i]
        s = h % 2
        n = len(lst)
        pb = Pb[i % 2]
        if kind == "c":
            P.act(pb[:, 0:n * 512], Sg[i % 2][:, 0:n * 512], AF.Exp, bias=bcol[:, arg, h:h + 1])
        else:
            P.act(pb[:, 0:n * 512], Sg[i % 2][:, 0:n * 512], AF.Exp)
        if n == 1:
            lsrc = pb[:, 0:512]
        else:
            lsrc = Psum[i % 2]
            P.tt("dve", lsrc, pb[:, 0:512], pb[:, 512:1024], ALU.add)
        oa, la = Oa[c], La[c]
        for j, item in enumerate(lst):
            kt = item if kind == "c" else item[0]
            pj = pb[:, j * 512:(j + 1) * 512]
            P.mm(oa, Vh[s][:, kt, :], pj, start=(gi == 0 and j == 0), stop=(gi == ng - 1 and j == n - 1))
        if gi > 0:
            P.mm(la, ones, pend["lsrc"], start=(gi == 1), stop=False)
        pend["lsrc"] = lsrc
        if gi == ng - 1:
            P.mm(la, ones, lsrc, start=(gi == 0), stop=True)

            def norm(c=c, oa=oa, la=la):
                P.act(rl[c], la, AF.Ln)
                P.act(rl[c], rl[c], AF.Exp, scale=-1.0)
                if c == 0:
                    P.tt("dve", o0s, oa, rl[0], ALU.mult)
                else:
                    P.tt("dve", o1s, oa, rl[1], ALU.mult)
                    P.stt("dve", o1s, o1s, neglam[:, 0:1], o0s, ALU.mult, ALU.add)
                    P.tt("pool", sqb, o1s, o1s, ALU.mult)
            deferred.append((i + 3, norm))
            if c == 1:
                def fin(h=h, qt=qt, s=s, la=la):
                    P.mm(la, ones, sqb)
                    P.act(rsd, la, AF.Ln, scale=1.0 / 128.0, bias=EPS)
                    P.act(rsd, rsd, AF.Exp, scale=-0.5)
                    P.tt("dve", o1s, o1s, rsd, ALU.mult)
                    P.ts("dve", OT[s][:, qt * 512:(qt + 1) * 512], o1s, sublnc[:, 0:1], None, ALU.mult)
                    if qt == nqt - 1:
                        P.dma("sp", AO[h * 128:(h + 1) * 128, 0:nqt * 512], OT[s])
                        if h + 2 < 8:
                            load_head(h + 2)
                deferred.append((i + 11, fin))

    def run_deferred(i, flush=False):
        while deferred and (flush or deferred[0][0] <= i):
            deferred.pop(0)[1]()

    load_head(0)
    load_head(1)
    for i in range(len(tasks) + 1):
        if i < len(tasks):
            qk_mm(i)
        if i >= 1:
            ex_pv(i - 1)
            run_deferred(i - 1)
    run_deferred(0, flush=True)
    P.barrier()
    M.off = off_proj

    Wout = M.alloc([8, 1024], BF16, "Wout1")
    wst = [M.alloc([1024], F32, "wo1st%d" % i) for i in range(2)]
    for kc in range(8):
        P.dma("sp", wst[kc % 2], io["c_w_out"][kc * 128:(kc + 1) * 128, :])
        P.tt("dve", Wout[:, kc, :], wst[kc % 2], gate_b, ALU.mult)
    AOs = [M.alloc([8, 512], BF16, "AO1s%d" % i) for i in range(2)]
    xo = [M.alloc([1024], F32, "x1o%d" % i) for i in range(4)]
    ys = [M.psum(0, 1024, "y10"), M.psum(1024, 1024, "y11")]

    def ld_ao(g):
        if g * 4 < ntile_own:
            P.dma("sp", AOs[g % 2], AO[:, g * 512:(g + 1) * 512].rearrange("(fc p) t -> p fc t", p=128))

    def ld_x(t):
        if t < ntile_own:
            P.dma("sp", xo[t % 4], xin(t))

    ld_ao(0)
    ld_x(0)
    ld_x(1)
    for t in range(ntile_own):
        g4, j4 = t // 4, t % 4
        a = AOs[g4 % 2]
        if j4 == 0:
            ld_ao(g4 + 1)
        ld_x(t + 2)
        x_t = xo[t % 4]
        y = ys[t % 2]
        for n in range(2):
            for fc in range(8):
                P.mm(y[:, n * 512:(n + 1) * 512], a[:, fc, j4 * 128:(j4 + 1) * 128],
                     Wout[:, fc, n * 512:(n + 1) * 512], start=(fc == 0), stop=(fc == 7))
        P.tt("dve", x_t, x_t, y, ALU.add)
        P.dma("sp", x_out[t * 128:(t + 1) * 128, :], x_t, accum_w=True)
    P.barrier()


L1_SPECS = {
    "x": ([S, D], F32), "ident": ([128, 128], F32), "jmat": ([128, 128], F32), "c_a": ([128, 8], F32),
    "ada_w": ([2, D, 6 * D], F32), "ada_b": ([2, 6 * D], F32), "ada_b_a": ([2, 128, 48], F32),
    "norm_mix_a": ([2, 128, 8], F32), "rel_bias": ([32, 8], F32),
    "bias_oh": ([32, GW + 1280], F32), "bias_sel": ([32, 3], F32),
    "c_w_in": ([D, C_IN], F32), "c_qn": ([1, 128], F32), "c_kn": ([1, 128], F32), "c_lam": ([1, 256], F32),
    "c_subln_a": ([128, 1], F32), "c_w_out": ([D, D], F32),
}


def build_l1_mixer(ntile_all=NT, ntile_own=NTO, debug=False):
    nc = bass.Bass("TRN2", target_bir_lowering=False)
    es = ExitStack()
    with es:
        P = Prog(nc, es)
        M = Mem(nc, es, P, SBUF_WORDS)
        io = declare_inputs(nc, P, L1_SPECS)
        x_out = dram(nc, P, "x1", [SH, D], F32, kind="ExternalOutput")
        dbg = {} if debug else None
        emit_l1_mixer(P, M, io, io["x"], x_out, ntile_all, ntile_own, dbg)
        if debug:
            for k in ("QT1", "KT1", "V1"):
                src = dbg[k]
                o = dram(nc, P, "dbg_" + k, list(src.shape), BF16, kind="ExternalOutput")
                P.dma("sp", o, src, sem="dbgout")
        P.barrier()
        P.emit()
    return nc


def host_inputs_l1(inputs, core, x_rot):
    b, half = core // 2, core % 2
    oh, sel = make_bias_onehots(half)
    return {
        "x": np.ascontiguousarray(x_rot, dtype=np.float32),
        "ident": np.eye(128, dtype=np.float32),
        "jmat": np.ascontiguousarray(np.eye(128, dtype=np.float32)[::-1]),
        "c_a": a_layout(inputs["c"][b]),
        "ada_w": np.asarray(inputs["ada_w"], np.float32),
        "ada_b": np.asarray(inputs["ada_b"], np.float32),
        "ada_b_a": np.stack([a_layout(inputs["ada_b"][l]) for l in range(2)]),
        "norm_mix_a": np.stack([a_layout(inputs["norm_mix"][l]) for l in range(2)]),
        "rel_bias": np.asarray(inputs["rel_bias"], np.float32),
        "bias_oh": oh, "bias_sel": sel,
        "c_w_in": np.asarray(inputs["c_w_in"][0], np.float32),
        "c_qn": np.asarray(inputs["c_qn"][0], np.float32).reshape(1, 128),
        "c_kn": np.asarray(inputs["c_kn"][0], np.float32).reshape(1, 128),
        "c_lam": np.concatenate([inputs["c_lam_q1"][0], inputs["c_lam_k1"][0],
                                 inputs["c_lam_q2"][0], inputs["c_lam_k2"][0]]).astype(np.float32).reshape(1, 256),
        "c_subln_a": np.asarray(inputs["c_subln"][0], np.float32).reshape(128, 1),
        "c_w_out": np.asarray(inputs["c_w_out"][0], np.float32),
    }


def fused_specs():
    sp = {}
    sp.update({
        "x": ([S, D], F32), "ident": ([128, 128], F32), "c_a": ([128, 8], F32),
        "ada_w": ([2, D, 6 * D], F32), "ada_b": ([2, 6 * D], F32), "ada_b_a": ([2, 128, 48], F32),
        "norm_mix_a": ([2, 128, 8], F32), "rt": ([S, 192], F32),
        "ab_w_in": ([D, AB_IN], F32), "ab_q_a_norm_a": ([128, 4], F32), "ab_w_qb": ([512, 768], F32),
        "ab_kv_a_norm_a": ([128, 2], F32), "ab_w_kvb": ([256, 1024], F32),
        "ab_mla_qn": ([1, 96], F32), "ab_mla_kn": ([1, 96], F32), "ab_gqa_qn": ([1, 64], F32),
        "ab_gqa_kn": ([1, 64], F32), "ab_w_out": ([D, D], F32),
    })
    sp.update({k: v for k, v in L1_SPECS.items() if k != "x"})
    sp.update({
        "norm_ffn": ([2, D], F32), "moe_wr": ([2, D, 36], F32), "moe_rb": ([2, 36], F32),
        "tri": ([128, 128], F32), "ebase": ([1, 32], F32), "tokid": ([128, NTO], I32),
        "moe_w_up": ([2, 32, D, D], F32), "moe_w_down": ([2, 32, 512, D], F32),
        "other_off": ([1, 1], I32),
    })
    return sp


def build_fused(na=NT, no=NTO, parts=(1, 1, 1, 1, 1)):
    nc = bass.Bass("TRN2", target_bir_lowering=False)
    es = ExitStack()
    with es:
        P = Prog(nc, es)
        M = Mem(nc, es, P, SBUF_WORDS)
        reg = es.enter_context(nc.gpsimd.register("r_off"))
        io = declare_inputs(nc, P, fused_specs())
        out = dram(nc, P, "out", [SH, D], F32, kind="ExternalOutput")
        XA = dram(nc, P, "XA", [SH, D], F32)
        XBf = dram(nc, P, "XBf", [S, D], F32)
        XG = dram(nc, P, "XG", [S, D], F32)
        XC = dram(nc, P, "XC", [SH, D], F32)
        if parts[0]:
            emit_l0_mixer(P, M, io, io["x"], XA, na, no)
        if parts[1]:
            emit_moe(P, M, io, 0, XA, XBf[0:SH, :], no)
        XB_own = T(XBf.ap[0:SH, :], XBf.buf)
        XB_oth = T(XBf.ap[SH:S, :], P.buf("XB_oth"))
        M.reset()
        osb = M.alloc([8], I32, "osb", parts=1)
        P.dma("pool", osb[:, 0:1], io["other_off"])
        o1 = osb.ap[0:1, 0:1]
        P.op("pool", lambda e: e.reg_load(reg, o1), reads=[osb.buf], noinc=True)
        CH = 256
        NCH = SH // CH
        P.bg.add("ccsem")
        for ci in range(NCH):
            in_ap = XBf.ap[ci * CH:(ci + 1) * CH, :]
            out_ap = XG.ap[ci * 2 * CH:(ci + 1) * 2 * CH, :]
            P.op("pool", (lambda e, a=in_ap, b=out_ap: e.collective_compute(
                "AllGather", ALU.bypass, replica_groups=[[0, 1], [2, 3], [4, 5], [6, 7]], ins=[a], outs=[b])),
                reads=[XBf.buf], writes=[XG.buf], dma="ccsem", dma_inc=1, accum_w=True)
        P.barrier()

        def xrows(t):
            if t < NTO:
                return XB_own[t * 128:(t + 1) * 128, :]
            return XB_oth[(t - NTO) * 128:(t - NTO + 1) * 128, :]

        def hook(t):
            if t == min(16, no - 1):
                src = T(bass.AP(XG.ap.tensor, reg, [[2 * CH * D, NCH], [D, CH], [1, D]]), XG.buf)
                P.dma("pool", XB_oth.rearrange("(c r) d -> c r d", c=NCH), src, sem="xchg", accum_w=True)
                P.bg.discard("ccsem")

        if parts[3]:
            emit_l1_mixer(P, M, io, xrows, XC, na, no, tile_hook=hook)
        if parts[4]:
            emit_moe(P, M, io, 1, XC, out, no)
        P.barrier()
        P.emit()
    return nc


def host_inputs_fused(inputs, core, shared):
    b, half = core // 2, core % 2
    oh, sel = make_bias_onehots(half)
    d = dict(shared)
    d.update({
        "x": np.ascontiguousarray(_rot(np.asarray(inputs["x"][b], np.float32), half)),
        "c_a": a_layout(inputs["c"][b]),
        "rt": make_rt(half),
        "bias_oh": oh, "bias_sel": sel,
        "other_off": np.array([[(1 - half) * 256 * D]], np.int32),
    })
    return d


def host_shared(inputs):
    f = lambda a: np.ascontiguousarray(np.asarray(a, np.float32))
    wr = np.stack([np.concatenate([inputs["moe_w_group"][l],
                                   np.transpose(inputs["moe_w_expert"][l], (1, 0, 2)).reshape(D, 32)], axis=1)
                   for l in range(2)]).astype(np.float32)
    rb = np.stack([np.concatenate([inputs["moe_b_group"][l], inputs["moe_b_expert"][l].reshape(32)])
                   for l in range(2)]).astype(np.float32)
    return {
        "ident": np.eye(128, dtype=np.float32),
        "jmat": np.ascontiguousarray(np.eye(128, dtype=np.float32)[::-1]),
        "ada_w": f(inputs["ada_w"]), "ada_b": f(inputs["ada_b"]),
        "ada_b_a": np.stack([a_layout(inputs["ada_b"][l]) for l in range(2)]),
        "norm_mix_a": np.stack([a_layout(inputs["norm_mix"][l]) for l in range(2)]),
        "ab_w_in": f(inputs["ab_w_in"][0]), "ab_q_a_norm_a": a_layout(inputs["ab_q_a_norm"][0]),
        "ab_w_qb": f(inputs["ab_w_qb"][0]), "ab_kv_a_norm_a": a_layout(inputs["ab_kv_a_norm"][0]),
        "ab_w_kvb": f(inputs["ab_w_kvb"][0]),
        "ab_mla_qn": f(inputs["ab_mla_qn"]).reshape(1, 96), "ab_mla_kn": f(inputs["ab_mla_kn"]).reshape(1, 96),
        "ab_gqa_qn": f(inputs["ab_gqa_qn"]).reshape(1, 64), "ab_gqa_kn": f(inputs["ab_gqa_kn"]).reshape(1, 64),
        "ab_w_out": f(inputs["ab_w_out"][0]),
        "rel_bias": f(inputs["rel_bias"]),
        "c_w_in": f(inputs["c_w_in"][0]),
        "c_qn": f(inputs["c_qn"][0]).reshape(1, 128), "c_kn": f(inputs["c_kn"][0]).reshape(1, 128),
        "c_lam": np.concatenate([inputs["c_lam_q1"][0], inputs["c_lam_k1"][0],
                                 inputs["c_lam_q2"][0], inputs["c_lam_k2"][0]]).astype(np.float32).reshape(1, 256),
        "c_subln_a": f(inputs["c_subln"][0]).reshape(128, 1),
        "c_w_out": f(inputs["c_w_out"][0]),
        "norm_ffn": f(inputs["norm_ffn"]), "moe_wr": wr, "moe_rb": rb,
        "tri": np.triu(np.ones((128, 128), np.float32), 1),
        "ebase": (np.arange(32, dtype=np.float32) * CAP).reshape(1, 32),
        "tokid": (np.arange(NTO)[None, :] * 128 + np.arange(128)[:, None]).astype(np.int32),
        "moe_w_up": f(inputs["moe_w_up"]), "moe_w_down": f(inputs["moe_w_down"]),
    }


_PROGS = {}


def _prog(name, fn):
    if name not in _PROGS:
        _PROGS[name] = fn()
    return _PROGS[name]


def _rot(xb, half):
    return np.concatenate([xb[half * SH:(half + 1) * SH], xb[(1 - half) * SH:(2 - half) * SH]], axis=0)


def kernel_unfused(**inputs):
    inputs = {k: np.asarray(v) for k, v in inputs.items()}
    cores = list(range(8))
    nc = _prog("l0", lambda: build_l0_mixer())
    res = run_bass_kernel_spmd(nc, [host_inputs_l0(inputs, c) for c in cores], core_ids=cores)
    x1 = [np.asarray(r["x1"]) for r in res.results]
    nc = _prog("moe0", lambda: build_moe(0))
    res = run_bass_kernel_spmd(nc, [host_inputs_moe(inputs, c, x1[c], 0) for c in cores], core_ids=cores)
    x2 = [np.asarray(r["x2"]) for r in res.results]
    xb = [np.concatenate([x2[2 * b], x2[2 * b + 1]], axis=0) for b in range(4)]
    nc = _prog("l1", lambda: build_l1_mixer())
    res = run_bass_kernel_spmd(nc, [host_inputs_l1(inputs, c, _rot(xb[c // 2], c % 2)) for c in cores], core_ids=cores)
    x3 = [np.asarray(r["x1"]) for r in res.results]
    nc = _prog("moe1", lambda: build_moe(1))
    res = run_bass_kernel_spmd(nc, [host_inputs_moe(inputs, c, x3[c], 1) for c in cores], core_ids=cores)
    x4 = [np.asarray(r["x2"]) for r in res.results]
    out = np.stack([np.concatenate([x4[2 * b], x4[2 * b + 1]], axis=0) for b in range(4)]).astype(np.float32)
    return out


def kernel(**inputs):
    inputs = {k: np.asarray(v) for k, v in inputs.items()}
    cores = list(range(8))
    nc = _prog("fused", build_fused)
    shared = host_shared(inputs)
    in_maps = [host_inputs_fused(inputs, c, shared) for c in cores]
    res = run_bass_kernel_spmd(nc, in_maps, core_ids=cores)
    xs = [np.asarray(r["out"]) for r in res.results]
    return np.stack([np.concatenate([xs[2 * b], xs[2 * b + 1]], axis=0) for b in range(4)]).astype(np.float32)
```

```python
import math
from contextlib import ExitStack

import numpy as np
import concourse.bass as bass
import concourse.mybir as mybir
from concourse.bass_utils import run_bass_kernel_spmd

F32 = mybir.dt.float32
BF16 = mybir.dt.bfloat16
I32 = mybir.dt.int32
ALU = mybir.AluOpType
AF = mybir.ActivationFunctionType
AX = mybir.AxisListType

D = 1024
S = 8192
SH = 4096
NT = 64
NTO = 32
EPS = 1e-6
AB_IN = 1568


class Buf:
    __slots__ = ("name", "w", "r", "sb")

    def __init__(self, name):
        self.name = name
        self.w = {}
        self.r = {}
        self.sb = False


class T:
    __slots__ = ("ap", "buf")

    def __init__(self, ap, buf):
        self.ap = ap
        self.buf = buf

    def __getitem__(self, k):
        return T(self.ap[k], self.buf)

    def rearrange(self, s, **kw):
        return T(self.ap.rearrange(s, **kw), self.buf)

    def bitcast(self, dt):
        return T(self.ap.bitcast(dt), self.buf)

    def bcast(self, shape):
        return T(self.ap.to_broadcast(list(shape)), self.buf)

    def unsq(self, ax):
        return T(self.ap.unsqueeze(ax), self.buf)

    def pbc(self, n=128):
        return T(self.ap.partition_broadcast(n), self.buf)

    @property
    def shape(self):
        return self.ap.shape


def _ap(x):
    return x.ap if isinstance(x, T) else x


def _bufs(*xs):
    out = []
    for x in xs:
        if isinstance(x, T) and x.buf not in out:
            out.append(x.buf)
    return out


class Prog:
    ENG = ("pe", "act", "dve", "pool", "sp")

    def __init__(self, nc, es):
        self.nc = nc
        self.es = es
        self.q = {e: [] for e in self.ENG}
        self.esem = {e: es.enter_context(nc.semaphore("s_" + e)) for e in ("pe", "act", "dve", "pool")}
        self.ecnt = {e: 0 for e in ("pe", "act", "dve", "pool")}
        self.waited = {e: {} for e in self.ENG}
        self.dsems = {}
        self.free_dsems = []
        self.bg = set()
        self.nsem = 0
        self.nbuf = 0

    def buf(self, name=None):
        self.nbuf += 1
        return Buf(name or ("b%d" % self.nbuf))

    def _dsem(self, name):
        if name not in self.dsems:
            if self.free_dsems:
                self.dsems[name] = self.free_dsems.pop()
            else:
                self.nsem += 1
                self.dsems[name] = [self.es.enter_context(self.nc.semaphore("d_%d" % self.nsem)), 0]
        return self.dsems[name]

    def op(self, eng, fn, reads=(), writes=(), dma=None, n=1, accum_w=False, dma_inc=16, noinc=False):
        need = {}

        def add(tok):
            sem, val, src = tok
            if src == "pe" and eng == "pe":
                return
            k = id(sem)
            if k not in need or need[k][1] < val:
                need[k] = (sem, val)

        for b in reads:
            for tok in b.w.values():
                add(tok)
        for b in writes:
            if not accum_w:
                for tok in b.w.values():
                    add(tok)
            for tok in b.r.values():
                add(tok)
        waits = []
        wd = self.waited[eng]
        for k, (sem, val) in need.items():
            if wd.get(k, 0) < val:
                wd[k] = val
                waits.append((sem, val))
        if dma is not None:
            rec = self._dsem(dma)
            rec[1] += dma_inc * n
            tok = (rec[0], rec[1], "dma")
            inc = (rec[0], dma_inc)
        else:
            self.ecnt[eng] += 1
            tok = (self.esem[eng], self.ecnt[eng], eng)
            inc = (self.esem[eng], 1)
        if noinc:
            self.ecnt[eng] -= 1
            self.q[eng].append((waits, fn, None))
            return None
        self.q[eng].append((waits, fn, inc))
        k = id(tok[0])
        for b in reads:
            b.r[k] = tok
        for b in writes:
            if accum_w:
                b.w[k] = tok
            else:
                b.w = {k: tok}
            b.r = {}
        return tok

    def mm(self, out, lhsT, rhs, start=True, stop=True, extra_r=()):
        o, a, b = _ap(out), _ap(lhsT), _ap(rhs)
        self.op("pe", lambda e: e.matmul(o, lhsT=a, rhs=b, start=start, stop=stop),
                reads=_bufs(lhsT, rhs) + list(extra_r), writes=_bufs(out))

    def tr(self, out, in_, ident):
        o, a, i = _ap(out), _ap(in_), _ap(ident)
        self.op("pe", lambda e: e.transpose(o, a, i), reads=_bufs(in_, ident), writes=_bufs(out))

    def act(self, out, in_, func, bias=None, scale=None, accum=None):
        o, a = _ap(out), _ap(in_)
        kw = {}
        if bias is not None:
            kw["bias"] = _ap(bias)
        if scale is not None:
            kw["scale"] = _ap(scale)
        if accum is not None:
            kw["accum_out"] = _ap(accum)
        self.op("act", lambda e: e.activation(out=o, in_=a, func=func, **kw),
                reads=_bufs(in_, bias, scale), writes=_bufs(out, accum))

    def tt(self, eng, out, in0, in1, op):
        o, a, b = _ap(out), _ap(in0), _ap(in1)
        self.op(eng, lambda e: e.tensor_tensor(out=o, in0=a, in1=b, op=op),
                reads=_bufs(in0, in1), writes=_bufs(out))

    def ts(self, eng, out, in0, s1, s2, op0, op1=None):
        o, a, x1, x2 = _ap(out), _ap(in0), _ap(s1), _ap(s2)
        if op1 is None:
            self.op(eng, lambda e: e.tensor_scalar(out=o, in0=a, scalar1=x1, scalar2=None, op0=op0),
                    reads=_bufs(in0, s1), writes=_bufs(out))
        else:
            self.op(eng, lambda e: e.tensor_scalar(out=o, in0=a, scalar1=x1, scalar2=x2, op0=op0, op1=op1),
                    reads=_bufs(in0, s1, s2), writes=_bufs(out))

    def stt(self, eng, out, in0, scalar, in1, op0, op1):
        o, a, s, b = _ap(out), _ap(in0), _ap(scalar), _ap(in1)
        self.op(eng, lambda e: e.scalar_tensor_tensor(out=o, in0=a, scalar=s, in1=b, op0=op0, op1=op1),
                reads=_bufs(in0, scalar, in1), writes=_bufs(out))

    def copy(self, eng, out, in_):
        o, a = _ap(out), _ap(in_)
        if eng == "act":
            self.op(eng, lambda e: e.activation(out=o, in_=a, func=AF.Copy), reads=_bufs(in_), writes=_bufs(out))
        else:
            self.op(eng, lambda e: e.tensor_copy(out=o, in_=a), reads=_bufs(in_), writes=_bufs(out))

    def memset(self, eng, out, val):
        o = _ap(out)
        self.op(eng, lambda e: e.memset(o, val), writes=_bufs(out))

    def reduce(self, eng, out, in_, op=ALU.add, axis=AX.X):
        o, a = _ap(out), _ap(in_)
        self.op(eng, lambda e: e.tensor_reduce(out=o, in_=a, axis=axis, op=op), reads=_bufs(in_), writes=_bufs(out))

    def recip(self, out, in_):
        o, a = _ap(out), _ap(in_)
        self.op("dve", lambda e: e.reciprocal(out=o, in_=a), reads=_bufs(in_), writes=_bufs(out))

    def dma(self, eng, out, in_, sem=None, accum_w=False):
        o, a = _ap(out), _ap(in_)
        if sem is None:
            sb = out if (isinstance(out, T) and getattr(out.buf, "sb", False)) else in_
            sem = sb.buf.name
        self.op(eng, lambda e: e.dma_start(out=o, in_=a), reads=_bufs(in_), writes=_bufs(out), dma=sem, accum_w=accum_w)

    def barrier(self):
        toks = [(self.esem[e], self.ecnt[e]) for e in self.esem if self.ecnt[e] > 0]
        toks += [(rec[0], rec[1]) for name, rec in self.dsems.items() if rec[1] > 0 and name not in self.bg]
        for eng in self.ENG:
            waits = []
            wd = self.waited[eng]
            for sem, val in toks:
                if wd.get(id(sem), 0) < val:
                    wd[id(sem)] = val
                    waits.append((sem, val))
            if waits:
                self.q[eng].append((waits, None, None))
        self.free_dsems.extend(r for n_, r in self.dsems.items() if n_ not in self.bg)
        self.dsems = {n_: r for n_, r in self.dsems.items() if n_ in self.bg}

    def emit(self):
        nc = self.nc
        block = self.es.enter_context(nc.Block())

        def run(engname):
            def f(e):
                for waits, fn, inc in self.q[engname]:
                    for sem, val in waits:
                        e.wait_ge(sem, val)
                    if fn is not None:
                        ins = fn(e)
                        if inc is not None:
                            ins.then_inc(inc[0], inc[1])
            return f

        block.tensor(run("pe"))
        block.scalar(run("act"))
        block.vector(run("dve"))
        block.gpsimd(run("pool"))
        block.sync(run("sp"))


class Mem:
    def __init__(self, nc, es, P, sbuf_words):
        self.P = P
        self.sb = es.enter_context(nc.sbuf_tensor("arena", [128, sbuf_words], F32))
        self.ps = es.enter_context(nc.psum_tensor("psum", [128, 4096], F32))
        self.words = sbuf_words
        self.off = 0
        self.keep = 0

    def reset(self):
        self.off = self.keep

    def alloc(self, free_shape, dt=F32, name=None, parts=128):
        n = 1
        for s in free_shape:
            n *= s
        sz = 4 if dt in (F32, I32) else 2
        words = (n * sz + 3) // 4
        words = (words + 7) // 8 * 8
        assert self.off + words <= self.words, ("SBUF arena overflow", name, self.off, words, self.words)
        ap = self.sb[0:parts, self.off:self.off + words]
        self.off += words
        if dt != F32:
            ap = ap.bitcast(dt)
        ap = ap[:, 0:n]
        if len(free_shape) == 2:
            ap = ap.rearrange("p (a b) -> p a b", a=free_shape[0], b=free_shape[1])
        elif len(free_shape) == 3:
            ap = ap.rearrange("p (a b c) -> p a b c", a=free_shape[0], b=free_shape[1], c=free_shape[2])
        b = self.P.buf(name)
        b.sb = True
        return T(ap, b)

    def psum(self, col0, ncols, name=None, dt=F32):
        ap = self.ps[:, col0:col0 + ncols]
        if dt != F32:
            ap = ap.bitcast(dt)
        b = self.P.buf(name)
        return T(ap, b)


def dram(nc, P, name, shape, dt, kind="Internal"):
    t = nc.dram_tensor(name, list(shape), dt, kind=kind)
    return T(t.ap(), P.buf(name))


def emit_consts(P, M, io):
    idb = M.alloc([128], BF16, "idb")
    idf = M.alloc([128], F32, "idf")
    P.dma("sp", idf, io["ident"])
    P.copy("dve", idb, idf)
    return {"idb": idb}


def emit_mod(P, M, io, l, need_a, need_b):
    outA = {c: M.alloc([8], F32, "modA%d" % c) for c in need_a}
    outB = {c: M.alloc([1024], F32, "modB%d" % c) for c in need_b}
    keep = M.off
    cc = M.alloc([8], F32, "c_col")
    P.dma("sp", cc, io["c_a"])
    cond = M.alloc([8], F32, "cond")
    P.act(cond, cc, AF.Silu)
    crep = M.alloc([8, 128], F32, "cond_rep")
    P.copy("dve", crep, cond.unsq(2).bcast([128, 8, 128]))
    adab_a = M.alloc([48], F32, "adab_a")
    P.dma("sp", adab_a, io["ada_b_a"][l])
    wbuf = [M.alloc([8, 1024], F32, "adaw%d" % i) for i in range(2)]
    psA = M.psum(0, 8, "psA")
    psB = M.psum(512, 1024, "psB")
    chunks = sorted(set(need_a) | set(need_b))
    for i, c in enumerate(chunks):
        w = wbuf[i % 2]
        P.dma("sp", w, io["ada_w"][l][:, c * 1024:(c + 1) * 1024].rearrange("(kc p) n -> p kc n", p=128))
        if c in need_a:
            for fc in range(8):
                for kc in range(8):
                    P.mm(psA[:, fc:fc + 1], w[:, kc, fc * 128:(fc + 1) * 128], cond[:, kc:kc + 1],
                         start=(kc == 0), stop=(kc == 7))
            P.tt("dve", outA[c], psA, adab_a[:, c * 8:(c + 1) * 8], ALU.add)
        if c in need_b:
            for n in range(2):
                for kc in range(8):
                    P.mm(psB[:, n * 512:(n + 1) * 512], crep[:, kc, :], w[:, kc, n * 512:(n + 1) * 512],
                         start=(kc == 0), stop=(kc == 7))
            bb = outB[c]
            P.dma("sp", bb, io["ada_b"][l:l + 1, c * 1024:(c + 1) * 1024].pbc())
            P.tt("dve", bb, psB, bb, ALU.add)
    P.barrier()
    M.off = keep
    return outA, outB


def emit_rstd(P, ss, out, n, tmp=None):
    P.act(out, ss, AF.Sqrt, scale=1.0 / n, bias=EPS)
    P.recip(out, out)


def emit_l0_mixer(P, M, io, x_in, x_out, ntile_all=NT, ntile_own=NTO, dbg=None):
    nc = P.nc
    M.reset()
    cst = emit_consts(P, M, io)
    idb = cst["idb"]
    modA, modB = emit_mod(P, M, io, 0, need_a=[0, 1], need_b=[2])
    shift_a, scale_a, gate_b = modA[0], modA[1], modB[2]
    nm_a = M.alloc([8], F32, "nm_a")
    P.dma("sp", nm_a, io["norm_mix_a"][0])
    gs_a = M.alloc([8], F32, "gs_a")
    P.stt("dve", gs_a, scale_a, 1.0, nm_a, ALU.add, ALU.mult)

    Wp = M.alloc([8, AB_IN], BF16, "Wp")
    bias_bc = M.alloc([AB_IN], F32, "bias_bc")
    Wqb = M.alloc([4, 768], BF16, "Wqb")
    Wkvb = M.alloc([2, 1024], BF16, "Wkvb")
    g_mq = M.alloc([96], F32, "g_mq")
    g_mk = M.alloc([96], F32, "g_mk")
    g_gq = M.alloc([64], F32, "g_gq")
    g_gk = M.alloc([64], F32, "g_gk")
    keep_off = M.off

    shrep = M.alloc([8, 128], BF16, "shift_rep")
    P.copy("dve", shrep, shift_a.unsq(2).bcast([128, 8, 128]))
    Wo = M.alloc([8, AB_IN], BF16, "Wo")
    stg = [M.alloc([AB_IN], F32, "wstg%d" % i) for i in range(2)]
    for kc in range(8):
        s = stg[kc % 2]
        P.dma("sp", s, io["ab_w_in"][kc * 128:(kc + 1) * 128, :])
        P.act(Wp[:, kc, :], s, AF.Copy, scale=gs_a[:, kc:kc + 1])
        P.copy("pool", Wo[:, kc, :], s)
    for n0 in range(0, AB_IN, 512):
        n1 = min(AB_IN, n0 + 512)
        ps = M.psum((n0 // 512) * 512, n1 - n0, "psbias%d" % n0)
        for kc in range(8):
            P.mm(ps, shrep[:, kc, :], Wo[:, kc, n0:n1], start=(kc == 0), stop=(kc == 7))
        P.copy("dve", bias_bc[:, n0:n1], ps)
    qan = M.alloc([4], F32, "qan")
    P.dma("sp", qan, io["ab_q_a_norm_a"])
    kvan = M.alloc([2], F32, "kvan")
    P.dma("sp", kvan, io["ab_kv_a_norm_a"])
    for kc in range(4):
        s = stg[kc % 2]
        P.dma("sp", s[:, 0:768], io["ab_w_qb"][kc * 128:(kc + 1) * 128, :])
        P.act(Wqb[:, kc, :], s[:, 0:768], AF.Copy, scale=qan[:, kc:kc + 1])
    for kc in range(2):
        s = stg[kc % 2]
        P.dma("sp", s[:, 0:1024], io["ab_w_kvb"][kc * 128:(kc + 1) * 128, :])
        P.act(Wkvb[:, kc, :], s[:, 0:1024], AF.Copy, scale=kvan[:, kc:kc + 1])
    P.dma("sp", g_mq, io["ab_mla_qn"].pbc())
    P.ts("dve", g_mq, g_mq, 1.0 / math.sqrt(96.0), None, ALU.mult)
    P.dma("sp", g_mk, io["ab_mla_kn"].pbc())
    P.dma("sp", g_gq, io["ab_gqa_qn"].pbc())
    P.ts("dve", g_gq, g_gq, 0.125, None, ALU.mult)
    P.dma("sp", g_gk, io["ab_gqa_kn"].pbc())
    P.barrier()
    M.off = keep_off

    QTm = dram(nc, P, "QTm", [8, 96, SH], BF16)
    QTg = dram(nc, P, "QTg", [8, 64, SH], BF16)
    KTm = dram(nc, P, "KTm", [8, 96, S], BF16)
    KTg = dram(nc, P, "KTg", [2, 64, S], BF16)
    Vm = dram(nc, P, "Vm", [8, 128, NT, 128], BF16)
    Vg = dram(nc, P, "Vg", [2, 128, NT, 128], BF16)
    AO = dram(nc, P, "AO", [1024, SH], BF16)

    off_proj = M.off
    xt = [M.alloc([1024], F32, "xt%d" % i) for i in range(2)]
    rt = [M.alloc([192], F32, "rt%d" % i) for i in range(2)]
    junk = M.alloc([1024], BF16, "junk")
    xb = M.alloc([1024], BF16, "xb")
    xT = M.alloc([1024], BF16, "xT")
    st = M.alloc([16], F32, "stats")
    cq = M.alloc([512], F32, "cq")
    ckvpe = M.alloc([288], F32, "ckvpe")
    qg = M.alloc([8, 64], F32, "qg")
    kvg = M.alloc([256], F32, "kvg")
    cqb = M.alloc([512], BF16, "cqb")
    cqT = M.alloc([512], BF16, "cqT")
    ckvb = M.alloc([256], BF16, "ckvb")
    ckvT = M.alloc([256], BF16, "ckvT")
    q = M.alloc([8, 96], F32, "q")
    sq = M.alloc([1024], F32, "sq")
    ssh = M.alloc([8], F32, "ssh")
    rsh = M.alloc([8], F32, "rsh")
    qpe = M.alloc([8, 32], F32, "qpe")
    t1 = M.alloc([8, 64], F32, "t1")
    t2 = M.alloc([8, 64], F32, "t2")
    qf = M.alloc([8, 96], BF16, "qf")
    qgf = M.alloc([8, 64], BF16, "qgf")
    kv = M.alloc([8, 128], F32, "kv")
    kf = M.alloc([8, 96], BF16, "kf")
    kpe = M.alloc([32], F32, "kpe")
    kgn = M.alloc([2, 64], F32, "kgn")
    kgf = M.alloc([2, 64], BF16, "kgf")
    QTs = [M.alloc([8, 512], BF16, "QTs%d" % i, parts=96) for i in range(2)]
    QTgs = [M.alloc([8, 512], BF16, "QTgs%d" % i, parts=64) for i in range(2)]
    KTs = [M.alloc([8, 512], BF16, "KTs%d" % i, parts=96) for i in range(2)]
    KTgs = [M.alloc([2, 512], BF16, "KTgs%d" % i, parts=64) for i in range(2)]
    Vs = [M.alloc([8, 8, 128], BF16, "Vs%d" % i) for i in range(2)]
    Vgs = [M.alloc([8, 2, 128], BF16, "Vgs%d" % i) for i in range(2)]
    for i in range(2):
        P.memset("pool", Vs[i], 1.0)
        P.memset("pool", Vgs[i], 1.0)

    psT = M.psum(0, 512, "psT", BF16)
    psA = M.psum(512, 512, "psA")
    psBk = M.psum(1024, 512, "psB")
    psC = M.psum(1536, 512, "psC")
    psD = M.psum(2048, 512, "psD")
    psQ = M.psum(2560, 1024, "psQ")
    psO = M.psum(3584, 512, "psO", BF16)

    def head_norm_rope(src3, nh, d, gain, rope_w, cosT, ssinT, dst_bf, name):
        P.tt("pool", sq[:, 0:nh * d].rearrange("p (h d) -> p h d", h=nh), src3, src3, ALU.mult)
        P.reduce("dve", ssh[:, 0:nh], sq[:, 0:nh * d].rearrange("p (h d) -> p h d", h=nh))
        emit_rstd(P, ssh[:, 0:nh], rsh[:, 0:nh], d)
        P.tt("dve", src3, src3, rsh[:, 0:nh].unsq(2).bcast([128, nh, d]), ALU.mult)
        return

    def ld_proj(t):
        if t < ntile_all:
            P.dma("sp", xt[t % 2], x_in[t * 128:(t + 1) * 128, :])
            P.dma("sp", rt[t % 2], io["rt"][t * 128:(t + 1) * 128, :])

    ld_proj(0)
    for t in range(ntile_all):
        own = t < ntile_own
        x_t = xt[t % 2]
        r_t = rt[t % 2]
        ld_proj(t + 1)
        P.act(junk, x_t, AF.Square, accum=st[:, 0:1])
        emit_rstd(P, st[:, 0:1], st[:, 1:2], 1024)
        rstd = st[:, 1:2]
        P.copy("pool", xb, x_t)
        for kc in range(8):
            P.tr(psT[:, kc * 128:(kc + 1) * 128], xb[:, kc * 128:(kc + 1) * 128], idb)
        P.copy("act", xT, psT)
        segs = []
        if own:
            segs += [(psA, 0, 512, cq), (psC, 800, 1312, qg.rearrange("p h d -> p (h d)"))]
        segs += [(psBk, 512, 800, ckvpe), (psD, 1312, 1568, kvg)]
        for ps, c0, c1, dst in segs:
            for kc in range(8):
                P.mm(ps[:, 0:c1 - c0], xT[:, kc * 128:(kc + 1) * 128], Wp[:, kc, c0:c1],
                     start=(kc == 0), stop=(kc == 7))
        for ps, c0, c1, dst in segs:
            P.stt("dve", dst, ps[:, 0:c1 - c0], rstd, bias_bc[:, c0:c1], ALU.mult, ALU.add)
        slot4 = (t // 4) % 2
        j4 = t % 4
        cosm, ssinm = r_t[:, 0:32], r_t[:, 32:64]
        cosa, ssina = r_t[:, 64:128], r_t[:, 128:192]
        if own:
            P.act(junk[:, 0:512], cq, AF.Square, accum=st[:, 2:3])
            emit_rstd(P, st[:, 2:3], st[:, 3:4], 512)
            P.copy("pool", cqb, cq)
            for kc in range(4):
                P.tr(psT[:, kc * 128:(kc + 1) * 128], cqb[:, kc * 128:(kc + 1) * 128], idb)
            P.copy("act", cqT, psT[:, 0:512])
            for n0, n1 in ((0, 512), (512, 768)):
                for kc in range(4):
                    P.mm(psQ[:, n0:n1], cqT[:, kc * 128:(kc + 1) * 128], Wqb[:, kc, n0:n1],
                         start=(kc == 0), stop=(kc == 3))
            qflat = q.rearrange("p h d -> p (h d)")
            P.ts("dve", qflat, psQ[:, 0:768], st[:, 3:4], None, ALU.mult)
            head_norm_rope(q, 8, 96, None, None, None, None, None, "q")
            P.tt("pool", qf[:, :, 0:64], q[:, :, 0:64], g_mq[:, 0:64].unsq(1).bcast([128, 8, 64]), ALU.mult)
            P.tt("dve", qpe, q[:, :, 64:96], g_mq[:, 64:96].unsq(1).bcast([128, 8, 32]), ALU.mult)
            P.tt("pool", t1[:, :, 0:32], qpe, cosm.unsq(1).bcast([128, 8, 32]), ALU.mult)
            P.tt("dve", t2[:, :, 0:16], qpe[:, :, 16:32], ssinm[:, 0:16].unsq(1).bcast([128, 8, 16]), ALU.mult)
            P.tt("dve", t2[:, :, 16:32], qpe[:, :, 0:16], ssinm[:, 16:32].unsq(1).bcast([128, 8, 16]), ALU.mult)
            P.tt("dve", qf[:, :, 64:96], t1[:, :, 0:32], t2[:, :, 0:32], ALU.add)
            for h in range(8):
                P.tr(psO[0:96, h * 128:(h + 1) * 128], qf[:, h, :], idb)
            P.copy("act", QTs[slot4][:, :, j4 * 128:(j4 + 1) * 128],
                   psO[0:96, :].rearrange("p (h t) -> p h t", h=8))
            head_norm_rope(qg, 8, 64, None, None, None, None, None, "qg")
            P.tt("pool", qg, qg, g_gq.unsq(1).bcast([128, 8, 64]), ALU.mult)
            P.tt("pool", t1, qg, cosa.unsq(1).bcast([128, 8, 64]), ALU.mult)
            qg5 = qg.rearrange("p h (b f e) -> p h b f e", b=2, f=2)
            t25 = t2.rearrange("p h (b f e) -> p h b f e", b=2, f=2)
            ss5 = ssina.rearrange("p (b f e) -> p b f e", b=2, f=2)
            for f in range(2):
                for bb in range(2):
                    P.tt("dve", t25[:, :, bb, f, :], qg5[:, :, bb, 1 - f, :],
                         ss5[:, bb, f, :].unsq(1).bcast([128, 8, 16]), ALU.mult)
            P.tt("dve", qgf, t1, t2, ALU.add)
            for h in range(8):
                P.tr(psO[0:64, h * 128:(h + 1) * 128], qgf[:, h, :], idb)
            P.copy("act", QTgs[slot4][:, :, j4 * 128:(j4 + 1) * 128],
                   psO[0:64, :].rearrange("p (h t) -> p h t", h=8))
        ckv = ckvpe[:, 0:256]
        P.act(junk[:, 0:256], ckv, AF.Square, accum=st[:, 4:5])
        emit_rstd(P, st[:, 4:5], st[:, 5:6], 256)
        P.copy("pool", ckvb, ckv)
        for kc in range(2):
            P.tr(psT[:, kc * 128:(kc + 1) * 128], ckvb[:, kc * 128:(kc + 1) * 128], idb)
        P.copy("act", ckvT, psT[:, 0:256])
        for n0, n1 in ((0, 512), (512, 1024)):
            for kc in range(2):
                P.mm(psQ[:, n0:n1], ckvT[:, kc * 128:(kc + 1) * 128], Wkvb[:, kc, n0:n1],
                     start=(kc == 0), stop=(kc == 1))
        kvflat = kv.rearrange("p h d -> p (h d)")
        P.ts("dve", kvflat, psQ, st[:, 5:6], None, ALU.mult)
        slot16 = (t // 8) % 2
        j16 = t % 8
        P.copy("pool", Vs[slot16][:, j16, :, 0:64], kv[:, :, 64:128])
        sq3 = sq[:, 0:512].rearrange("p (h d) -> p h d", h=8)
        P.tt("pool", sq3, kv[:, :, 0:64], kv[:, :, 0:64], ALU.mult)
        P.reduce("dve", ssh, sq3)
        P.act(junk[:, 0:32], ckvpe[:, 256:288], AF.Square, accum=st[:, 6:7])
        P.ts("dve", ssh, ssh, st[:, 6:7], None, ALU.add)
        emit_rstd(P, ssh, rsh, 96)
        P.tt("dve", kv[:, :, 0:64], kv[:, :, 0:64], rsh.unsq(2).bcast([128, 8, 64]), ALU.mult)
        P.tt("pool", kf[:, :, 0:64], kv[:, :, 0:64], g_mk[:, 0:64].unsq(1).bcast([128, 8, 64]), ALU.mult)
        P.tt("dve", kpe, ckvpe[:, 256:288], g_mk[:, 64:96], ALU.mult)
        tk1 = t1[:, 0, 0:32]
        tk2 = t2[:, 0, 0:32]
        P.tt("dve", tk1, kpe, cosm, ALU.mult)
        P.tt("dve", tk2[:, 0:16], kpe[:, 16:32], ssinm[:, 0:16], ALU.mult)
        P.tt("dve", tk2[:, 16:32], kpe[:, 0:16], ssinm[:, 16:32], ALU.mult)
        P.tt("dve", kpe, tk1, tk2, ALU.add)
        P.tt("dve", kf[:, :, 64:96], kpe.unsq(1).bcast([128, 8, 32]), rsh.unsq(2).bcast([128, 8, 32]), ALU.mult)
        for h in range(8):
            P.tr(psO[0:96, h * 128:(h + 1) * 128], kf[:, h, :], idb)
        P.copy("act", KTs[slot4][:, :, j4 * 128:(j4 + 1) * 128],
               psO[0:96, :].rearrange("p (h t) -> p h t", h=8))
        kg = kvg[:, 0:128].rearrange("p (h d) -> p h d", h=2)
        P.copy("pool", Vgs[slot16][:, j16, :, 0:64], kvg[:, 128:256].rearrange("p (h d) -> p h d", h=2))
        sqg = sq[:, 0:128].rearrange("p (h d) -> p h d", h=2)
        P.tt("pool", sqg, kg, kg, ALU.mult)
        P.reduce("dve", ssh[:, 0:2], sqg)
        emit_rstd(P, ssh[:, 0:2], rsh[:, 0:2], 64)
        P.tt("dve", kgn, kg, rsh[:, 0:2].unsq(2).bcast([128, 2, 64]), ALU.mult)
        P.tt("dve", kgn, kgn, g_gk.unsq(1).bcast([128, 2, 64]), ALU.mult)
        tg1 = t1[:, 0:2, :]
        tg2 = t2[:, 0:2, :]
        P.tt("pool", tg1, kgn, cosa.unsq(1).bcast([128, 2, 64]), ALU.mult)
        kg5 = kgn.rearrange("p h (b f e) -> p h b f e", b=2, f=2)
        tg25 = tg2.rearrange("p h (b f e) -> p h b f e", b=2, f=2)
        ss5 = ssina.rearrange("p (b f e) -> p b f e", b=2, f=2)
        for f in range(2):
            for bb in range(2):
                P.tt("dve", tg25[:, :, bb, f, :], kg5[:, :, bb, 1 - f, :],
                     ss5[:, bb, f, :].unsq(1).bcast([128, 2, 16]), ALU.mult)
        P.tt("dve", kgf, tg1, tg2, ALU.add)
        for h in range(2):
            P.tr(psO[0:64, h * 128:(h + 1) * 128], kgf[:, h, :], idb)
        P.copy("act", KTgs[slot4][:, :, j4 * 128:(j4 + 1) * 128],
               psO[0:64, 0:256].rearrange("p (h t) -> p h t", h=2))
        if j4 == 3:
            g4 = t // 4
            c0 = g4 * 512
            if own:
                P.dma("sp", QTm[:, :, c0:c0 + 512].rearrange("h p t -> p h t"), QTs[slot4], accum_w=True)
                P.dma("sp", QTg[:, :, c0:c0 + 512].rearrange("h p t -> p h t"), QTgs[slot4], accum_w=True)
            P.dma("sp", KTm[:, :, c0:c0 + 512].rearrange("h p t -> p h t"), KTs[slot4], accum_w=True)
            P.dma("sp", KTg[:, :, c0:c0 + 512].rearrange("h p t -> p h t"), KTgs[slot4], accum_w=True)
        if j16 == 7 or t == ntile_all - 1:
            g16 = t // 8
            nt16 = j16 + 1
            for hh in range(8):
                P.dma("sp", Vm[hh][:, g16 * 8:g16 * 8 + nt16, :], Vs[slot16][:, 0:nt16, hh, :], accum_w=True)
            for hh in range(2):
                P.dma("sp", Vg[hh][:, g16 * 8:g16 * 8 + nt16, :], Vgs[slot16][:, 0:nt16, hh, :], accum_w=True)
    if dbg is not None:
        dbg["QTm"], dbg["QTg"], dbg["KTm"], dbg["KTg"], dbg["Vm"], dbg["Vg"] = QTm, QTg, KTm, KTg, Vm, Vg
    P.barrier()
    M.off = off_proj

    nkt = ntile_all
    nqt = ntile_own // 4
    KT = [M.alloc([nkt * 128], BF16, "KT%d" % i, parts=96) for i in range(2)]
    Vh = [M.alloc([nkt, 128], BF16, "Vh%d" % i) for i in range(2)]
    QT = [M.alloc([nqt * 512], BF16, "QT%d" % i, parts=96) for i in range(2)]
    OT = [M.alloc([nqt * 512], BF16, "OT%d" % i, parts=64) for i in range(2)]
    Pb = [M.alloc([1536], BF16, "Pb%d" % i) for i in range(2)]
    rl = M.alloc([512], F32, "rl", parts=64)
    Sg = [M.psum(0, 1536, "Sg0"), M.psum(1536, 1536, "Sg1")]
    Oa = [M.psum(3072, 512, "Oa0"), M.psum(3584, 512, "Oa1")]
    groups = []
    k0 = 0
    while k0 < nkt:
        n = 3 if (nkt - k0) not in (2, 4) else 2
        n = min(n, nkt - k0)
        groups.append(list(range(k0, k0 + n)))
        k0 += n

    def load_head(h):
        s = h % 2
        if h < 8:
            P.dma("sp", KT[s], KTm[h][:, 0:nkt * 128])
            P.dma("sp", Vh[s], Vm[h][:, 0:nkt])
            P.dma("sp", QT[s], QTm[h][:, 0:nqt * 512])
        else:
            P.dma("sp", KT[s][0:64], KTg[(h - 8) // 4][:, 0:nkt * 128])
            P.dma("sp", Vh[s], Vg[(h - 8) // 4][:, 0:nkt])
            P.dma("sp", QT[s][0:64], QTg[h - 8][:, 0:nqt * 512])

    tasks = []
    for h in range(16):
        for qt in range(nqt):
            for gi, g in enumerate(groups):
                tasks.append((h, qt, gi, g))

    def qk(i):
        h, qt, gi, g = tasks[i]
        s = h % 2
        d = 96 if h < 8 else 64
        for j, kt in enumerate(g):
            P.mm(Sg[i % 2][:, j * 512:(j + 1) * 512], KT[s][0:d, kt * 128:(kt + 1) * 128],
                 QT[s][0:d, qt * 512:(qt + 1) * 512])

    def ex_pv(i):
        h, qt, gi, g = tasks[i]
        s = h % 2
        n = len(g)
        P.act(Pb[i % 2][:, 0:n * 512], Sg[i % 2][:, 0:n * 512], AF.Exp)
        oa = Oa[(h * nqt + qt) % 2]
        for j, kt in enumerate(g):
            P.mm(oa, Vh[s][:, kt, :], Pb[i % 2][:, j * 512:(j + 1) * 512],
                 start=(kt == 0), stop=(kt == nkt - 1))
        if gi == len(groups) - 1:
            P.recip(rl, oa[64:128, :])
            P.tt("dve", OT[s][:, qt * 512:(qt + 1) * 512], oa[0:64, :], rl, ALU.mult)
            if qt == nqt - 1:
                P.dma("sp", AO[h * 64:(h + 1) * 64, 0:nqt * 512], OT[s])
                if h + 2 < 16:
                    load_head(h + 2)

    load_head(0)
    load_head(1)
    for i in range(len(tasks) + 1):
        if i < len(tasks):
            qk(i)
        if i >= 1:
            ex_pv(i - 1)
    P.barrier()
    M.off = off_proj

    Wout = M.alloc([8, 1024], BF16, "Wout")
    wst = [M.alloc([1024], F32, "wost%d" % i) for i in range(2)]
    for kc in range(8):
        P.dma("sp", wst[kc % 2], io["ab_w_out"][kc * 128:(kc + 1) * 128, :])
        P.tt("dve", Wout[:, kc, :], wst[kc % 2], gate_b, ALU.mult)
    AOs = [M.alloc([8, 512], BF16, "AOs%d" % i) for i in range(2)]
    xo = [M.alloc([1024], F32, "xo%d" % i) for i in range(4)]
    ys = [M.psum(i * 1024, 1024, "y%d" % i) for i in range(4)]

    def ld_ao(g):
        if g * 4 < ntile_own:
            P.dma("sp", AOs[g % 2], AO[:, g * 512:(g + 1) * 512].rearrange("(fc p) t -> p fc t", p=128))

    def ld_x(t):
        if t < ntile_own:
            P.dma("sp", xo[t % 4], x_in[t * 128:(t + 1) * 128, :])

    ld_ao(0)
    ld_x(0)
    ld_x(1)
    for t in range(ntile_own):
        g4 = t // 4
        j4 = t % 4
        a = AOs[g4 % 2]
        if j4 == 0:
            ld_ao(g4 + 1)
        ld_x(t + 2)
        x_t = xo[t % 4]
        y = ys[t % 4]
        for n in range(2):
            for fc in range(8):
                P.mm(y[:, n * 512:(n + 1) * 512], a[:, fc, j4 * 128:(j4 + 1) * 128],
                     Wout[:, fc, n * 512:(n + 1) * 512], start=(fc == 0), stop=(fc == 7))
        P.tt("dve", x_t, x_t, y, ALU.add)
        P.dma("sp", x_out[t * 128:(t + 1) * 128, :], x_t, accum_w=True)
    P.barrier()


def rope_tables_np(pos, dim):
    inv = (np.float32(10000.0) ** (-np.arange(0, dim, 2, dtype=np.float32) / np.float32(dim))).astype(np.float32)
    ang = pos.astype(np.float32)[:, None] * inv[None, :]
    ang = np.concatenate([ang, ang], axis=-1)
    return np.cos(ang).astype(np.float32), np.sin(ang).astype(np.float32)


def signed_sin(sin):
    h = sin.shape[-1] // 2
    return np.concatenate([-sin[..., :h], sin[..., h:]], axis=-1)


def make_rt(half):
    pos = (np.arange(S) + half * SH) % S
    cm, sm = rope_tables_np(pos, 32)
    row, col = pos // 64, pos % 64
    rc, rs = rope_tables_np(row, 32)
    cc, cs = rope_tables_np(col, 32)
    return np.concatenate([cm, signed_sin(sm), rc, cc, signed_sin(rs), signed_sin(cs)], axis=-1).astype(np.float32)


def a_layout(v):
    return np.ascontiguousarray(np.asarray(v, np.float32).reshape(-1, 128).T)


def declare_inputs(nc, P, specs):
    io = {}
    for name, (shape, dt) in specs.items():
        io[name] = dram(nc, P, name, shape, dt, kind="ExternalInput")
    return io


SBUF_WORDS = 48 * 1024


def build_l0_mixer(ntile_all=NT, ntile_own=NTO, debug=False):
    nc = bass.Bass("TRN2", target_bir_lowering=False)
    es = ExitStack()
    with es:
        P = Prog(nc, es)
        M = Mem(nc, es, P, SBUF_WORDS)
        specs = {
            "x": ([S, D], F32), "ident": ([128, 128], F32), "c_a": ([128, 8], F32),
            "ada_w": ([2, D, 6 * D], F32), "ada_b": ([2, 6 * D], F32), "ada_b_a": ([2, 128, 48], F32),
            "norm_mix_a": ([2, 128, 8], F32), "rt": ([S, 192], F32),
            "ab_w_in": ([D, AB_IN], F32), "ab_q_a_norm_a": ([128, 4], F32), "ab_w_qb": ([512, 768], F32),
            "ab_kv_a_norm_a": ([128, 2], F32), "ab_w_kvb": ([256, 1024], F32),
            "ab_mla_qn": ([1, 96], F32), "ab_mla_kn": ([1, 96], F32), "ab_gqa_qn": ([1, 64], F32),
            "ab_gqa_kn": ([1, 64], F32), "ab_w_out": ([D, D], F32),
        }
        io = declare_inputs(nc, P, specs)
        x_out = dram(nc, P, "x1", [SH, D], F32, kind="ExternalOutput")
        dbg = {} if debug else None
        emit_l0_mixer(P, M, io, io["x"], x_out, ntile_all, ntile_own, dbg)
        if debug:
            for k in ("QTm", "KTm", "Vm", "QTg", "KTg", "Vg"):
                src = dbg[k]
                o = dram(nc, P, "dbg_" + k, list(src.shape), BF16, kind="ExternalOutput")
                P.dma("sp", o, src, sem="dbgout")
        P.barrier()
        P.emit()
    return nc


def host_inputs_l0(inputs, core):
    b, half = core // 2, core % 2
    x = np.asarray(inputs["x"][b], np.float32)
    xr = np.concatenate([x[half * SH:(half + 1) * SH], x[(1 - half) * SH:(2 - half) * SH]], axis=0)
    return {
        "x": np.ascontiguousarray(xr),
        "ident": np.eye(128, dtype=np.float32),
        "c_a": a_layout(inputs["c"][b]),
        "ada_w": np.asarray(inputs["ada_w"], np.float32),
        "ada_b": np.asarray(inputs["ada_b"], np.float32),
        "ada_b_a": np.stack([a_layout(inputs["ada_b"][l]) for l in range(2)]),
        "norm_mix_a": np.stack([a_layout(inputs["norm_mix"][l]) for l in range(2)]),
        "rt": make_rt(half),
        "ab_w_in": np.asarray(inputs["ab_w_in"][0], np.float32),
        "ab_q_a_norm_a": a_layout(inputs["ab_q_a_norm"][0]),
        "ab_w_qb": np.asarray(inputs["ab_w_qb"][0], np.float32),
        "ab_kv_a_norm_a": a_layout(inputs["ab_kv_a_norm"][0]),
        "ab_w_kvb": np.asarray(inputs["ab_w_kvb"][0], np.float32),
        "ab_mla_qn": np.asarray(inputs["ab_mla_qn"], np.float32).reshape(1, 96),
        "ab_mla_kn": np.asarray(inputs["ab_mla_kn"], np.float32).reshape(1, 96),
        "ab_gqa_qn": np.asarray(inputs["ab_gqa_qn"], np.float32).reshape(1, 64),
        "ab_gqa_kn": np.asarray(inputs["ab_gqa_kn"], np.float32).reshape(1, 64),
        "ab_w_out": np.asarray(inputs["ab_w_out"][0], np.float32),
    }


CAPB = 8
CAP = CAPB * 128
NSLOT = 32 * CAP
TRASH = NSLOT


def _idma(P, out, in_, out_off=None, in_off=None, sem=None, accum_w=False, bounds=None):
    o, a = _ap(out), _ap(in_)
    oo = bass.IndirectOffsetOnAxis(ap=_ap(out_off), axis=0) if out_off is not None else None
    io_ = bass.IndirectOffsetOnAxis(ap=_ap(in_off), axis=0) if in_off is not None else None
    if sem is None:
        sb = out if (isinstance(out, T) and out.buf.sb) else in_
        sem = sb.buf.name
    P.op("pool", lambda e: e.indirect_dma_start(out=o, out_offset=oo, in_=a, in_offset=io_,
                                                bounds_check=bounds, oob_is_err=False),
         reads=_bufs(in_, out_off, in_off), writes=_bufs(out), dma=sem, accum_w=accum_w)


def emit_moe(P, M, io, l, x_in, x_out, ntile=NTO, dbg=None, wl=None):
    nc = P.nc
    wl = l if wl is None else wl
    M.reset()
    cst = emit_consts(P, M, io)
    idb = cst["idb"]
    modA, modB = emit_mod(P, M, io, l, need_a=[], need_b=[3, 4, 5])
    shift_b, scale_b, gate_b = modB[3], modB[4], modB[5]
    gs_b = scale_b
    nf = M.alloc([1024], F32, "nf_b")
    P.dma("sp", nf, io["norm_ffn"][l:l + 1, :].pbc())
    P.stt("dve", gs_b, scale_b, 1.0, nf, ALU.add, ALU.mult)
    wr = M.alloc([8, 36], F32, "wr")
    P.dma("sp", wr, io["moe_wr"][wl].rearrange("(kc p) n -> p kc n", p=128))
    whi = M.alloc([8, 36], BF16, "whi")
    wlo = M.alloc([8, 36], BF16, "wlo")
    P.copy("dve", whi, wr)
    P.tt("dve", wlo, wr, whi, ALU.subtract)
    rbias = M.alloc([36], F32, "rbias")
    P.dma("sp", rbias, io["moe_rb"][wl:wl + 1, :].pbc())
    trif = M.alloc([128], F32, "trif")
    P.dma("sp", trif, io["tri"])
    tri = M.alloc([128], BF16, "tri")
    P.copy("dve", tri, trif)
    ones = M.alloc([128], BF16, "ones")
    P.memset("dve", ones, 1.0)
    ebase = M.alloc([32], F32, "ebase")
    P.dma("sp", ebase, io["ebase"].pbc())
    tokid = M.alloc([ntile + 8], I32, "tokid")
    P.dma("sp", tokid[:, 0:ntile], io["tokid"][:, 0:ntile])
    LG = M.alloc([ntile, 36], F32, "LG")
    gates = M.alloc([ntile, 2], F32, "gates")
    slots_i = M.alloc([ntile + 4, 2], I32, "slots_i")
    keep_off = M.off

    H2 = dram(nc, P, "H2_%d" % l, [SH, D], BF16)
    SLOT_T = dram(nc, P, "SLOT_T%d" % l, [NSLOT + 128, 1], I32)
    Y = dram(nc, P, "Y_%d" % l, [NSLOT + 128, D], F32)

    zt = M.alloc([1024], F32, "zt")
    P.memset("dve", zt, 0.0)
    P.dma("sp", SLOT_T.rearrange("(p j) o -> p (j o)", p=128), zt[:, 0:(NSLOT + 128) // 128].bitcast(I32))
    P.dma("sp", Y[NSLOT:NSLOT + 128, :], zt)

    xt = [M.alloc([1024], F32, "mxt%d" % i) for i in range(4)]
    junk2 = [M.alloc([1024], BF16, "mjunk%d" % i) for i in range(2)]
    st2 = [M.alloc([8], F32, "mst%d" % i) for i in range(2)]
    h22 = [M.alloc([1024], F32, "h2_%d" % i) for i in range(2)]
    hi = [M.alloc([1024], BF16, "hi%d" % i) for i in range(2)]
    lo2 = [M.alloc([1024], BF16, "lo%d" % i) for i in range(2)]
    hT2 = [M.alloc([1024], BF16, "hT%d" % i) for i in range(2)]
    lT2 = [M.alloc([1024], BF16, "lT%d" % i) for i in range(2)]
    psT = [M.psum(0, 512, "mpsT0", BF16), M.psum(512, 512, "mpsT1", BF16)]
    psL2 = [M.psum(1024, 36, "psL0"), M.psum(1536, 36, "psL1")]

    def ld_mx(t):
        if t < ntile:
            P.dma("sp", xt[t % 4], x_in[t * 128:(t + 1) * 128, :])

    ld_mx(0)
    ld_mx(1)
    for t in range(ntile):
        ld_mx(t + 2)
        x_t = xt[t % 4]
        junk, st, h2, lo, hT, lT, psL = junk2[t % 2], st2[t % 2], h22[t % 2], lo2[t % 2], hT2[t % 2], lT2[t % 2], psL2[t % 2]
        P.act(junk, x_t, AF.Square, accum=st[:, 0:1])
        emit_rstd(P, st[:, 0:1], st[:, 1:2], 1024)
        P.stt("dve", h2, x_t, st[:, 1:2], gs_b, ALU.mult, ALU.mult)
        P.tt("pool", h2, h2, shift_b, ALU.add)
        hi_t = hi[t % 2]
        P.copy("act", hi_t, h2)
        P.tt("dve", lo, h2, hi_t, ALU.subtract)
        P.dma("sp", H2[t * 128:(t + 1) * 128, :], hi_t, accum_w=True)
        for kc in range(8):
            P.tr(psT[0][:, kc * 128:(kc + 1) * 128], hi_t[:, kc * 128:(kc + 1) * 128], idb)
        P.copy("act", hT, psT[0])
        for kc in range(8):
            P.tr(psT[1][:, kc * 128:(kc + 1) * 128], lo[:, kc * 128:(kc + 1) * 128], idb)
        P.copy("act", lT, psT[1])
        n = 0
        for a, w in ((hT, whi), (hT, wlo), (lT, whi)):
            for kc in range(8):
                P.mm(psL, a[:, kc * 128:(kc + 1) * 128], w[:, kc, :], start=(n == 0), stop=(n == 23))
                n += 1
        P.tt("dve", LG[:, t, :], psL, rbias, ALU.add)
    nt = ntile
    w4 = M.alloc([nt, 4], F32, "w4")
    ohg = M.alloc([nt, 4], F32, "ohg")
    c1 = M.alloc([nt], F32, "c1")
    c2 = M.alloc([nt], F32, "c2")
    c3 = M.alloc([nt], F32, "c3")
    pgt = M.alloc([nt], F32, "pgt")
    el = M.alloc([nt, 8], F32, "el")
    el2 = M.alloc([nt, 8], F32, "el2")
    w8 = M.alloc([nt, 8], F32, "w8")
    oh1 = M.alloc([nt, 8], F32, "oh1")
    oh2 = M.alloc([nt, 8], F32, "oh2")
    A1 = M.alloc([nt, 4, 8], F32, "A1")
    A2 = M.alloc([nt, 4, 8], F32, "A2")
    Ab = M.alloc([nt, 32], BF16, "Ab")
    Acum = M.alloc([nt + 1, 32], BF16, "Acum")
    pos = M.alloc([nt, 32], F32, "pos")
    w32 = M.alloc([nt, 32], F32, "w32")
    lg4 = LG[:, :, 0:4]
    P.reduce("dve", c1, lg4, op=ALU.max)
    P.tt("dve", ohg, lg4, c1.unsq(2).bcast([128, nt, 4]), ALU.is_equal)
    P.tt("dve", w4, lg4, c1.unsq(2).bcast([128, nt, 4]), ALU.subtract)
    P.act(w4, w4, AF.Exp)
    P.reduce("dve", c2, w4)
    P.recip(pgt, c2)
    for g in range(4):
        src = LG[:, :, 4 + 8 * g:12 + 8 * g]
        og = ohg[:, :, g:g + 1].bcast([128, nt, 8])
        if g == 0:
            P.tt("dve", el, src, og, ALU.mult)
        else:
            P.tt("dve", w8, src, og, ALU.mult)
            P.tt("dve", el, el, w8, ALU.add)
    P.reduce("dve", c1, el, op=ALU.max)
    P.tt("dve", oh1, el, c1.unsq(2).bcast([128, nt, 8]), ALU.is_equal)
    P.stt("dve", el2, oh1, -1e30, el, ALU.mult, ALU.add)
    P.reduce("dve", c2, el2, op=ALU.max)
    P.tt("dve", oh2, el2, c2.unsq(2).bcast([128, nt, 8]), ALU.is_equal)
    P.tt("dve", c3, c2, c1, ALU.subtract)
    P.act(c3, c3, AF.Exp)
    P.ts("dve", c3, c3, 1.0, None, ALU.add)
    P.recip(c3, c3)
    P.tt("dve", gates[:, :, 0], pgt, c3, ALU.mult)
    P.tt("dve", gates[:, :, 1], pgt, gates[:, :, 0], ALU.subtract)
    P.tt("dve", A1, ohg.unsq(3).bcast([128, nt, 4, 8]), oh1.unsq(2).bcast([128, nt, 4, 8]), ALU.mult)
    P.tt("dve", A2, ohg.unsq(3).bcast([128, nt, 4, 8]), oh2.unsq(2).bcast([128, nt, 4, 8]), ALU.mult)
    A1f = A1.rearrange("p t g e -> p t (g e)")
    A2f = A2.rearrange("p t g e -> p t (g e)")
    P.tt("dve", Ab, A1f, A2f, ALU.add)
    P.memset("dve", Acum[:, 0, :], 0.0)
    for t in range(nt):
        P.tt("dve", Acum[:, t + 1, :], Acum[:, t, :], Ab[:, t, :], ALU.add)
    psP = M.psum(2048, nt * 32, "psP")
    for t in range(nt):
        P.mm(psP[:, t * 32:(t + 1) * 32], tri, Ab[:, t, :], start=True, stop=False)
        P.mm(psP[:, t * 32:(t + 1) * 32], ones, Acum[:, t, :], start=False, stop=True)
    P.copy("dve", pos.rearrange("p t e -> p (t e)"), psP)
    for k, Ak in enumerate((A1f, A2f)):
        P.tt("dve", w32, Ak, pos, ALU.mult)
        P.reduce("dve", c1, w32)
        P.tt("dve", w32, Ak, ebase.unsq(1).bcast([128, nt, 32]), ALU.mult)
        P.reduce("dve", c2, w32)
        P.ts("dve", c3, c1, CAP - 0.5, None, ALU.is_lt)
        P.tt("dve", c2, c2, c1, ALU.add)
        P.ts("dve", c2, c2, float(TRASH), None, ALU.subtract)
        P.tt("dve", c2, c2, c3, ALU.mult)
        P.ts("dve", c2, c2, float(TRASH), None, ALU.add)
        P.tt("dve", gates[:, :, k], gates[:, :, k], c3, ALU.mult)
        P.copy("dve", slots_i[:, 0:nt, k], c2)
    istg = [M.alloc([8], I32, "istg%d" % i) for i in range(4)]
    for t in range(nt):
        for k in range(2):
            ist = istg[(2 * t + k) % 4]
            P.copy("pool", ist[:, 0:1], slots_i[:, t, k:k + 1])
            _idma(P, SLOT_T, tokid[:, t:t + 1], out_off=ist[:, 0:1], sem=ist.buf.name, accum_w=True)
    if dbg is not None:
        dbg["LG"], dbg["gates"], dbg["slots_i"], dbg["pos"] = LG, gates, slots_i[:, 0:nt, :], pos
    P.barrier()
    M.off = keep_off

    NH = CAP // 512
    Wup = [M.alloc([8, 1024], BF16, "Wup%d" % i) for i in range(2)]
    Wdn = [M.alloc([4, 1024], BF16, "Wdn%d" % i) for i in range(2)]
    idx = [M.alloc([CAPB], I32, "idx%d" % i) for i in range(2)]
    Xg = [M.alloc([CAPB, 1024], BF16, "Xg%d" % i) for i in range(2)]
    XT = M.alloc([8, CAP], BF16, "XT")
    actT = M.alloc([4, CAP], BF16, "actT")
    sg = [M.alloc([512], F32, "sg%d" % i) for i in range(2)]
    Ysb = [M.alloc([1024], F32, "Ysb%d" % i) for i in range(2)]
    psXh = [M.psum(0, 256, "psXa", BF16), M.psum(3584, 256, "psXb", BF16)]
    psU = [(M.psum(512, 512, "psG0"), M.psum(1024, 512, "psU0")),
           (M.psum(1536, 512, "psG1"), M.psum(2048, 512, "psU1"))]
    psYh = [M.psum(2560, 512, "psY0"), M.psum(3072, 512, "psY1")]
    Yv = Y[0:NSLOT, :].rearrange("(e p j) d -> e p j d", e=32, p=128)
    SLv = SLOT_T[0:NSLOT, :].rearrange("(e p j) o -> e p (j o)", e=32, p=128)

    def load_expert(e):
        s = e % 2
        P.dma("pool", Wup[s], io["moe_w_up"][wl, e].rearrange("(kc p) n -> p kc n", p=128))
        P.dma("pool", Wdn[s], io["moe_w_down"][wl, e].rearrange("(kc p) n -> p kc n", p=128))
        P.dma("sp", idx[s], SLv[e])
        for j in range(CAPB):
            _idma(P, Xg[s][:, j, :], H2, in_off=idx[s][:, j:j + 1], sem="Xg%d" % s, accum_w=True)

    load_expert(0)
    load_expert(1)
    cnt = 0
    for e in range(32):
        s = e % 2
        for j in range(CAPB):
            for hx in range(2):
                for k4 in range(4):
                    kc = hx * 4 + k4
                    P.tr(psXh[hx][:, k4 * 128:(k4 + 1) * 128], Xg[s][:, j, kc * 128:(kc + 1) * 128], idb)
                P.copy("act" if hx == 0 else "dve", XT[:, hx * 4:(hx + 1) * 4, j * 128:(j + 1) * 128],
                       psXh[hx].rearrange("p (k t) -> p k t", k=4))
        for hf in range(NH):
            cs = slice(hf * 512, (hf + 1) * 512)
            for hc in range(4):
                pg_, pu_ = psU[cnt % 2]
                for kc in range(8):
                    P.mm(pg_, Wup[s][:, kc, hc * 128:(hc + 1) * 128], XT[:, kc, cs], start=(kc == 0), stop=(kc == 7))
                for kc in range(8):
                    P.mm(pu_, Wup[s][:, kc, 512 + hc * 128:512 + (hc + 1) * 128], XT[:, kc, cs],
                         start=(kc == 0), stop=(kc == 7))
                P.act(sg[cnt % 2], pg_, AF.Silu)
                P.tt("dve", actT[:, hc, cs], sg[cnt % 2], pu_, ALU.mult)
                cnt += 1
        for j in range(CAPB):
            ysb = Ysb[j % 2]
            for n in range(2):
                for hc in range(4):
                    P.mm(psYh[n], actT[:, hc, j * 128:(j + 1) * 128],
                         Wdn[s][:, hc, n * 512:(n + 1) * 512], start=(hc == 0), stop=(hc == 3))
                P.copy("act" if n == 0 else "dve", ysb[:, n * 512:(n + 1) * 512], psYh[n])
            P.dma("sp", Yv[e][:, j, :], ysb, accum_w=True)
        if e + 2 < 32:
            load_expert(e + 2)
    P.barrier()
    M.off = keep_off

    xo = [M.alloc([1024], F32, "cxo%d" % i) for i in range(3)]
    y1 = [M.alloc([1024], F32, "cy1%d" % i) for i in range(3)]
    y2 = [M.alloc([1024], F32, "cy2%d" % i) for i in range(3)]
    cidx = [M.alloc([8], I32, "cidx%d" % i) for i in range(3)]

    def ld_c(t):
        if t < ntile:
            s_ = t % 3
            P.dma("sp", xo[s_], x_in[t * 128:(t + 1) * 128, :])
            P.copy("pool", cidx[s_][:, 0:1], slots_i[:, t, 0:1])
            P.copy("pool", cidx[s_][:, 1:2], slots_i[:, t, 1:2])
            _idma(P, y1[s_], Y, in_off=cidx[s_][:, 0:1])
            _idma(P, y2[s_], Y, in_off=cidx[s_][:, 1:2])

    ld_c(0)
    ld_c(1)
    for t in range(ntile):
        s = t % 3
        P.ts("dve", y1[s], y1[s], gates[:, t, 0:1], None, ALU.mult)
        P.stt("dve", y1[s], y2[s], gates[:, t, 1:2], y1[s], ALU.mult, ALU.add)
        P.tt("dve", y1[s], y1[s], gate_b, ALU.mult)
        P.tt("dve", xo[s], xo[s], y1[s], ALU.add)
        P.dma("sp", x_out[t * 128:(t + 1) * 128, :], xo[s], accum_w=True)
        ld_c(t + 2)
    P.barrier()


MOE_SPECS = {
    "x": ([SH, D], F32), "ident": ([128, 128], F32), "c_a": ([128, 8], F32),
    "ada_w": ([2, D, 6 * D], F32), "ada_b": ([2, 6 * D], F32), "ada_b_a": ([2, 128, 48], F32),
    "norm_ffn": ([2, D], F32), "moe_wr": ([1, D, 36], F32), "moe_rb": ([1, 36], F32),
    "tri": ([128, 128], F32), "ebase": ([1, 32], F32), "tokid": ([128, NTO], I32),
    "moe_w_up": ([1, 32, D, D], F32), "moe_w_down": ([1, 32, 512, D], F32),
}


def build_moe(l, ntile=NTO, debug=False):
    nc = bass.Bass("TRN2", target_bir_lowering=False)
    es = ExitStack()
    with es:
        P = Prog(nc, es)
        M = Mem(nc, es, P, SBUF_WORDS)
        io = declare_inputs(nc, P, MOE_SPECS)
        x_out = dram(nc, P, "x2", [SH, D], F32, kind="ExternalOutput")
        dbg = {} if debug else None
        emit_moe(P, M, io, l, io["x"], x_out, ntile, dbg, wl=0)
        if debug:
            for k, dt in (("LG", F32), ("gates", F32), ("slots_i", I32), ("pos", F32)):
                src = dbg[k]
                shp = list(src.shape)
                o = dram(nc, P, "dbg_" + k, shp, dt, kind="ExternalOutput")
                P.dma("sp", o, src, sem="dbgout")
        P.barrier()
        P.emit()
    return nc


def host_inputs_moe(inputs, core, x_own, l):
    b = core // 2
    wr = np.concatenate([inputs["moe_w_group"][l],
                         np.transpose(inputs["moe_w_expert"][l], (1, 0, 2)).reshape(D, 32)], axis=1)[None].astype(np.float32)
    rb = np.concatenate([inputs["moe_b_group"][l], inputs["moe_b_expert"][l].reshape(32)])[None].astype(np.float32)
    return {
        "x": np.ascontiguousarray(x_own, dtype=np.float32),
        "ident": np.eye(128, dtype=np.float32),
        "c_a": a_layout(inputs["c"][b]),
        "ada_w": np.asarray(inputs["ada_w"], np.float32),
        "ada_b": np.asarray(inputs["ada_b"], np.float32),
        "ada_b_a": np.stack([a_layout(inputs["ada_b"][k]) for k in range(2)]),
        "norm_ffn": np.asarray(inputs["norm_ffn"], np.float32),
        "moe_wr": wr, "moe_rb": rb,
        "tri": np.triu(np.ones((128, 128), np.float32), 1),
        "ebase": (np.arange(32, dtype=np.float32) * CAP).reshape(1, 32),
        "tokid": (np.arange(NTO)[None, :] * 128 + np.arange(128)[:, None]).astype(np.int32),
        "moe_w_up": np.asarray(inputs["moe_w_up"][l:l + 1], np.float32),
        "moe_w_down": np.asarray(inputs["moe_w_down"][l:l + 1], np.float32),
    }


C_IN = 3072
LAMBDA_INIT1 = 0.8 - 0.6 * math.exp(-0.3 * 1)
GW = 1280
HKW = 1152


def t5_bucket_np(rel):
    rel = np.asarray(rel, np.int64)
    nb = 16
    max_exact = 8
    ret = np.where(rel > 0, nb, 0)
    n = np.abs(rel)
    nf = np.maximum(n, 1).astype(np.float32)
    large = max_exact + (np.log(nf / np.float32(max_exact)) / np.float32(math.log(128 / max_exact))
                         * np.float32(nb - max_exact)).astype(np.int32)
    large = np.minimum(large, nb - 1)
    return ret + np.where(n < max_exact, n, large)


def make_bias_onehots(half):
    m = np.arange(GW)
    r_own = 639 - m
    d32 = 512 - half * 8192
    d63 = 8064 - half * 8192
    mm_ = np.arange(640)
    r32 = 127 - mm_ + d32
    r63 = 127 - mm_ + d63
    rel = np.concatenate([r_own, r32, r63])
    bk = t5_bucket_np(rel)
    oh = (bk[None, :] == np.arange(32)[:, None]).astype(np.float32)
    sel = np.zeros((32, 3), np.float32)
    sel[15, 0] = 1.0
    sel[31, 1] = 1.0
    sel[31 if half == 0 else 15, 2] = 1.0
    return oh, sel


def emit_l1_mixer(P, M, io, x_in, x_out, ntile_all=NT, ntile_own=NTO, dbg=None, tile_hook=None):
    nc = P.nc
    xin = x_in if callable(x_in) else (lambda t: x_in[t * 128:(t + 1) * 128, :])
    M.reset()
    cst = emit_consts(P, M, io)
    idb = cst["idb"]
    modA, modB = emit_mod(P, M, io, 1, need_a=[0, 1], need_b=[2])
    shift_a, scale_a, gate_b = modA[0], modA[1], modB[2]
    nm_a = M.alloc([8], F32, "nm_a")
    P.dma("sp", nm_a, io["norm_mix_a"][1])
    gs_a = M.alloc([8], F32, "gs_a")
    P.stt("dve", gs_a, scale_a, 1.0, nm_a, ALU.add, ALU.mult)
    g_q = M.alloc([2, 64], F32, "g_q1")
    g_k = M.alloc([2, 64], F32, "g_k1")
    neglam = M.alloc([1], F32, "neglam")
    sublnc = M.alloc([1], F32, "sublnc")
    bcol = M.alloc([3, 8], F32, "bcol")
    Hk = M.alloc([8, HKW], BF16, "Hk")
    Hx = [M.alloc([8, 512], BF16, "Hx%d" % i) for i in range(2)]
    Jb = M.alloc([128], BF16, "Jb")
    ones = M.alloc([128], BF16, "ones1")
    keep_attn = M.off
    Wp = M.alloc([8, C_IN], BF16, "Wp1")
    bias_bc = M.alloc([C_IN], F32, "bias_bc1")
    keep_off = M.off
    P.memset("dve", ones, 1.0)

    jf = M.alloc([128], F32, "jf")
    P.dma("sp", jf, io["jmat"])
    P.copy("dve", Jb, jf)
    P.dma("sp", g_q.rearrange("p c d -> p (c d)"), io["c_qn"].pbc())
    P.ts("dve", g_q, g_q, 0.125, None, ALU.mult)
    P.dma("sp", g_k.rearrange("p c d -> p (c d)"), io["c_kn"].pbc())
    lamv = M.alloc([4, 64], F32, "lamv")
    P.dma("sp", lamv.rearrange("p a d -> p (a d)"), io["c_lam"].pbc())
    lw = M.alloc([2, 64], F32, "lw")
    P.tt("dve", lw[:, 0, :], lamv[:, 0, :], lamv[:, 1, :], ALU.mult)
    P.tt("dve", lw[:, 1, :], lamv[:, 2, :], lamv[:, 3, :], ALU.mult)
    ls = M.alloc([2], F32, "ls")
    P.reduce("dve", ls, lw)
    P.act(ls, ls, AF.Exp)
    P.tt("dve", neglam, ls[:, 1:2], ls[:, 0:1], ALU.subtract)
    P.ts("dve", neglam, neglam, -LAMBDA_INIT1, None, ALU.add)
    P.dma("sp", sublnc, io["c_subln_a"])
    P.ts("dve", sublnc, sublnc, 1.0 - LAMBDA_INIT1, None, ALU.mult)
    rb = M.alloc([8], F32, "rb", parts=32)
    P.dma("sp", rb, io["rel_bias"])
    oh = M.alloc([GW + 1280], F32, "oh", parts=32)
    P.dma("sp", oh, io["bias_oh"])
    sel = M.alloc([3], F32, "sel", parts=32)
    P.dma("sp", sel, io["bias_sel"])
    selrep = M.alloc([3, 128], F32, "selrep", parts=32)
    P.copy("dve", selrep, sel.unsq(2).bcast([32, 3, 128]))
    psc = M.psum(0, 24, "psc")
    for i in range(3):
        P.mm(psc[:, i * 8:(i + 1) * 8], selrep[:, i, :], rb)
    P.copy("dve", bcol.rearrange("p a h -> p (a h)"), psc)
    GT = GW + 1280
    Gd = dram(nc, P, "Gd", [8, GT], F32)
    gsb = M.alloc([GT], F32, "gsb", parts=8)
    for c0 in range(0, GT, 512):
        c1 = min(GT, c0 + 512)
        psg = M.psum(512 + (c0 // 512) * 512, c1 - c0, "psg%d" % c0)
        P.mm(psg[0:8, :], rb, oh[:, c0:c1])
        P.copy("dve", gsb[:, c0:c1], psg[0:8, :])
    P.dma("sp", Gd, gsb)
    hkf = M.alloc([HKW], F32, "hkf")
    for h in range(8):
        src = T(bass.AP(Gd.ap.tensor, h * GT, [[1, 128], [1, HKW]]), Gd.buf)
        P.dma("sp", hkf, src)
        P.copy("dve", Hk[:, h, :], hkf)
        for xi in range(2):
            src = T(bass.AP(Gd.ap.tensor, h * GT + GW + xi * 640, [[1, 128], [1, 512]]), Gd.buf)
            P.dma("sp", hkf[:, 0:512], src)
            P.copy("dve", Hx[xi][:, h, :], hkf[:, 0:512])
    P.barrier()
    M.off = keep_off
    shrep = M.alloc([8, 128], BF16, "shift_rep1")
    P.copy("dve", shrep, shift_a.unsq(2).bcast([128, 8, 128]))
    Wo = M.alloc([8, C_IN], BF16, "Wo1")
    stg = [M.alloc([C_IN], F32, "w1stg%d" % i) for i in range(2)]
    for kc in range(8):
        s = stg[kc % 2]
        P.dma("sp", s, io["c_w_in"][kc * 128:(kc + 1) * 128, :])
        P.act(Wp[:, kc, :], s, AF.Copy, scale=gs_a[:, kc:kc + 1])
        P.copy("pool", Wo[:, kc, :], s)
    for n0 in range(0, C_IN, 512):
        ps = M.psum((n0 // 512) * 512, 512, "ps1bias%d" % n0)
        for kc in range(8):
            P.mm(ps, shrep[:, kc, :], Wo[:, kc, n0:n0 + 512], start=(kc == 0), stop=(kc == 7))
        P.copy("dve", bias_bc[:, n0:n0 + 512], ps)
    P.barrier()
    M.off = keep_off

    QT1 = dram(nc, P, "QT1", [8, 128, SH], BF16)
    KT1 = dram(nc, P, "KT1", [8, 128, S], BF16)
    V1 = dram(nc, P, "V1", [8, 128, NT, 128], BF16)
    AO = dram(nc, P, "AO1", [1024, SH], BF16)

    xt = [M.alloc([1024], F32, "x1t%d" % i) for i in range(2)]
    junk = M.alloc([1024], BF16, "junk1")
    xb = M.alloc([1024], BF16, "xb1")
    xT = M.alloc([1024], BF16, "xT1")
    st = M.alloc([8], F32, "stats1")
    qk = M.alloc([16, 64], F32, "qk1")
    sq = M.alloc([16, 64], F32, "sq1")
    ssh = M.alloc([16], F32, "ssh1")
    rsh = M.alloc([16], F32, "rsh1")
    qkf = M.alloc([16, 64], BF16, "qkf1")
    QTs = [M.alloc([8, 512], BF16, "Q1s%d" % i) for i in range(2)]
    KTs = [M.alloc([8, 512], BF16, "K1s%d" % i) for i in range(2)]
    Vs = [M.alloc([8, 8, 128], BF16, "V1s%d" % i) for i in range(2)]
    psT = M.psum(0, 512, "ps1T", BF16)
    psP = [M.psum(512 * (1 + i), 512, "ps1P%d" % i) for i in range(6)]
    psO = M.psum(3584, 512, "ps1O", BF16)
    for t in range(ntile_all):
        own = t < ntile_own
        if tile_hook is not None:
            tile_hook(t)
        x_t = xt[t % 2]
        if t == 0:
            P.dma("sp", xt[0], xin(0))
        if t + 1 < ntile_all:
            P.dma("sp", xt[(t + 1) % 2], xin(t + 1))
        P.act(junk, x_t, AF.Square, accum=st[:, 0:1])
        emit_rstd(P, st[:, 0:1], st[:, 1:2], 1024)
        rstd = st[:, 1:2]
        P.copy("pool", xb, x_t)
        for kc in range(8):
            P.tr(psT[:, kc * 128:(kc + 1) * 128], xb[:, kc * 128:(kc + 1) * 128], idb)
        P.copy("act", xT, psT)
        cols = ([0, 512] if own else []) + [1024, 1536, 2048, 2560]
        for i, c0 in enumerate(cols):
            for kc in range(8):
                P.mm(psP[i], xT[:, kc * 128:(kc + 1) * 128], Wp[:, kc, c0:c0 + 512], start=(kc == 0), stop=(kc == 7))
        slot4, j4 = (t // 4) % 2, t % 4
        slot8, j8 = (t // 8) % 2, t % 8
        bi = 0
        for which in (["q"] if own else []) + ["k"]:
            c0 = 0 if which == "q" else 1024
            gain = g_q if which == "q" else g_k
            qflat = qk.rearrange("p h d -> p (h d)")
            for hf in range(2):
                P.stt("dve", qflat[:, hf * 512:(hf + 1) * 512], psP[bi], rstd,
                      bias_bc[:, c0 + hf * 512:c0 + (hf + 1) * 512], ALU.mult, ALU.add)
                bi += 1
            P.tt("pool", sq, qk, qk, ALU.mult)
            P.reduce("dve", ssh, sq)
            emit_rstd(P, ssh, rsh, 64)
            P.tt("dve", qk, qk, rsh.unsq(2).bcast([128, 16, 64]), ALU.mult)
            P.tt("pool", qkf.rearrange("p (h c) d -> p h c d", c=2), qk.rearrange("p (h c) d -> p h c d", c=2),
                 gain.unsq(1).bcast([128, 8, 2, 64]), ALU.mult)
            for h in range(8):
                P.tr(psO[:, h * 128:(h + 1) * 128], qkf[:, 2 * h:2 * h + 2, :].rearrange("p c d -> p (c d)"), idb)
            dst = (QTs if which == "q" else KTs)[slot4]
            P.copy("act", dst[:, :, j4 * 128:(j4 + 1) * 128], psO.rearrange("p (h t) -> p h t", h=8))
        vdst = Vs[slot8][:, j8, :, :].rearrange("p h d -> p (h d)")
        for hf in range(2):
            P.stt("dve", vdst[:, hf * 512:(hf + 1) * 512], psP[bi], rstd,
                  bias_bc[:, 2048 + hf * 512:2048 + (hf + 1) * 512], ALU.mult, ALU.add)
            bi += 1
        if j4 == 3:
            c0 = (t // 4) * 512
            if own:
                P.dma("sp", QT1[:, :, c0:c0 + 512].rearrange("h p t -> p h t"), QTs[slot4], accum_w=True)
            P.dma("sp", KT1[:, :, c0:c0 + 512].rearrange("h p t -> p h t"), KTs[slot4], accum_w=True)
        if j8 == 7 or t == ntile_all - 1:
            g8 = t // 8
            n8 = j8 + 1
            for hh in range(8):
                P.dma("sp", V1[hh][:, g8 * 8:g8 * 8 + n8, :], Vs[slot8][:, 0:n8, hh, :], accum_w=True)
    if dbg is not None:
        dbg["QT1"], dbg["KT1"], dbg["V1"] = QT1, KT1, V1
    P.barrier()
    M.off = keep_attn
    off_proj = keep_attn

    nkt = ntile_all
    nqt = ntile_own // 4
    nko = ntile_own
    KT = [M.alloc([nkt * 128], BF16, "K1T%d" % i) for i in range(2)]
    Vh = [M.alloc([nkt, 128], BF16, "V1h%d" % i) for i in range(2)]
    QT = [M.alloc([nqt * 512], BF16, "Q1T%d" % i) for i in range(2)]
    OT = [M.alloc([nqt * 512], BF16, "O1T%d" % i) for i in range(2)]
    QTz = [[M.alloc([nqt * 512], BF16, "Q1z%d_%d" % (i, c)) for c in range(2)] for i in range(2)]
    for i in range(2):
        for c in range(2):
            P.memset("pool", QTz[i][c], 0.0)
    GSZ = 2
    Pb = [M.alloc([GSZ * 512], BF16, "P1b%d" % i) for i in range(2)]
    Psum = [M.alloc([512], BF16, "P1sum%d" % i) for i in range(2)]
    rl = [M.alloc([512], F32, "rl1_%d" % i) for i in range(2)]
    o0s = M.alloc([512], F32, "o0s")
    o1s = M.alloc([512], F32, "o1s")
    sqb = M.alloc([512], BF16, "sqb")
    rsd = M.alloc([512], F32, "rsd")
    Sg = [M.psum(0, GSZ * 512, "S1g0"), M.psum(1024, GSZ * 512, "S1g1")]
    Oa = [M.psum(2048, 512, "O1a0"), M.psum(3072, 512, "O1a1")]
    La = [M.psum(2560, 512, "L1a0"), M.psum(3584, 512, "L1a1")]

    def seglist(qt):
        segs = []
        lo, hi = 4 * qt - 1, 4 * qt + 4
        left = [k for k in range(0, max(lo, 0))]
        band = [k for k in range(max(lo, 0), min(hi, nko - 1) + 1)]
        right = [k for k in range(min(hi, nko - 1) + 1, nko)]
        other = list(range(nko, nkt))
        cross = []
        if nkt > nko:
            if qt == nqt - 1 and nko in other:
                other.remove(nko)
                cross.append((nko, 0))
            if qt == 0 and (nkt - 1) in other:
                other.remove(nkt - 1)
                cross.append((nkt - 1, 1))
        if left:
            segs.append(("c", 0, left))
        if band:
            segs.append(("b", None, [(k, "own", 512 - (k * 128 - qt * 512)) for k in band]))
        if cross:
            segs.append(("b", None, [(k, xi, 0) for k, xi in cross]))
        if right:
            segs.append(("c", 1, right))
        if other:
            segs.append(("c", 2, other))
        return segs

    tasks = []
    for h in range(8):
        for qt in range(nqt):
            for c in range(2):
                gl = []
                for kind, arg, lst in seglist(qt):
                    for i in range(0, len(lst), GSZ):
                        gl.append((kind, arg, lst[i:i + GSZ]))
                for gi, (kind, arg, lst) in enumerate(gl):
                    tasks.append((h, qt, c, gi, len(gl), kind, arg, lst))

    def load_head(h):
        s = h % 2
        P.dma("sp", KT[s], KT1[h][:, 0:nkt * 128])
        P.dma("sp", Vh[s], V1[h][:, 0:nkt])
        P.dma("sp", QT[s], QT1[h][:, 0:nqt * 512])
        for c in range(2):
            P.copy("pool", QTz[s][c][c * 64:(c + 1) * 64, :], QT[s][c * 64:(c + 1) * 64, :])

    def qk_mm(i):
        h, qt, c, gi, ng, kind, arg, lst = tasks[i]
        s = h % 2
        pr = slice(c * 64, (c + 1) * 64)
        for j, item in enumerate(lst):
            kt = item if kind == "c" else item[0]
            P.mm(Sg[i % 2][:, j * 512:(j + 1) * 512], KT[s][:, kt * 128:(kt + 1) * 128],
                 QTz[s][c][:, qt * 512:(qt + 1) * 512], start=True, stop=(kind == "c"))
            if kind == "b":
                _, tab, off = item
                src = Hk[:, h, off:off + 512] if tab == "own" else Hx[tab][:, h, :]
                P.mm(Sg[i % 2][:, j * 512:(j + 1) * 512], Jb, src, start=False, stop=True)

    pend = {}
    deferred = []

    def ex_pv(i):
        h, qt, c, gi, ng, kind, arg, lst = tasks[i]
        s = h % 2
        n = len(lst)
        pb = Pb[i % 2]
        if kind == "c":
            P.act(pb[:, 0:n * 512], Sg[i % 2][:, 0:n * 512], AF.Exp, bias=bcol[:, arg, h:h + 1])
        else:
            P.act(pb[:, 0:n * 512], Sg[i % 2][:, 0:n * 512], AF.Exp)
        if n == 1:
            lsrc = pb[:, 0:512]
        else:
            lsrc = Psum[i % 2]
            P.tt("dve", lsrc, pb[:, 0:512], pb[:, 512:1024], ALU.add)
        oa, la = Oa[c], La[c]
        for j, item in enumerate(lst):
            kt = item if kind == "c" else item[0]
            pj = pb[:, j * 512:(j + 1) * 512]
            P.mm(oa, Vh[s][:, kt, :], pj, start=(gi == 0 and j == 0), stop=(gi == ng - 1 and j == n - 1))
        if gi > 0:
            P.mm(la, ones, pend["lsrc"], start=(gi == 1), stop=False)
        pend["lsrc"] = lsrc
        if gi == ng - 1:
            P.mm(la, ones, lsrc, start=(gi == 0), stop=True)

            def norm(c=c, oa=oa, la=la):
                P.act(rl[c], la, AF.Ln)
                P.act(rl[c], rl[c], AF.Exp, scale=-1.0)
                if c == 0:
                    P.tt("dve", o0s, oa, rl[0], ALU.mult)
                else:
                    P.tt("dve", o1s, oa, rl[1], ALU.mult)
                    P.stt("dve", o1s, o1s, neglam[:, 0:1], o0s, ALU.mult, ALU.add)
                    P.tt("pool", sqb, o1s, o1s, ALU.mult)
            deferred.append((i + 3, norm))
            if c == 1:
                def fin(h=h, qt=qt, s=s, la=la):
                    P.mm(la, ones, sqb)
                    P.act(rsd, la, AF.Ln, scale=1.0 / 128.0, bias=EPS)
                    P.act(rsd, rsd, AF.Exp, scale=-0.5)
                    P.tt("dve", o1s, o1s, rsd, ALU.mult)
                    P.ts("dve", OT[s][:, qt * 512:(qt + 1) * 512], o1s, sublnc[:, 0:1], None, ALU.mult)
                    if qt == nqt - 1:
                        P.dma("sp", AO[h * 128:(h + 1) * 128, 0:nqt * 512], OT[s])
                        if h + 2 < 8:
                            load_head(h + 2)
                deferred.append((i + 11, fin))

    def run_deferred(i, flush=False):
        while deferred and (flush or deferred[0][0] <= i):
            deferred.pop(0)[1]()

    load_head(0)
    load_head(1)
    for i in range(len(tasks) + 1):
        if i < len(tasks):
            qk_mm(i)
        if i >= 1:
            ex_pv(i - 1)
            run_deferred(i - 1)
    run_deferred(0, flush=True)
    P.barrier()
    M.off = off_proj

    Wout = M.alloc([8, 1024], BF16, "Wout1")
    wst = [M.alloc([1024], F32, "wo1st%d" % i) for i in range(2)]
    for kc in range(8):
        P.dma("sp", wst[kc % 2], io["c_w_out"][kc * 128:(kc + 1) * 128, :])
        P.tt("dve", Wout[:, kc, :], wst[kc % 2], gate_b, ALU.mult)
    AOs = [M.alloc([8, 512], BF16, "AO1s%d" % i) for i in range(2)]
    xo = [M.alloc([1024], F32, "x1o%d" % i) for i in range(4)]
    ys = [M.psum(i * 1024, 1024, "y1%d" % i) for i in range(4)]

    def ld_ao(g):
        if g * 4 < ntile_own:
            P.dma("sp", AOs[g % 2], AO[:, g * 512:(g + 1) * 512].rearrange("(fc p) t -> p fc t", p=128))

    def ld_x(t):
        if t < ntile_own:
            P.dma("sp", xo[t % 4], xin(t))

    ld_ao(0)
    ld_x(0)
    ld_x(1)
    for t in range(ntile_own):
        g4, j4 = t // 4, t % 4
        a = AOs[g4 % 2]
        if j4 == 0:
            ld_ao(g4 + 1)
        ld_x(t + 2)
        x_t = xo[t % 4]
        y = ys[t % 4]
        for n in range(2):
            for fc in range(8):
                P.mm(y[:, n * 512:(n + 1) * 512], a[:, fc, j4 * 128:(j4 + 1) * 128],
                     Wout[:, fc, n * 512:(n + 1) * 512], start=(fc == 0), stop=(fc == 7))
        P.tt("dve", x_t, x_t, y, ALU.add)
        P.dma("sp", x_out[t * 128:(t + 1) * 128, :], x_t, accum_w=True)
    P.barrier()


L1_SPECS = {
    "x": ([S, D], F32), "ident": ([128, 128], F32), "jmat": ([128, 128], F32), "c_a": ([128, 8], F32),
    "ada_w": ([2, D, 6 * D], F32), "ada_b": ([2, 6 * D], F32), "ada_b_a": ([2, 128, 48], F32),
    "norm_mix_a": ([2, 128, 8], F32), "rel_bias": ([32, 8], F32),
    "bias_oh": ([32, GW + 1280], F32), "bias_sel": ([32, 3], F32),
    "c_w_in": ([D, C_IN], F32), "c_qn": ([1, 128], F32), "c_kn": ([1, 128], F32), "c_lam": ([1, 256], F32),
    "c_subln_a": ([128, 1], F32), "c_w_out": ([D, D], F32),
}


def build_l1_mixer(ntile_all=NT, ntile_own=NTO, debug=False):
    nc = bass.Bass("TRN2", target_bir_lowering=False)
    es = ExitStack()
    with es:
        P = Prog(nc, es)
        M = Mem(nc, es, P, SBUF_WORDS)
        io = declare_inputs(nc, P, L1_SPECS)
        x_out = dram(nc, P, "x1", [SH, D], F32, kind="ExternalOutput")
        dbg = {} if debug else None
        emit_l1_mixer(P, M, io, io["x"], x_out, ntile_all, ntile_own, dbg)
        if debug:
            for k in ("QT1", "KT1", "V1"):
                src = dbg[k]
                o = dram(nc, P, "dbg_" + k, list(src.shape), BF16, kind="ExternalOutput")
                P.dma("sp", o, src, sem="dbgout")
        P.barrier()
        P.emit()
    return nc


def host_inputs_l1(inputs, core, x_rot):
    b, half = core // 2, core % 2
    oh, sel = make_bias_onehots(half)
    return {
        "x": np.ascontiguousarray(x_rot, dtype=np.float32),
        "ident": np.eye(128, dtype=np.float32),
        "jmat": np.ascontiguousarray(np.eye(128, dtype=np.float32)[::-1]),
        "c_a": a_layout(inputs["c"][b]),
        "ada_w": np.asarray(inputs["ada_w"], np.float32),
        "ada_b": np.asarray(inputs["ada_b"], np.float32),
        "ada_b_a": np.stack([a_layout(inputs["ada_b"][l]) for l in range(2)]),
        "norm_mix_a": np.stack([a_layout(inputs["norm_mix"][l]) for l in range(2)]),
        "rel_bias": np.asarray(inputs["rel_bias"], np.float32),
        "bias_oh": oh, "bias_sel": sel,
        "c_w_in": np.asarray(inputs["c_w_in"][0], np.float32),
        "c_qn": np.asarray(inputs["c_qn"][0], np.float32).reshape(1, 128),
        "c_kn": np.asarray(inputs["c_kn"][0], np.float32).reshape(1, 128),
        "c_lam": np.concatenate([inputs["c_lam_q1"][0], inputs["c_lam_k1"][0],
                                 inputs["c_lam_q2"][0], inputs["c_lam_k2"][0]]).astype(np.float32).reshape(1, 256),
        "c_subln_a": np.asarray(inputs["c_subln"][0], np.float32).reshape(128, 1),
        "c_w_out": np.asarray(inputs["c_w_out"][0], np.float32),
    }


def fused_specs():
    sp = {}
    sp.update({
        "x": ([S, D], F32), "ident": ([128, 128], F32), "c_a": ([128, 8], F32),
        "ada_w": ([2, D, 6 * D], F32), "ada_b": ([2, 6 * D], F32), "ada_b_a": ([2, 128, 48], F32),
        "norm_mix_a": ([2, 128, 8], F32), "rt": ([S, 192], F32),
        "ab_w_in": ([D, AB_IN], F32), "ab_q_a_norm_a": ([128, 4], F32), "ab_w_qb": ([512, 768], F32),
        "ab_kv_a_norm_a": ([128, 2], F32), "ab_w_kvb": ([256, 1024], F32),
        "ab_mla_qn": ([1, 96], F32), "ab_mla_kn": ([1, 96], F32), "ab_gqa_qn": ([1, 64], F32),
        "ab_gqa_kn": ([1, 64], F32), "ab_w_out": ([D, D], F32),
    })
    sp.update({k: v for k, v in L1_SPECS.items() if k != "x"})
    sp.update({
        "norm_ffn": ([2, D], F32), "moe_wr": ([2, D, 36], F32), "moe_rb": ([2, 36], F32),
        "tri": ([128, 128], F32), "ebase": ([1, 32], F32), "tokid": ([128, NTO], I32),
        "moe_w_up": ([2, 32, D, D], F32), "moe_w_down": ([2, 32, 512, D], F32),
        "other_off": ([1, 1], I32),
    })
    return sp


def build_fused(na=NT, no=NTO, parts=(1, 1, 1, 1, 1)):
    nc = bass.Bass("TRN2", target_bir_lowering=False)
    es = ExitStack()
    with es:
        P = Prog(nc, es)
        M = Mem(nc, es, P, SBUF_WORDS)
        reg = es.enter_context(nc.gpsimd.register("r_off"))
        io = declare_inputs(nc, P, fused_specs())
        out = dram(nc, P, "out", [SH, D], F32, kind="ExternalOutput")
        XA = dram(nc, P, "XA", [SH, D], F32)
        XBf = dram(nc, P, "XBf", [S, D], F32)
        XG = dram(nc, P, "XG", [S, D], F32)
        XC = dram(nc, P, "XC", [SH, D], F32)
        if parts[0]:
            emit_l0_mixer(P, M, io, io["x"], XA, na, no)
        if parts[1]:
            emit_moe(P, M, io, 0, XA, XBf[0:SH, :], no)
        XB_own = T(XBf.ap[0:SH, :], XBf.buf)
        XB_oth = T(XBf.ap[SH:S, :], P.buf("XB_oth"))
        M.reset()
        osb = M.alloc([8], I32, "osb", parts=1)
        P.dma("pool", osb[:, 0:1], io["other_off"])
        o1 = osb.ap[0:1, 0:1]
        P.op("pool", lambda e: e.reg_load(reg, o1), reads=[osb.buf], noinc=True)
        CH = 256
        NCH = SH // CH
        P.bg.add("ccsem")
        for ci in range(NCH):
            in_ap = XBf.ap[ci * CH:(ci + 1) * CH, :]
            out_ap = XG.ap[ci * 2 * CH:(ci + 1) * 2 * CH, :]
            P.op("pool", (lambda e, a=in_ap, b=out_ap: e.collective_compute(
                "AllGather", ALU.bypass, replica_groups=[[0, 1], [2, 3], [4, 5], [6, 7]], ins=[a], outs=[b])),
                reads=[XBf.buf], writes=[XG.buf], dma="ccsem", dma_inc=1, accum_w=True)
        P.barrier()

        def xrows(t):
            if t < NTO:
                return XB_own[t * 128:(t + 1) * 128, :]
            return XB_oth[(t - NTO) * 128:(t - NTO + 1) * 128, :]

        def hook(t):
            if t == min(16, no - 1):
                src = T(bass.AP(XG.ap.tensor, reg, [[2 * CH * D, NCH], [D, CH], [1, D]]), XG.buf)
                P.dma("pool", XB_oth.rearrange("(c r) d -> c r d", c=NCH), src, sem="xchg", accum_w=True)
                P.bg.discard("ccsem")

        if parts[3]:
            emit_l1_mixer(P, M, io, xrows, XC, na, no, tile_hook=hook)
        if parts[4]:
            emit_moe(P, M, io, 1, XC, out, no)
        P.barrier()
        P.emit()
    return nc


def host_inputs_fused(inputs, core, shared):
    b, half = core // 2, core % 2
    oh, sel = make_bias_onehots(half)
    d = dict(shared)
    d.update({
        "x": np.ascontiguousarray(_rot(np.asarray(inputs["x"][b], np.float32), half)),
        "c_a": a_layout(inputs["c"][b]),
        "rt": make_rt(half),
        "bias_oh": oh, "bias_sel": sel,
        "other_off": np.array([[(1 - half) * 256 * D]], np.int32),
    })
    return d


def host_shared(inputs):
    f = lambda a: np.ascontiguousarray(np.asarray(a, np.float32))
    wr = np.stack([np.concatenate([inputs["moe_w_group"][l],
                                   np.transpose(inputs["moe_w_expert"][l], (1, 0, 2)).reshape(D, 32)], axis=1)
                   for l in range(2)]).astype(np.float32)
    rb = np.stack([np.concatenate([inputs["moe_b_group"][l], inputs["moe_b_expert"][l].reshape(32)])
                   for l in range(2)]).astype(np.float32)
    return {
        "ident": np.eye(128, dtype=np.float32),
        "jmat": np.ascontiguousarray(np.eye(128, dtype=np.float32)[::-1]),
        "ada_w": f(inputs["ada_w"]), "ada_b": f(inputs["ada_b"]),
        "ada_b_a": np.stack([a_layout(inputs["ada_b"][l]) for l in range(2)]),
        "norm_mix_a": np.stack([a_layout(inputs["norm_mix"][l]) for l in range(2)]),
        "ab_w_in": f(inputs["ab_w_in"][0]), "ab_q_a_norm_a": a_layout(inputs["ab_q_a_norm"][0]),
        "ab_w_qb": f(inputs["ab_w_qb"][0]), "ab_kv_a_norm_a": a_layout(inputs["ab_kv_a_norm"][0]),
        "ab_w_kvb": f(inputs["ab_w_kvb"][0]),
        "ab_mla_qn": f(inputs["ab_mla_qn"]).reshape(1, 96), "ab_mla_kn": f(inputs["ab_mla_kn"]).reshape(1, 96),
        "ab_gqa_qn": f(inputs["ab_gqa_qn"]).reshape(1, 64), "ab_gqa_kn": f(inputs["ab_gqa_kn"]).reshape(1, 64),
        "ab_w_out": f(inputs["ab_w_out"][0]),
        "rel_bias": f(inputs["rel_bias"]),
        "c_w_in": f(inputs["c_w_in"][0]),
        "c_qn": f(inputs["c_qn"][0]).reshape(1, 128), "c_kn": f(inputs["c_kn"][0]).reshape(1, 128),
        "c_lam": np.concatenate([inputs["c_lam_q1"][0], inputs["c_lam_k1"][0],
                                 inputs["c_lam_q2"][0], inputs["c_lam_k2"][0]]).astype(np.float32).reshape(1, 256),
        "c_subln_a": f(inputs["c_subln"][0]).reshape(128, 1),
        "c_w_out": f(inputs["c_w_out"][0]),
        "norm_ffn": f(inputs["norm_ffn"]), "moe_wr": wr, "moe_rb": rb,
        "tri": np.triu(np.ones((128, 128), np.float32), 1),
        "ebase": (np.arange(32, dtype=np.float32) * CAP).reshape(1, 32),
        "tokid": (np.arange(NTO)[None, :] * 128 + np.arange(128)[:, None]).astype(np.int32),
        "moe_w_up": f(inputs["moe_w_up"]), "moe_w_down": f(inputs["moe_w_down"]),
    }


_PROGS = {}


def _prog(name, fn):
    if name not in _PROGS:
        _PROGS[name] = fn()
    return _PROGS[name]


def _rot(xb, half):
    return np.concatenate([xb[half * SH:(half + 1) * SH], xb[(1 - half) * SH:(2 - half) * SH]], axis=0)


def kernel_unfused(**inputs):
    inputs = {k: np.asarray(v) for k, v in inputs.items()}
    cores = list(range(8))
    nc = _prog("l0", lambda: build_l0_mixer())
    res = run_bass_kernel_spmd(nc, [host_inputs_l0(inputs, c) for c in cores], core_ids=cores)
    x1 = [np.asarray(r["x1"]) for r in res.results]
    nc = _prog("moe0", lambda: build_moe(0))
    res = run_bass_kernel_spmd(nc, [host_inputs_moe(inputs, c, x1[c], 0) for c in cores], core_ids=cores)
    x2 = [np.asarray(r["x2"]) for r in res.results]
    xb = [np.concatenate([x2[2 * b], x2[2 * b + 1]], axis=0) for b in range(4)]
    nc = _prog("l1", lambda: build_l1_mixer())
    res = run_bass_kernel_spmd(nc, [host_inputs_l1(inputs, c, _rot(xb[c // 2], c % 2)) for c in cores], core_ids=cores)
    x3 = [np.asarray(r["x1"]) for r in res.results]
    nc = _prog("moe1", lambda: build_moe(1))
    res = run_bass_kernel_spmd(nc, [host_inputs_moe(inputs, c, x3[c], 1) for c in cores], core_ids=cores)
    x4 = [np.asarray(r["x2"]) for r in res.results]
    out = np.stack([np.concatenate([x4[2 * b], x4[2 * b + 1]], axis=0) for b in range(4)]).astype(np.float32)
    return out


def kernel(**inputs):
    inputs = {k: np.asarray(v) for k, v in inputs.items()}
    cores = list(range(8))
    nc = _prog("fused", build_fused)
    shared = host_shared(inputs)
    in_maps = [host_inputs_fused(inputs, c, shared) for c in cores]
    res = run_bass_kernel_spmd(nc, in_maps, core_ids=cores)
    xs = [np.asarray(r["out"]) for r in res.results]
    return np.stack([np.concatenate([xs[2 * b], xs[2 * b + 1]], axis=0) for b in range(4)]).astype(np.float32)
```
